# Optimizing a Trainium2 kernel written in Bass

```python
import jax, jax.numpy as jnp
from jax import lax
import numpy as np

D_MODEL = 1024
BATCH = 4
SEQ = 8192
DEPTH = 1

CHUNK = 64
HEAD_DIM = 64
SB_HEADS = 8
FOX_HEADS = 8
SB_WIDTH = SB_HEADS * HEAD_DIM
FOX_WIDTH = FOX_HEADS * HEAD_DIM
MIX_WIDTH = SB_WIDTH + FOX_WIDTH
QUERY_BLOCK = 128
OFF_Q_SB = 0
OFF_K_SB = OFF_Q_SB + SB_WIDTH
OFF_V_SB = OFF_K_SB + SB_WIDTH
OFF_Q_FX = OFF_V_SB + SB_WIDTH
OFF_K_FX = OFF_Q_FX + FOX_WIDTH
OFF_V_FX = OFF_K_FX + FOX_WIDTH
OFF_F = OFF_V_FX + FOX_WIDTH
IN_COLS = OFF_F + FOX_HEADS
N_GROUPS = 4
EXPERTS_PER_GROUP = 8
N_EXPERTS = N_GROUPS * EXPERTS_PER_GROUP
TOP_K = 2
D_EXPERT = 512
DISPATCH_BLOCK = 128
PLE_DIM = 256
ALPHA = (2 * DEPTH) ** 0.25
BETA_INIT = (8 * DEPTH) ** -0.25
LN_EPS = 1e-5

kernel_name = 'sb_fox_hier_moe_deepnorm_ple_layer'


def layer_norm(x, g, b):
    xf = x.astype(jnp.float32)
    mu = jnp.mean(xf, axis=-1, keepdims=True)
    var = jnp.mean(jnp.square(xf - mu), axis=-1, keepdims=True)
    return ((xf - mu) * lax.rsqrt(var + LN_EPS) * g + b).astype(x.dtype)


def stick_breaking_block(q, k, v, t0):
    n_q, n_k = q.shape[2], k.shape[2]
    z = jnp.einsum('bhqd,bhkd->bhqk', q, k).astype(jnp.float32) * (HEAD_DIM ** -0.5)
    t_pos = t0 + jnp.arange(n_q)[:, None]
    s_pos = jnp.arange(n_k)[None, :]
    earlier = s_pos < t_pos
    log_keep = jnp.where(earlier, jax.nn.log_sigmoid(-z), 0.0)
    after_s = lax.cumsum(log_keep, axis=3, reverse=True) - log_keep
    w = jnp.where(earlier, jnp.exp(jax.nn.log_sigmoid(z) + after_s), 0.0)
    return jnp.einsum('bhqk,bhkd->bhqd', w.astype(v.dtype), v)


def forgetting_block(q, k, v, cf_q, cf_k, t0):
    n_q, n_k = q.shape[2], k.shape[2]
    z = jnp.einsum('bhqd,bhkd->bhqk', q, k).astype(jnp.float32) * (HEAD_DIM ** -0.5)
    z = z + cf_q[..., :, None] - cf_k[..., None, :]
    visible = jnp.arange(n_k)[None, :] <= (t0 + jnp.arange(n_q)[:, None])
    probs = jax.nn.softmax(jnp.where(visible, z, -jnp.inf), axis=-1)
    return jnp.einsum('bhqk,bhkd->bhqd', probs.astype(v.dtype), v)


def hybrid_token_mixer(x, w_in, b_forget, w_out):
    bsz, seq, _ = x.shape
    proj = jnp.einsum('bsd,de->bse', x, w_in)

    def heads(lo, n_heads):
        t = proj[..., lo:lo + n_heads * HEAD_DIM]
        return t.reshape(bsz, seq, n_heads, HEAD_DIM).transpose(0, 2, 1, 3)

    q_sb, k_sb, v_sb = heads(OFF_Q_SB, SB_HEADS), heads(OFF_K_SB, SB_HEADS), heads(OFF_V_SB, SB_HEADS)
    q_fx, k_fx, v_fx = heads(OFF_Q_FX, FOX_HEADS), heads(OFF_K_FX, FOX_HEADS), heads(OFF_V_FX, FOX_HEADS)
    f_logit = proj[..., OFF_F:OFF_F + FOX_HEADS].astype(jnp.float32) + b_forget.astype(jnp.float32)
    cum_log_f = jnp.cumsum(jax.nn.log_sigmoid(f_logit), axis=1).transpose(0, 2, 1)

    sb_out, fx_out = [], []
    for t0 in range(0, seq, QUERY_BLOCK):
        t1 = t0 + QUERY_BLOCK
        sb_out.append(stick_breaking_block(q_sb[:, :, t0:t1], k_sb[:, :, :t1], v_sb[:, :, :t1], t0))
        fx_out.append(forgetting_block(q_fx[:, :, t0:t1], k_fx[:, :, :t1], v_fx[:, :, :t1],
                                       cum_log_f[:, :, t0:t1], cum_log_f[:, :, :t1], t0))

    def merge(blocks):
        o = jnp.concatenate(blocks, axis=2)
        return o.transpose(0, 2, 1, 3).reshape(bsz, seq, -1)

    mixed = jnp.concatenate([merge(sb_out), merge(fx_out)], axis=-1)
    return jnp.einsum('bse,ed->bsd', mixed, w_out)


def hierarchical_moe(x_tok, w_group, b_group, w_router, b_router, w_gate, w_up, w_down):
    n, d = x_tok.shape
    xf = x_tok.astype(jnp.float32)
    g_logits = xf @ w_group.astype(jnp.float32) + b_group.astype(jnp.float32)
    g_prob = jax.nn.softmax(g_logits, axis=-1)
    g_sel = jnp.argmax(g_logits, axis=-1).astype(jnp.int32)
    e_logits = (xf @ w_router.astype(jnp.float32) + b_router.astype(jnp.float32))
    e_logits = e_logits.reshape(n, N_GROUPS, EXPERTS_PER_GROUP)
    e_in_group = jnp.take_along_axis(e_logits, g_sel[:, None, None], axis=1)[:, 0]
    top_val, top_loc = lax.top_k(e_in_group, TOP_K)
    gate = jax.nn.softmax(top_val, axis=-1) * jnp.take_along_axis(g_prob, g_sel[:, None], axis=1)
    expert_id = (g_sel[:, None] * EXPERTS_PER_GROUP + top_loc).astype(jnp.int32)

    m = n * TOP_K
    flat_e = expert_id.reshape(m)
    flat_tok = jnp.arange(m, dtype=jnp.int32) // TOP_K
    flat_w = gate.reshape(m)
    order = jnp.argsort(flat_e)
    e_sorted = flat_e[order]
    counts = jnp.bincount(flat_e, length=N_EXPERTS)
    starts = jnp.cumsum(counts) - counts
    padded = ((counts + DISPATCH_BLOCK - 1) // DISPATCH_BLOCK) * DISPATCH_BLOCK
    pad_end = jnp.cumsum(padded)
    pad_start = pad_end - padded
    dest = pad_start[e_sorted] + (jnp.arange(m, dtype=jnp.int32) - starts[e_sorted])
    cap = m + N_EXPERTS * DISPATCH_BLOCK
    n_blk = cap // DISPATCH_BLOCK
    tok_buf = jnp.zeros((cap,), jnp.int32).at[dest].set(flat_tok[order])
    w_buf = jnp.zeros((cap,), jnp.float32).at[dest].set(flat_w[order])
    blk_expert = jnp.minimum(
        jnp.searchsorted(pad_end, jnp.arange(n_blk) * DISPATCH_BLOCK, side='right'),
        N_EXPERTS - 1).astype(jnp.int32)

    def run_block(args):
        tok, e = args
        xb = x_tok[tok]
        hdn = jax.nn.silu(xb @ w_gate[e]) * (xb @ w_up[e])
        return hdn @ w_down[e]

    y = lax.map(run_block, (tok_buf.reshape(n_blk, DISPATCH_BLOCK), blk_expert))
    y = y.reshape(cap, d) * w_buf[:, None].astype(y.dtype)
    return jnp.zeros_like(x_tok).at[tok_buf].add(y)


def setup_inputs(seed: int = 0) -> dict:
    key = jax.random.key(seed)
    ks = jax.random.split(key, 20)
    nrm = jax.random.normal
    col_scale = np.ones((IN_COLS,), np.float32)
    col_scale[OFF_V_SB:OFF_V_SB + SB_WIDTH] = BETA_INIT
    col_scale[OFF_V_FX:OFF_V_FX + FOX_WIDTH] = BETA_INIT
    return {
        'x': nrm(ks[0], (BATCH, SEQ, D_MODEL), jnp.float32),
        'p': nrm(ks[1], (DEPTH, BATCH, SEQ, PLE_DIM), jnp.float32),
        'w_in': nrm(ks[2], (DEPTH, D_MODEL, IN_COLS), jnp.float32) * (D_MODEL ** -0.5) * jnp.asarray(col_scale),
        'b_forget': 3.0 + 0.1 * nrm(ks[3], (DEPTH, FOX_HEADS), jnp.float32),
        'w_out': nrm(ks[4], (DEPTH, MIX_WIDTH, D_MODEL), jnp.float32) * (MIX_WIDTH ** -0.5) * BETA_INIT,
        'ln_mix_g': 1.0 + 0.02 * nrm(ks[5], (DEPTH, D_MODEL), jnp.float32),
        'ln_mix_b': 0.02 * nrm(ks[6], (DEPTH, D_MODEL), jnp.float32),
        'w_group': nrm(ks[7], (DEPTH, D_MODEL, N_GROUPS), jnp.float32) * (D_MODEL ** -0.5),
        'b_group': 0.01 * nrm(ks[8], (DEPTH, N_GROUPS), jnp.float32),
        'w_router': nrm(ks[9], (DEPTH, D_MODEL, N_EXPERTS), jnp.float32) * (D_MODEL ** -0.5),
        'b_router': 0.01 * nrm(ks[10], (DEPTH, N_EXPERTS), jnp.float32),
        'w_gate': nrm(ks[11], (DEPTH, N_EXPERTS, D_MODEL, D_EXPERT), jnp.float32) * (D_MODEL ** -0.5),
        'w_up': nrm(ks[12], (DEPTH, N_EXPERTS, D_MODEL, D_EXPERT), jnp.float32) * (D_MODEL ** -0.5) * BETA_INIT,
        'w_down': nrm(ks[13], (DEPTH, N_EXPERTS, D_EXPERT, D_MODEL), jnp.float32) * (D_EXPERT ** -0.5) * BETA_INIT,
        'w_ple': nrm(ks[14], (DEPTH, PLE_DIM, D_MODEL), jnp.float32) * (PLE_DIM ** -0.5),
        'w_ple_gate': nrm(ks[15], (DEPTH, D_MODEL, D_MODEL), jnp.float32) * (D_MODEL ** -0.5),
        'ln_ffn_g': 1.0 + 0.02 * nrm(ks[16], (DEPTH, D_MODEL), jnp.float32),
        'ln_ffn_b': 0.02 * nrm(ks[17], (DEPTH, D_MODEL), jnp.float32),
    }


def reference(x, p, w_in, b_forget, w_out, ln_mix_g, ln_mix_b, w_group, b_group, w_router, b_router,
              w_gate, w_up, w_down, w_ple, w_ple_gate, ln_ffn_g, ln_ffn_b):
    bsz, seq, d = x.shape
    h = x
    for i in range(DEPTH):
        mix = hybrid_token_mixer(h, w_in[i], b_forget[i], w_out[i])
        h = layer_norm(ALPHA * h + mix, ln_mix_g[i], ln_mix_b[i])
        ffn = hierarchical_moe(h.reshape(bsz * seq, d), w_group[i], b_group[i], w_router[i], b_router[i],
                               w_gate[i], w_up[i], w_down[i]).reshape(bsz, seq, d)
        ple = jnp.einsum('bse,ed->bsd', p[i], w_ple[i]) * jax.nn.sigmoid(jnp.einsum('bsd,de->bse', h, w_ple_gate[i]))
        h = layer_norm(ALPHA * h + ffn + ple, ln_ffn_g[i], ln_ffn_b[i])
    return h
```

```python
import contextlib
import os
import numpy as np
import concourse.bass as bass
import concourse.mybir as mybir
from concourse.bass_utils import run_bass_kernel_spmd

F32 = mybir.dt.float32
BF16 = mybir.dt.bfloat16
I32 = mybir.dt.int32
AF = mybir.ActivationFunctionType
ALU = mybir.AluOpType
AX = mybir.AxisListType

D = 1024
SEQ = 8192
NB = 4
NCORE = 8
NLOC = 4096
NTT = 32
NBLK = 96
CAP = NBLK * 128
ALPHA = 2 ** 0.25
LN_EPS = 1e-5
BIG = -30000.0
G_PAR = ([0, 3, 4, 7, 8, 11, 12, 15], [1, 2, 5, 6, 9, 10, 13, 14])


class Sched:
    ENGS = ("pe", "act", "dve", "pool", "sp")

    def __init__(self, nc, n_dma_sems=10):
        self.nc = nc
        self.ops = {e: [] for e in self.ENGS}
        self.cnt = {e: 0 for e in self.ENGS}
        self.last_w = {}
        self.readers = {}
        self.seen = {e: {} for e in self.ENGS}
        self.n_dma_sems = n_dma_sems
        self.dma_used = {}
        self.dma_rr = {"sp": 0, "pool": 0, "act": 0}
        self.out_tokens = []

    def _deps(self, eng, reads, writes):
        deps = {}

        def add(k, v):
            if deps.get(k, 0) < v:
                deps[k] = v

        for k in reads:
            t = self.last_w.get(k)
            if t:
                add(*t)
        for k in writes:
            t = self.last_w.get(k)
            if t:
                add(*t)
            for kk, vv in self.readers.get(k, {}).items():
                add(kk, vv)
        waits = []
        for k, v in deps.items():
            if k == "pe" and eng == "pe":
                continue
            if self.seen[eng].get(k, 0) >= v:
                continue
            self.seen[eng][k] = v
            waits.append((k, v))
        return waits

    def _commit(self, tok, reads, writes):
        for k in reads:
            r = self.readers.setdefault(k, {})
            if r.get(tok[0], 0) < tok[1]:
                r[tok[0]] = tok[1]
        for k in writes:
            self.last_w[k] = tok
            self.readers[k] = {}

    def op(self, eng, fn, reads=(), writes=()):
        waits = self._deps(eng, reads, writes)
        self.cnt[eng] += 1
        tok = (eng, self.cnt[eng])
        self.ops[eng].append((fn, waits, (eng, 1)))
        self._commit(tok, reads, writes)
        return tok

    def dma(self, q, fn, reads=(), writes=(), is_output=False):
        waits = self._deps(q, reads, writes)
        slot = self.dma_rr[q] % self.n_dma_sems
        self.dma_rr[q] += 1
        key = "dma_%s_%d" % (q, slot)
        used = self.dma_used.get(key, 0)
        if used > 0 and self.seen[q].get(key, 0) < 16 * used:
            self.seen[q][key] = 16 * used
            waits.append((key, 16 * used))
        self.dma_used[key] = used + 1
        tok = (key, 16 * (used + 1))
        self.ops[q].append((fn, waits, (key, 16)))
        self._commit(tok, reads, writes)
        if is_output:
            self.out_tokens.append(tok)
        return tok

    def barrier(self):
        toks = [(e, self.cnt[e]) for e in self.ENGS if self.cnt[e] > 0]
        toks += [(k, 16 * u) for k, u in self.dma_used.items()]
        for e in self.ENGS:
            waits = []
            for k, v in toks:
                if k == e and e == "pe":
                    continue
                if self.seen[e].get(k, 0) >= v:
                    continue
                self.seen[e][k] = v
                waits.append((k, v))
            if waits:
                self.ops[e].append((None, waits, None))

    def emit(self):
        nc = self.nc
        fin = []
        for e in self.ENGS:
            if self.cnt[e] > 0 and self.seen["sp"].get(e, 0) < self.cnt[e]:
                fin.append((e, self.cnt[e]))
        for k, u in self.dma_used.items():
            if self.seen["sp"].get(k, 0) < 16 * u:
                fin.append((k, 16 * u))
        self.ops["sp"].append((None, fin, None))
        keys = set()
        for e in self.ENGS:
            for fn, waits, inc in self.ops[e]:
                for k, v in waits:
                    keys.add(k)
                if inc is not None:
                    keys.add(inc[0])
        with contextlib.ExitStack() as st:
            sems = {}
            for k in sorted(keys):
                sems[k] = st.enter_context(nc.semaphore("s_" + k))
            block = st.enter_context(nc.Block())

            def run(engname):
                def body(eng):
                    for fn, waits, inc in self.ops[engname]:
                        for k, v in waits:
                            eng.wait_ge(sems[k], v)
                        if fn is not None:
                            ins = fn(eng)
                            ins.then_inc(sems[inc[0]], inc[1])
                return body

            block.tensor(run("pe"))
            block.scalar(run("act"))
            block.vector(run("dve"))
            block.gpsimd(run("pool"))
            block.sync(run("sp"))


class Arena:
    def __init__(self, nc, limit=229376 - 256):
        self.nc = nc
        self.off = 16640
        self.limit = limit
        self.n = 0

    def alloc(self, name, shape, dtype):
        esz = {F32: 4, BF16: 2, I32: 4}[dtype]
        per = esz
        for s in shape[1:]:
            per *= s
        self.off = (self.off + 63) // 64 * 64
        self.n += 1
        t = self.nc.alloc_sbuf_tensor_at("%s_%d" % (name, self.n), list(shape), dtype, offset=self.off)
        self.off += per
        assert self.off <= self.limit, (name, self.off)
        return t.ap()

    def mark(self):
        return self.off

    def reset(self, m):
        self.off = m


def build(debug=False, stop=99):
    nc = bass.Bass("TRN2", target_bir_lowering=False)
    S = Sched(nc)
    A = Arena(nc)

    def din(name, shape, dt=F32):
        return nc.dram_tensor(name, list(shape), dt, kind="ExternalInput").ap()

    def dscr(name, shape, dt, dbg=False):
        return nc.dram_tensor(name, list(shape), dt, kind=("ExternalOutput" if (dbg and debug) else "Internal")).ap()

    xT_all = din("xT_all", [128, 8, SEQ])
    xT_loc = din("xT_loc", [128, 8, NLOC])
    x_loc = din("x_loc", [NLOC, D])
    pT_loc = din("pT_loc", [128, 2, NLOC])
    w_kp = din("w_kp", [128, 8, 1024])
    w_qp = din("w_qp", [128, 8, 1024])
    w_v = din("w_v", [128, 8, 1024])
    w_f = din("w_f", [128, 8, 8])
    bf_t = din("bf_t", [1, 512])
    w_out = din("w_out", [128, 8, 1024])
    ln1g = din("ln1g", [1, D]); ln1b = din("ln1b", [1, D])
    ln2g = din("ln2g", [1, D]); ln2b = din("ln2b", [1, D])
    w_r36 = din("w_r36", [128, 8, 36])
    b_r36 = din("b_r36", [1, 32 * 36])
    w_gate = din("w_gate", [32 * 128, 4096])
    w_up = din("w_up", [32 * 128, 4096])
    w_down = din("w_down", [32 * 128, 4096])
    w_ple = din("w_ple", [128, 2, 1024])
    w_pg = din("w_pg", [128, 8, 1024])
    masks = din("masks", [128, 32, 512])
    selx = din("selx", [1, 1024])
    consts = din("consts", [128, 7, 128])
    iot = din("iot", [128, 12 + 32 + 96], F32)
    out = nc.dram_tensor("out", [NLOC, D], F32, kind="ExternalOutput").ap()

    kt_scr = dscr("kt_scr", [8, 128, SEQ], BF16)
    v_scr = dscr("v_scr", [8, 128, 64 * 128], BF16)
    qt_scr = dscr("qt_scr", [8, 128, NLOC], BF16)
    h_scr = dscr("h_scr", [NLOC, D], F32, dbg=True)
    pre_scr = dscr("pre_scr", [NLOC, D], F32, dbg=True)
    xs_scr = dscr("xs_scr", [CAP, D], BF16)
    ys_scr = dscr("ys_scr", [CAP, D], F32)
    dbg_r = dscr("dbg_r", [128, 4096], F32, dbg=True)

    PSALL = nc.alloc_psum_tensor("psall", [128, 4096], F32).ap()
    PS = [PSALL[:, i * 512:(i + 1) * 512] for i in range(8)]

    c_f32 = A.alloc("c_f32", [128, 7, 128], F32)
    c_bf = A.alloc("c_bf", [128, 7, 128], BF16)
    IDENT, TRII, STRICT, ONES, NEGTRI, NEGID, NEGONE = range(7)
    cfpos = A.alloc("cfpos", [128, 512], F32)
    cref = A.alloc("cref", [128, 64], F32)
    L_all = A.alloc("L_all", [128, 32 * 36], F32)
    g0_all = A.alloc("g0_all", [128, 32], F32)
    g1_all = A.alloc("g1_all", [128, 32], F32)
    dest0_i = A.alloc("dest0_i", [128, 32], I32)
    dest1_i = A.alloc("dest1_i", [128, 32], I32)
    idxA = A.alloc("idxA", [128, NBLK], I32)
    idxB = A.alloc("idxB", [128, NBLK], I32)
    iot_sb = A.alloc("iot_sb", [128, 140], F32)
    eps_c = A.alloc("eps_c", [128, 1], F32)
    st6 = A.alloc("st6", [128, 12], F32)
    mv = A.alloc("mv", [128, 4], F32)
    PBASE = A.mark()

    S.dma("sp", lambda e: e.dma_start(out=c_f32, in_=consts), writes=["c_f32"])
    S.dma("pool", lambda e: e.dma_start(out=c_bf, in_=consts), writes=["c_bf"])
    S.dma("sp", lambda e: e.dma_start(out=iot_sb, in_=iot), writes=["iot"])
    S.op("dve", lambda e: e.memset(eps_c, LN_EPS), writes=["eps_c"])

    rr = {"ev": 0}

    def evac(out_ap, in_ap, reads, writes, scale=None):
        rr["ev"] += 1
        if rr["ev"] % 2 == 0:
            if scale is None:
                S.op("act", lambda e: e.activation(out=out_ap, in_=in_ap, func=AF.Copy), reads=reads, writes=writes)
            else:
                S.op("act", lambda e: e.activation(out=out_ap, in_=in_ap, func=AF.Copy, scale=scale), reads=reads, writes=writes)
        else:
            if scale is None:
                S.op("dve", lambda e: e.tensor_copy(out=out_ap, in_=in_ap), reads=reads, writes=writes)
            else:
                S.op("dve", lambda e: e.tensor_scalar(out=out_ap, in0=in_ap, scalar1=scale, scalar2=None, op0=ALU.mult),
                     reads=reads, writes=writes)

    Wkp = A.alloc("Wkp", [128, 8, 1024], BF16)
    Wqp = A.alloc("Wqp", [128, 8, 1024], BF16)
    Wv = A.alloc("Wv", [128, 8, 1024], BF16)
    Wf = A.alloc("Wf", [128, 8, 8], F32)
    bF = A.alloc("bF", [128, 512], F32)
    selx_b = A.alloc("selx_b", [128, 1024], F32)
    xTb = [A.alloc("xTb", [128, 8, 512], BF16) for _ in range(2)]
    xTf = [A.alloc("xTf", [128, 8, 512], F32) for _ in range(2)]
    KTst = [A.alloc("KTst", [128, 8, 512], BF16) for _ in range(2)]
    Vst = [A.alloc("Vst", [128, 8, 512], BF16) for _ in range(2)]

    for j in range(8):
        S.dma("pool", lambda e, j=j: e.dma_start(out=Wkp[:, j, :], in_=w_kp[:, j, :]), writes=["Wkp"])
        S.dma("pool", lambda e, j=j: e.dma_start(out=Wv[:, j, :], in_=w_v[:, j, :]), writes=["Wv"])
        S.dma("pool", lambda e, j=j: e.dma_start(out=Wqp[:, j, :], in_=w_qp[:, j, :]), writes=["Wqp"])
    mask_bf = nc.alloc_sbuf_tensor_at("mask_bf_fix", [128, 32, 512], BF16, offset=194560).ap()
    S.dma("sp", lambda e: e.dma_start(out=Wf, in_=w_f), writes=["Wf"])
    S.dma("sp", lambda e: e.dma_start(out=bF, in_=bf_t.partition_broadcast(128)), writes=["bF"])
    S.dma("sp", lambda e: e.dma_start(out=selx_b, in_=selx.partition_broadcast(128)), writes=["selx_b"])

    PF = PS[7]
    bank = {"i": 0}

    def nxt_bank(n=7):
        bank["i"] = (bank["i"] + 1) % n
        return bank["i"]

    for g in range(16):
        b2 = g % 2
        for c in range(8):
            S.dma("pool", lambda e, g=g, b2=b2, c=c: e.dma_start(out=xTb[b2][:, c, :], in_=xT_all[:, c, g * 512:(g + 1) * 512]),
                  writes=["xTb%d" % b2])
        S.dma("sp", lambda e, g=g, b2=b2: e.dma_start(out=xTf[b2], in_=xT_all[:, :, g * 512:(g + 1) * 512]),
              writes=["xTf%d" % b2])
        for hp in range(8):
            bi = nxt_bank()
            for c in range(8):
                S.op("pe", lambda e, bi=bi, hp=hp, c=c, b2=b2: e.matmul(PS[bi], lhsT=Wkp[:, c, hp * 128:(hp + 1) * 128],
                                                                       rhs=xTb[b2][:, c, :], start=(c == 0), stop=(c == 7)),
                     reads=["Wkp", "xTb%d" % b2], writes=["ps%d" % bi])
            evac(KTst[b2][:, hp, :], PS[bi], ["ps%d" % bi], ["KTst%d" % b2])
        S.dma("sp", lambda e, g=g, b2=b2: e.dma_start(out=kt_scr[:, :, g * 512:(g + 1) * 512].rearrange("h p t -> p h t"),
                                                      in_=KTst[b2]),
              reads=["KTst%d" % b2], writes=["kt_scr"])
        for j in range(4):
            for half in range(2):
                bi = nxt_bank()
                for c in range(8):
                    S.op("pe", lambda e, bi=bi, j=j, half=half, c=c, b2=b2: e.matmul(
                        PS[bi], lhsT=xTb[b2][:, c, j * 128:(j + 1) * 128], rhs=Wv[:, c, half * 512:(half + 1) * 512],
                        start=(c == 0), stop=(c == 7)), reads=["Wv", "xTb%d" % b2], writes=["ps%d" % bi])
                dst = Vst[b2].rearrange("p h (j t e) -> p h j t e", j=4, t=2, e=64)[:, :, j, half, :]
                evac(dst, PS[bi].rearrange("p (h e) -> p h e", h=8, e=64), ["ps%d" % bi], ["Vst%d" % b2])
        S.dma("sp", lambda e, g=g, b2=b2: e.dma_start(
            out=v_scr[:, :, g * 512:(g + 1) * 512].rearrange("h p t -> p h t"), in_=Vst[b2]),
            reads=["Vst%d" % b2], writes=["v_scr"])
        for j in range(4):
            kb = 4 * g + j
            for c in range(8):
                S.op("pe", lambda e, kb=kb, j=j, c=c, b2=b2: e.matmul(PF[:, kb * 8:(kb + 1) * 8], lhsT=xTf[b2][:, c, j * 128:(j + 1) * 128],
                                                                     rhs=Wf[:, c, :], start=(c == 0), stop=(c == 7)),
                     reads=["Wf", "xTf%d" % b2], writes=["PF"])
    for j in range(32):
        S.dma("pool", lambda e, j=j: e.dma_start(out=mask_bf[:, j, :], in_=masks[:, j, :]), writes=["mask_bf"])
    m1 = A.mark()
    lf = A.alloc("lf", [128, 512], F32)
    lfn = A.alloc("lfn", [128, 512], F32)
    Ta = A.alloc("Ta", [128, 512], F32)
    Tb = A.alloc("Tb", [128, 512], F32)
    T0 = A.alloc("T0", [128, 512], F32)
    prod = A.alloc("prod", [128, 128], F32)
    S.op("dve", lambda e: e.tensor_tensor(out=lf, in0=PF, in1=bF, op=ALU.add), reads=["PF", "bF"], writes=["lf"])
    S.op("act", lambda e: e.activation(out=lfn, in_=lf, func=AF.Exp, scale=-1.0), reads=["lf"], writes=["lfn"])
    S.op("act", lambda e: e.activation(out=lf, in_=lfn, func=AF.Ln, bias=1.0), reads=["lfn"], writes=["lf"])
    S.op("pe", lambda e: e.matmul(PS[5], lhsT=c_f32[:, TRII, :], rhs=lf, start=True, stop=True), reads=["c_f32", "lf"], writes=["ps5"])
    S.op("pe", lambda e: e.matmul(PS[6], lhsT=c_f32[:, ONES, :], rhs=lf, start=True, stop=True), reads=["c_f32", "lf"], writes=["ps6"])
    S.op("dve", lambda e: e.tensor_copy(out=T0, in_=PS[6]), reads=["ps6"], writes=["T0"])
    S.op("dve", lambda e: e.tensor_copy(out=Ta, in_=PS[6]), reads=["ps6"], writes=["Ta"])
    cur, oth, cn, on = Ta, Tb, "Ta", "Tb"
    for dsh in (1, 2, 4, 8, 16, 32):
        w_ = 8 * dsh
        S.op("dve", lambda e, cur=cur, oth=oth, w_=w_: e.tensor_tensor(out=oth[:, w_:], in0=cur[:, w_:], in1=cur[:, :512 - w_], op=ALU.add),
             reads=[cn], writes=[on])
        S.op("dve", lambda e, cur=cur, oth=oth, w_=w_: e.tensor_copy(out=oth[:, :w_], in_=cur[:, :w_]), reads=[cn, on], writes=[on])
        cur, oth, cn, on = oth, cur, on, cn
    Tincl, tin = cur, cn
    S.op("dve", lambda e: e.tensor_tensor(out=lfn, in0=Tincl, in1=T0, op=ALU.subtract), reads=[tin, "T0"], writes=["lfn"])
    S.op("dve", lambda e: e.tensor_tensor(out=cfpos, in0=PS[5], in1=lfn, op=ALU.add), reads=["ps5", "lfn"], writes=["cfpos"])
    Tlast = Tincl.rearrange("p (g j h) -> p g j h", g=16, j=4, h=8)[:, :, 3, :]
    for s in range(8):
        S.op("dve", lambda e, s=s: e.tensor_tensor(out=prod.rearrange("p (h g) -> p g h", h=8, g=16), in0=Tlast,
                                                  in1=selx_b[:, s * 128:(s + 1) * 128].rearrange("p (g h) -> p g h", g=16, h=8),
                                                  op=ALU.mult), reads=[tin, "selx_b"], writes=["prod"])
        S.op("dve", lambda e, s=s: e.tensor_reduce(out=cref[:, s * 8:(s + 1) * 8], in_=prod.rearrange("p (h g) -> p h g", h=8, g=16),
                                                  axis=AX.X, op=ALU.add), reads=["prod"], writes=["cref"])

    for s in range(8):
        b2 = s % 2
        S.dma("sp", lambda e, s=s, b2=b2: e.dma_start(out=xTf[b2], in_=xT_loc[:, :, s * 512:(s + 1) * 512]),
              writes=["xTf%d" % b2])
        S.op("act", lambda e, b2=b2: e.activation(out=xTb[b2][:, 0:4, :], in_=xTf[b2][:, 0:4, :], func=AF.Copy),
             reads=["xTf%d" % b2], writes=["xTb%d" % b2])
        S.op("dve", lambda e, b2=b2: e.tensor_copy(out=xTb[b2][:, 4:8, :], in_=xTf[b2][:, 4:8, :]),
             reads=["xTf%d" % b2, "xTb%d" % b2], writes=["xTb%d" % b2])
        for hp in range(8):
            bi = nxt_bank(5)
            for c in range(8):
                S.op("pe", lambda e, bi=bi, hp=hp, c=c, b2=b2: e.matmul(PS[bi], lhsT=Wqp[:, c, hp * 128:(hp + 1) * 128],
                                                                       rhs=xTb[b2][:, c, :], start=(c == 0), stop=(c == 7)),
                     reads=["Wqp", "xTb%d" % b2], writes=["ps%d" % bi])
            evac(KTst[b2][:, hp, :], PS[bi], ["ps%d" % bi], ["KTst%d" % b2], scale=0.125)
        S.dma("sp", lambda e, s=s, b2=b2: e.dma_start(out=qt_scr[:, :, s * 512:(s + 1) * 512].rearrange("h p t -> p h t"),
                                                      in_=KTst[b2]),
              reads=["KTst%d" % b2], writes=["qt_scr"])

    if stop < 2:
        S.emit()
        return nc
    S.barrier()
    A.reset(PBASE)
    mixT = A.alloc("mixT", [128, 8, NLOC], BF16)
    M3 = A.mark()
    KT = A.alloc("KT", [128, SEQ], BF16)
    Vt = A.alloc("Vt", [128, 64 * 192], BF16)
    QT = A.alloc("QT", [128, NLOC], BF16)
    Et2 = [A.alloc("Et2", [128, 1024], F32) for _ in range(2)]
    SP2 = [A.alloc("SP2", [128, 1024], BF16) for _ in range(2)]
    Wt2 = [A.alloc("Wt2", [128, 1024], BF16) for _ in range(2)]
    Ybf = [A.alloc("Ybf", [128, 512], BF16) for _ in range(2)]
    Pt = [A.alloc("Pt", [128, 512], BF16) for _ in range(2)]
    biasF = [A.alloc("biasF", [128, 64], F32) for _ in range(2)]
    rinv = A.alloc("rinv", [128, 512], F32)
    XSA = [PSALL[:, 0:1024], PSALL[:, 1024:2048]]
    XF1 = PS[4]
    YB, OB, OFB = PS[5], PS[6], PS[7]
    Vt4 = Vt.rearrange("p (k t e) -> p k t e", k=64, t=3, e=64)
    S.op("dve", lambda e: e.memset(Vt4[:, :, 2, :], 1.0), writes=["Vones"])
    cf3 = cfpos.rearrange("p (k h) -> p k h", k=64, h=8)

    tiles = []
    for hp in range(8):
        for s in range(8):
            nkb = 8 * s + 8
            for kb in range(nkb - 1, -1, -1):
                tiles.append(dict(hp=hp, s=s, kb=kb, first=(kb == nkb - 1), last=(kb == 0), j=kb - 8 * s,
                                  newhp=(s == 0 and kb == nkb - 1), news=(kb == nkb - 1)))
    NT = len(tiles)
    NP = NT // 2

    def st_load(t):
        hp = t["hp"]
        S.dma("sp", lambda e: e.dma_start(out=KT, in_=kt_scr[hp]), reads=["kt_scr"], writes=["KT"])
        S.dma("sp", lambda e: e.dma_start(out=QT, in_=qt_scr[hp]), reads=["qt_scr"], writes=["QT"])
        S.dma("sp", lambda e: e.dma_start(out=Vt4[:, :, 0:2, :], in_=v_scr[hp].rearrange("p (k t e) -> p k t e", k=64, t=2, e=64)),
              reads=["v_scr"], writes=["Vt"])

    def st_bias(t):
        hp, s = t["hp"], t["s"]
        nkb = 8 * s + 8
        bb = s % 2
        S.op("dve", lambda e: e.tensor_scalar(out=biasF[bb][:, 0:nkb], in0=cf3[:, 0:nkb, hp], scalar1=cref[:, s * 8 + hp:s * 8 + hp + 1],
                                              scalar2=None, op0=ALU.subtract), reads=["cfpos", "cref"], writes=["biasF%d" % bb])

    def s1p(n):
        for h in range(2):
            t = tiles[2 * n + h]
            hp, s, kb, j = t["hp"], t["s"], t["kb"], t["j"]
            if t["newhp"]:
                st_load(t)
            if t["news"]:
                st_bias(t)
            xs = XSA[n % 2][:, h * 512:(h + 1) * 512]
            key = "XS%d_%d" % (n % 2, h)
            S.op("pe", lambda e, xs=xs, kb=kb, s=s, j=j: e.matmul(xs, lhsT=KT[0:64, kb * 128:(kb + 1) * 128], rhs=QT[0:64, s * 512:(s + 1) * 512],
                                                                 start=True, stop=False), reads=["KT", "QT"], writes=[key])
            if j >= 0:
                mi = (0 * 2 + s % 2) * 8 + j
                S.op("pe", lambda e, xs=xs, mi=mi: e.matmul(xs, lhsT=c_bf[:, IDENT, :], rhs=mask_bf[:, mi, :], start=False, stop=False),
                     reads=["c_bf", "mask_bf"], writes=[key])

    def f1(n, h):
        t = tiles[2 * n + h]
        hp, s, kb, j = t["hp"], t["s"], t["kb"], t["j"]
        S.op("pe", lambda e: e.matmul(XF1, lhsT=KT[64:128, kb * 128:(kb + 1) * 128], rhs=QT[64:128, s * 512:(s + 1) * 512],
                                      start=True, stop=(j < 0)), reads=["KT", "QT"], writes=["XF"])
        if j >= 0:
            mi = (1 * 2 + s % 2) * 8 + j
            S.op("pe", lambda e: e.matmul(XF1, lhsT=c_bf[:, IDENT, :], rhs=mask_bf[:, mi, :], start=False, stop=True),
                 reads=["c_bf", "mask_bf"], writes=["XF"])

    def s2p(n):
        q = n % 2
        S.op("act", lambda e: e.activation(out=Et2[q], in_=XSA[q], func=AF.Exp), reads=["XS%d_0" % q, "XS%d_1" % q], writes=["Et2_%d" % q])

    def s3p(n):
        q = n % 2
        S.op("act", lambda e: e.activation(out=SP2[q], in_=Et2[q], func=AF.Ln, bias=1.0), reads=["Et2_%d" % q], writes=["SP2_%d" % q])

    def s4p(n):
        ta, tb = tiles[2 * n], tiles[2 * n + 1]
        q = n % 2
        xa = XSA[q][:, 0:512]; xb_ = XSA[q][:, 512:1024]
        spa = SP2[q][:, 0:512]; spb = SP2[q][:, 512:1024]
        ka, kb_ = "XS%d_0" % q, "XS%d_1" % q
        spk = "SP2_%d" % q
        yprev = Ybf[1 - q]; ypk = "Ybf%d" % (1 - q)
        S.op("pe", lambda e: e.matmul(xa, lhsT=c_bf[:, NEGTRI, :], rhs=spa, start=False, stop=ta["first"]), reads=["c_bf", spk], writes=[ka])
        if not ta["first"]:
            S.op("pe", lambda e: e.matmul(xa, lhsT=c_bf[:, NEGID, :], rhs=yprev, start=False, stop=True), reads=["c_bf", ypk], writes=[ka])
        S.op("pe", lambda e: e.matmul(xb_, lhsT=c_bf[:, NEGTRI, :], rhs=spb, start=False, stop=False), reads=["c_bf", spk], writes=[kb_])
        if not ta["first"]:
            S.op("pe", lambda e: e.matmul(xb_, lhsT=c_bf[:, NEGID, :], rhs=yprev, start=False, stop=False), reads=["c_bf", ypk], writes=[kb_])
        S.op("pe", lambda e: e.matmul(xb_, lhsT=c_bf[:, NEGONE, :], rhs=spa, start=False, stop=True), reads=["c_bf", spk], writes=[kb_])
        S.op("pe", lambda e: e.matmul(YB, lhsT=c_bf[:, ONES, :], rhs=spa, start=ta["first"], stop=False), reads=["c_bf", spk], writes=["Y"])
        S.op("pe", lambda e: e.matmul(YB, lhsT=c_bf[:, ONES, :], rhs=spb, start=False, stop=tb["last"]), reads=["c_bf", spk], writes=["Y"])
        if not tb["last"]:
            S.op("dve", lambda e: e.tensor_copy(out=Ybf[q], in_=YB), reads=["Y"], writes=["Ybf%d" % q])

    def s6p(n):
        q = n % 2
        S.op("act", lambda e: e.activation(out=Wt2[q], in_=XSA[q], func=AF.Exp), reads=["XS%d_0" % q, "XS%d_1" % q], writes=["Wt2_%d" % q])

    def s7(n, h):
        t = tiles[2 * n + h]
        q = n % 2
        hp, s, kb = t["hp"], t["s"], t["kb"]
        S.op("pe", lambda e: e.matmul(OB[0:64, :], lhsT=Vt4[:, kb, 0, :], rhs=Wt2[q][:, h * 512:(h + 1) * 512], start=t["first"], stop=t["last"]),
             reads=["Vt", "Wt2_%d" % q], writes=["O"])
        if t["last"]:
            po = (hp % 2) * 64
            S.op("dve", lambda e: e.tensor_copy(out=mixT[po:po + 64, hp // 2, s * 512:(s + 1) * 512], in_=OB[0:64, :]),
                 reads=["O"], writes=["mixT"])

    def f2(n, h):
        t = tiles[2 * n + h]
        bb = t["s"] % 2; kb = t["kb"]
        S.op("act", lambda e: e.activation(out=Pt[h], in_=XF1, func=AF.Exp, bias=biasF[bb][:, kb:kb + 1]),
             reads=["XF", "biasF%d" % bb], writes=["Pt%d" % h])

    def f3(n, h):
        t = tiles[2 * n + h]
        hp, s, kb = t["hp"], t["s"], t["kb"]
        S.op("pe", lambda e: e.matmul(OFB, lhsT=Vt4[:, kb, 1:3, :].rearrange("p a b -> p (a b)"), rhs=Pt[h], start=t["first"], stop=t["last"]),
             reads=["Vt", "Vones", "Pt%d" % h], writes=["OF"])
        if t["last"]:
            po = (hp % 2) * 64
            S.op("dve", lambda e: e.reciprocal(out=rinv[0:64, :], in_=OFB[64:128, :]), reads=["OF"], writes=["rinv"])
            S.op("dve", lambda e: e.tensor_tensor(out=mixT[po:po + 64, 4 + hp // 2, s * 512:(s + 1) * 512], in0=OFB[0:64, :],
                                                  in1=rinv[0:64, :], op=ALU.mult), reads=["OF", "rinv"], writes=["mixT"])

    s1p(0)
    f1(0, 0)
    flushed = True
    for n in range(NP):
        boundary = (n + 1 < NP) and tiles[2 * (n + 1)]["newhp"]
        s2p(n)
        if not flushed:
            s6p(n - 1)
            s7(n - 1, 0)
            s7(n - 1, 1)
        flushed = False
        if n + 1 < NP and not boundary:
            s1p(n + 1)
        f2(n, 0)
        f1(n, 1)
        s3p(n)
        s4p(n)
        f3(n, 0)
        f2(n, 1)
        f3(n, 1)
        if n + 1 < NP and not boundary:
            f1(n + 1, 0)
        if boundary or n + 1 == NP:
            s6p(n)
            s7(n, 0)
            s7(n, 1)
            flushed = True
            if boundary:
                s1p(n + 1)
                f1(n + 1, 0)

    if stop < 3:
        S.emit()
        return nc
    S.barrier()
    A.reset(M3)
    Wout = A.alloc("Wout", [128, 8, 1024], BF16)
    Wpg = A.alloc("Wpg", [128, 8, 1024], BF16)
    Wple = A.alloc("Wple", [128, 2, 1024], BF16)
    Wr = A.alloc("Wr", [128, 8, 36], F32)
    g1b = A.alloc("g1b", [128, D], F32); b1b = A.alloc("b1b", [128, D], F32)
    b36 = A.alloc("b36", [128, 32 * 36], F32)
    xt = [A.alloc("xt", [128, D], F32) for _ in range(2)]
    pTb = [A.alloc("pTb", [128, 2, 128], BF16) for _ in range(2)]
    rt = A.alloc("rt", [128, D], F32)
    ht = [A.alloc("ht", [128, D], F32) for _ in range(2)]
    hhi = A.alloc("hhi", [128, D], BF16)
    hlo = A.alloc("hlo", [128, D], BF16)
    hTlo = A.alloc("hTlo", [128, 8, 128], BF16)
    Wrh = A.alloc("Wrh", [128, 8, 36], BF16)
    Wrl = A.alloc("Wrl", [128, 8, 36], BF16)
    hTb = A.alloc("hTb", [128, 8, 128], BF16)
    sg = A.alloc("sg", [128, D], F32)
    pre = [A.alloc("pre", [128, D], F32) for _ in range(2)]
    for j in range(8):
        S.dma("pool", lambda e, j=j: e.dma_start(out=Wout[:, j, :], in_=w_out[:, j, :]), writes=["Wout"])
        S.dma("pool", lambda e, j=j: e.dma_start(out=Wpg[:, j, :], in_=w_pg[:, j, :]), writes=["Wpg"])
    for j in range(2):
        S.dma("pool", lambda e, j=j: e.dma_start(out=Wple[:, j, :], in_=w_ple[:, j, :]), writes=["Wple"])
    S.dma("sp", lambda e: e.dma_start(out=Wr, in_=w_r36), writes=["Wr"])
    S.dma("sp", lambda e: e.dma_start(out=g1b, in_=ln1g.partition_broadcast(128)), writes=["g1b"])
    S.dma("sp", lambda e: e.dma_start(out=b1b, in_=ln1b.partition_broadcast(128)), writes=["b1b"])
    S.dma("sp", lambda e: e.dma_start(out=b36, in_=b_r36.partition_broadcast(128)), writes=["b36"])

    def layer_norm(src, dst, gb, bb, rk, wk, gk):
        for hf in range(2):
            S.op("dve", lambda e, hf=hf: e.bn_stats(out=st6[:, hf * 6:(hf + 1) * 6], in_=src[:, hf * 512:(hf + 1) * 512]),
                 reads=[rk], writes=["st6_%d" % hf])
        S.op("dve", lambda e: e.bn_aggr(out=mv[:, 0:2], in_=st6), reads=["st6_0", "st6_1"], writes=["mv"])
        S.op("act", lambda e: e.activation(out=mv[:, 2:3], in_=mv[:, 1:2], func=AF.Ln, bias=eps_c), reads=["mv", "eps_c"], writes=["mv2"])
        S.op("act", lambda e: e.activation(out=mv[:, 3:4], in_=mv[:, 2:3], func=AF.Exp, scale=-0.5), reads=["mv2"], writes=["mv3"])
        S.op("dve", lambda e: e.tensor_scalar(out=dst, in0=src, scalar1=mv[:, 0:1], scalar2=mv[:, 3:4], op0=ALU.subtract, op1=ALU.mult),
             reads=[rk, "mv", "mv3"], writes=[wk])
        S.op("dve", lambda e: e.tensor_tensor(out=dst, in0=dst, in1=gb, op=ALU.mult), reads=[wk] + gk, writes=[wk])
        S.op("dve", lambda e: e.tensor_tensor(out=dst, in0=dst, in1=bb, op=ALU.add), reads=[wk] + gk, writes=[wk])

    PA = [PS[0], PS[1]]; PTr = [PS[2], PS[3]]; PG = [PS[4], PS[5]]; PR = PS[6]
    PThi = PS[2].bitcast(BF16); PTlo = PS[3].bitcast(BF16)
    S.op("dve", lambda e: e.tensor_copy(out=Wrh, in_=Wr), reads=["Wr"], writes=["Wrh"])
    S.op("dve", lambda e: e.tensor_tensor(out=Wrl, in0=Wr, in1=Wrh, op=ALU.subtract), reads=["Wr", "Wrh"], writes=["Wrl"])
    hTb2 = [hTb, A.alloc("hTb_b", [128, 8, 128], BF16)]
    hTlo2 = [hTlo, A.alloc("hTlo_b", [128, 8, 128], BF16)]
    ple_sb = [A.alloc("ple_sb", [128, D], F32) for _ in range(2)]
    PP = PS[7]

    def p1(tt):
        b2 = tt % 2
        tsl = slice(tt * 128, (tt + 1) * 128)
        S.dma("sp", lambda e: e.dma_start(out=xt[b2], in_=x_loc[tsl, :]), writes=["xt%d" % b2])
        S.dma("pool", lambda e: e.dma_start(out=pTb[b2], in_=pT_loc[:, :, tsl]), writes=["pTb%d" % b2])
        for hf in range(2):
            for c in range(8):
                S.op("pe", lambda e, hf=hf, c=c: e.matmul(PA[hf], lhsT=mixT[:, c, tsl], rhs=Wout[:, c, hf * 512:(hf + 1) * 512],
                                                         start=(c == 0), stop=(c == 7)), reads=["mixT", "Wout"], writes=["PA%d" % hf])
            S.op("dve", lambda e, hf=hf: e.scalar_tensor_tensor(out=rt[:, hf * 512:(hf + 1) * 512], in0=xt[b2][:, hf * 512:(hf + 1) * 512],
                                                               scalar=ALPHA, in1=PA[hf], op0=ALU.mult, op1=ALU.add),
                 reads=["xt%d" % b2, "PA%d" % hf], writes=["rt"])
        for hf in range(2):
            for c in range(2):
                S.op("pe", lambda e, hf=hf, c=c: e.matmul(PP, lhsT=pTb[b2][:, c, :], rhs=Wple[:, c, hf * 512:(hf + 1) * 512],
                                                         start=(c == 0), stop=(c == 1)), reads=["pTb%d" % b2, "Wple"], writes=["PP"])
            S.op("act", lambda e, hf=hf: e.activation(out=ple_sb[b2][:, hf * 512:(hf + 1) * 512], in_=PP, func=AF.Copy),
                 reads=["PP"], writes=["ple%d_%d" % (b2, hf)])

    def ln_a(src, rk):
        for hf in range(2):
            S.op("dve", lambda e, hf=hf: e.bn_stats(out=st6[:, hf * 6:(hf + 1) * 6], in_=src[:, hf * 512:(hf + 1) * 512]),
                 reads=[rk], writes=["st6_%d" % hf])
        S.op("dve", lambda e: e.bn_aggr(out=mv[:, 0:2], in_=st6), reads=["st6_0", "st6_1"], writes=["mv"])
        S.op("act", lambda e: e.activation(out=mv[:, 2:3], in_=mv[:, 1:2], func=AF.Ln, bias=eps_c), reads=["mv", "eps_c"], writes=["mv2"])
        S.op("act", lambda e: e.activation(out=mv[:, 3:4], in_=mv[:, 2:3], func=AF.Exp, scale=-0.5), reads=["mv2"], writes=["mv3"])

    def ln_b(src, dst, gb, bb, rk, wk, gk):
        S.op("dve", lambda e: e.tensor_scalar(out=dst, in0=src, scalar1=mv[:, 0:1], scalar2=mv[:, 3:4], op0=ALU.subtract, op1=ALU.mult),
             reads=[rk, "mv", "mv3"], writes=[wk])
        S.op("dve", lambda e: e.tensor_tensor(out=dst, in0=dst, in1=gb, op=ALU.mult), reads=[wk] + gk, writes=[wk])
        S.op("dve", lambda e: e.tensor_tensor(out=dst, in0=dst, in1=bb, op=ALU.add), reads=[wk] + gk, writes=[wk])

    def p2a(tt):
        ln_a(rt, "rt")

    def p2(tt):
        b2 = tt % 2
        tsl = slice(tt * 128, (tt + 1) * 128)
        ln_b(rt, ht[b2], g1b, b1b, "rt", "ht%d" % b2, ["g1b", "b1b"])
        S.dma("sp", lambda e: e.dma_start(out=h_scr[tsl, :], in_=ht[b2]), reads=["ht%d" % b2], writes=["h_scr"])
        S.op("dve", lambda e: e.tensor_copy(out=hhi, in_=ht[b2]), reads=["ht%d" % b2], writes=["hhi"])
        S.op("dve", lambda e: e.tensor_tensor(out=hlo, in0=ht[b2], in1=hhi, op=ALU.subtract), reads=["ht%d" % b2, "hhi"], writes=["hlo"])

    def p3(tt):
        b2 = tt % 2
        for c in range(8):
            S.op("pe", lambda e, c=c: e.transpose(out=PThi[:, c * 128:(c + 1) * 128], in_=hhi[:, c * 128:(c + 1) * 128],
                                                 identity=c_bf[:, IDENT, :]), reads=["hhi", "c_bf"], writes=["PTr0"])
        for c in range(8):
            S.op("pe", lambda e, c=c: e.transpose(out=PTlo[:, c * 128:(c + 1) * 128], in_=hlo[:, c * 128:(c + 1) * 128],
                                                 identity=c_bf[:, IDENT, :]), reads=["hlo", "c_bf"], writes=["PTr1"])
        S.op("act", lambda e: e.activation(out=hTb2[b2].rearrange("p a b -> p (a b)"), in_=PThi, func=AF.Copy), reads=["PTr0"], writes=["hTb%d" % b2])
        S.op("dve", lambda e: e.tensor_copy(out=hTlo2[b2].rearrange("p a b -> p (a b)"), in_=PTlo), reads=["PTr1"], writes=["hTlo%d" % b2])

    def q_pe(tt):
        b2 = tt % 2
        k3 = 0
        for c in range(8):
            for (lt, ln_, rt_, rn_) in ((hTb2[b2], "hTb%d" % b2, Wrh, "Wrh"), (hTb2[b2], "hTb%d" % b2, Wrl, "Wrl"), (hTlo2[b2], "hTlo%d" % b2, Wrh, "Wrh")):
                S.op("pe", lambda e, c=c, lt=lt, rt_=rt_, k3=k3: e.matmul(PR[:, 0:36], lhsT=lt[:, c, :], rhs=rt_[:, c, :], start=(k3 == 0), stop=(k3 == 23)),
                     reads=[ln_, rn_], writes=["PR"])
                k3 += 1
        for hf in range(2):
            for c in range(8):
                S.op("pe", lambda e, hf=hf, c=c: e.matmul(PG[hf], lhsT=hTb2[b2][:, c, :], rhs=Wpg[:, c, hf * 512:(hf + 1) * 512],
                                                         start=(c == 0), stop=(c == 7)), reads=["hTb%d" % b2, "Wpg"], writes=["PG%d" % hf])
            S.op("act", lambda e, hf=hf: e.activation(out=sg[:, hf * 512:(hf + 1) * 512], in_=PG[hf], func=AF.Sigmoid),
                 reads=["PG%d" % hf], writes=["sg%d" % hf])

    def q_dve(tt):
        b2 = tt % 2
        tsl = slice(tt * 128, (tt + 1) * 128)
        S.op("dve", lambda e: e.tensor_tensor(out=L_all[:, tt * 36:(tt + 1) * 36], in0=PR[:, 0:36], in1=b36[:, tt * 36:(tt + 1) * 36], op=ALU.add),
             reads=["PR", "b36"], writes=["L_all"])
        for hf in range(2):
            S.op("dve", lambda e, hf=hf: e.tensor_tensor(out=sg[:, hf * 512:(hf + 1) * 512], in0=sg[:, hf * 512:(hf + 1) * 512],
                                                        in1=ple_sb[b2][:, hf * 512:(hf + 1) * 512], op=ALU.mult),
                 reads=["sg%d" % hf, "ple%d_%d" % (b2, hf)], writes=["sg%d" % hf])
        S.op("dve", lambda e: e.scalar_tensor_tensor(out=pre[b2], in0=ht[b2], scalar=ALPHA, in1=sg, op0=ALU.mult, op1=ALU.add),
             reads=["ht%d" % b2, "sg0", "sg1"], writes=["pre%d" % b2])
        S.dma("sp", lambda e: e.dma_start(out=pre_scr[tsl, :], in_=pre[b2]), reads=["pre%d" % b2], writes=["pre_scr"])

    p1(0)
    p2a(0)
    p2(0)
    p3(0)
    for tt in range(NTT):
        if tt + 1 < NTT:
            p1(tt + 1)
        q_pe(tt)
        if tt + 1 < NTT:
            p2a(tt + 1)
        q_dve(tt)
        if tt + 1 < NTT:
            p2(tt + 1)
            p3(tt + 1)

    if stop < 4:
        S.emit()
        return nc
    S.barrier()
    A.reset(PBASE)
    L3 = L_all.rearrange("p (t c) -> p t c", t=32, c=36)
    Lg = L3[:, :, 0:4]
    Le = L3[:, :, 4:36]
    gmax = A.alloc("gmax", [128, 32], F32)
    gm4 = A.alloc("gm4", [128, 32, 4], F32)
    eg = A.alloc("eg", [128, 32, 4], F32)
    gsum = A.alloc("gsum", [128, 32], F32)
    gp = A.alloc("gp", [128, 32], F32)
    Lm = A.alloc("Lm", [128, 32, 32], F32)
    top8 = A.alloc("top8", [128, 32, 8], F32)
    Oh0 = A.alloc("Oh0", [128, 32, 32], F32)
    Oh1 = A.alloc("Oh1", [128, 32, 32], F32)
    Oh2b = A.alloc("Oh2b", [128, 32 * 32], BF16)
    dv = A.alloc("dv", [128, 32], F32)
    ev = A.alloc("ev", [128, 32], F32)
    Ra = A.alloc("Ra", [128, 1024], F32)
    Rb = A.alloc("Rb", [128, 1024], F32)
    R0 = A.alloc("R0", [128, 1024], F32)
    cnt = A.alloc("cnt", [128, 32], F32)
    nb = A.alloc("nb", [128, 32], F32)
    pa = A.alloc("pa", [128, 32], F32)
    pb = A.alloc("pb", [128, 32], F32)
    pstart = A.alloc("pstart", [128, 32], F32)
    dfl = A.alloc("dfl", [128, 64], F32)
    cmp3 = A.alloc("cmp3", [128, NBLK, 32], F32)
    bexp = A.alloc("bexp", [128, NBLK], F32)
    idxf = A.alloc("idxf", [128, NBLK * 8], F32)

    def dv_(fn, reads, writes):
        S.op("dve", fn, reads=reads, writes=writes)

    dv_(lambda e: e.tensor_reduce(out=gmax, in_=Lg, axis=AX.X, op=ALU.max), ["L_all"], ["gmax"])
    gmax_b = gmax.unsqueeze(2).to_broadcast([128, 32, 4])
    dv_(lambda e: e.tensor_tensor(out=gm4, in0=Lg, in1=gmax_b, op=ALU.is_ge), ["L_all", "gmax"], ["gm4"])
    dv_(lambda e: e.tensor_tensor(out=eg, in0=Lg, in1=gmax_b, op=ALU.subtract), ["L_all", "gmax"], ["eg"])
    S.op("act", lambda e: e.activation(out=eg, in_=eg, func=AF.Exp), reads=["eg"], writes=["eg"])
    dv_(lambda e: e.tensor_reduce(out=gsum, in_=eg, axis=AX.X, op=ALU.add), ["eg"], ["gsum"])
    dv_(lambda e: e.reciprocal(out=gp, in_=gsum), ["gsum"], ["gp"])
    dv_(lambda e: e.tensor_scalar(out=gm4, in0=gm4, scalar1=1.0, scalar2=1e30, op0=ALU.subtract, op1=ALU.mult), ["gm4"], ["gm4"])
    dv_(lambda e: e.tensor_tensor(out=Lm.rearrange("p t (g k) -> p t g k", g=4, k=8), in0=Le.rearrange("p t (g k) -> p t g k", g=4, k=8),
                                  in1=gm4.unsqueeze(3).to_broadcast([128, 32, 4, 8]), op=ALU.add), ["L_all", "gm4"], ["Lm"])
    for t in range(32):
        dv_(lambda e, t=t: e.max(out=top8[:, t, :], in_=Lm[:, t, :]), ["Lm"], ["top8"])
    v0 = top8[:, :, 0]
    v1 = top8[:, :, 1]
    dv_(lambda e: e.tensor_tensor(out=Oh0, in0=Lm, in1=top8[:, :, 0:1].to_broadcast([128, 32, 32]), op=ALU.is_equal), ["Lm", "top8"], ["Oh0"])
    dv_(lambda e: e.tensor_tensor(out=Oh1, in0=Lm, in1=top8[:, :, 1:2].to_broadcast([128, 32, 32]), op=ALU.is_equal), ["Lm", "top8"], ["Oh1"])
    dv_(lambda e: e.tensor_tensor(out=dv, in0=v1, in1=v0, op=ALU.subtract), ["top8"], ["dv"])
    S.op("act", lambda e: e.activation(out=ev, in_=dv, func=AF.Exp), reads=["dv"], writes=["ev"])
    dv_(lambda e: e.tensor_scalar(out=dv, in0=ev, scalar1=1.0, scalar2=None, op0=ALU.add), ["ev"], ["dv"])
    dv_(lambda e: e.reciprocal(out=gsum, in_=dv), ["dv"], ["gsum"])
    dv_(lambda e: e.tensor_tensor(out=g0_all, in0=gsum, in1=gp, op=ALU.mult), ["gsum", "gp"], ["g0_all"])
    dv_(lambda e: e.tensor_tensor(out=g1_all, in0=g0_all, in1=ev, op=ALU.mult), ["g0_all", "ev"], ["g1_all"])
    dv_(lambda e: e.tensor_tensor(out=Oh2b.rearrange("p (t c) -> p t c", t=32, c=32), in0=Oh0, in1=Oh1, op=ALU.add), ["Oh0", "Oh1"], ["Oh2b"])
    for hf in range(2):
        S.op("pe", lambda e, hf=hf: e.matmul(PS[hf], lhsT=c_bf[:, STRICT, :], rhs=Oh2b[:, hf * 512:(hf + 1) * 512], start=True, stop=True),
             reads=["c_bf", "Oh2b"], writes=["ps%d" % hf])
        S.op("pe", lambda e, hf=hf: e.matmul(PS[2 + hf], lhsT=c_bf[:, ONES, :], rhs=Oh2b[:, hf * 512:(hf + 1) * 512], start=True, stop=True),
             reads=["c_bf", "Oh2b"], writes=["ps%d" % (2 + hf)])
        dv_(lambda e, hf=hf: e.tensor_copy(out=R0[:, hf * 512:(hf + 1) * 512], in_=PS[2 + hf]), ["ps%d" % (2 + hf)], ["R0"])
        dv_(lambda e, hf=hf: e.tensor_copy(out=Ra[:, hf * 512:(hf + 1) * 512], in_=PS[2 + hf]), ["ps%d" % (2 + hf)], ["Ra"])
    cur, oth, cn, on = Ra, Rb, "Ra", "Rb"
    for dsh in (1, 2, 4, 8, 16):
        w_ = 32 * dsh
        dv_(lambda e, cur=cur, oth=oth, w_=w_: e.tensor_tensor(out=oth[:, w_:], in0=cur[:, w_:], in1=cur[:, :1024 - w_], op=ALU.add), [cn], [on])
        dv_(lambda e, cur=cur, oth=oth, w_=w_: e.tensor_copy(out=oth[:, :w_], in_=cur[:, :w_]), [cn, on], [on])
        cur, oth, cn, on = oth, cur, on, cn
    Rincl, rin = cur, cn
    Rk, rkn = oth, on
    dv_(lambda e: e.tensor_copy(out=cnt, in_=Rincl[:, 31 * 32:32 * 32]), [rin], ["cnt"])
    dv_(lambda e: e.tensor_tensor(out=Rk, in0=Rincl, in1=R0, op=ALU.subtract), [rin, "R0"], [rkn])
    for hf in range(2):
        dv_(lambda e, hf=hf: e.tensor_tensor(out=Rk[:, hf * 512:(hf + 1) * 512], in0=Rk[:, hf * 512:(hf + 1) * 512], in1=PS[hf], op=ALU.add),
            [rkn, "ps%d" % hf], [rkn])
    dv_(lambda e: e.memset(nb, 0.0), [], ["nb"])
    for j in range(32):
        dv_(lambda e, j=j: e.scalar_tensor_tensor(out=nb, in0=cnt, scalar=float(128 * j), in1=nb, op0=ALU.is_gt, op1=ALU.add), ["cnt", "nb"], ["nb"])
    dv_(lambda e: e.tensor_scalar(out=nb, in0=nb, scalar1=128.0, scalar2=None, op0=ALU.mult), ["nb"], ["nb"])
    dv_(lambda e: e.tensor_copy(out=pa, in_=nb), ["nb"], ["pa"])
    cur, oth, cn, on = pa, pb, "pa", "pb"
    for dsh in (1, 2, 4, 8, 16):
        dv_(lambda e, cur=cur, oth=oth, dsh=dsh: e.tensor_tensor(out=oth[:, dsh:], in0=cur[:, dsh:], in1=cur[:, :32 - dsh], op=ALU.add), [cn], [on])
        dv_(lambda e, cur=cur, oth=oth, dsh=dsh: e.tensor_copy(out=oth[:, :dsh], in_=cur[:, :dsh]), [cn, on], [on])
        cur, oth, cn, on = oth, cur, on, cn
    pend, pen_n = cur, cn
    dv_(lambda e: e.tensor_tensor(out=pstart, in0=pend, in1=nb, op=ALU.subtract), [pen_n, "nb"], ["pstart"])
    Rk3 = Rk.rearrange("p (t c) -> p t c", t=32, c=32)
    dv_(lambda e: e.tensor_tensor(out=Rk3, in0=Rk3, in1=pstart.unsqueeze(1).to_broadcast([128, 32, 32]), op=ALU.add), [rkn, "pstart"], [rkn])
    dv_(lambda e: e.tensor_tensor(out=Oh0, in0=Oh0, in1=Rk3, op=ALU.mult), ["Oh0", rkn], ["Oh0"])
    dv_(lambda e: e.tensor_tensor(out=Oh1, in0=Oh1, in1=Rk3, op=ALU.mult), ["Oh1", rkn], ["Oh1"])
    dv_(lambda e: e.tensor_reduce(out=dfl[:, 0:32], in_=Oh0, axis=AX.X, op=ALU.add), ["Oh0"], ["dfl0"])
    dv_(lambda e: e.tensor_reduce(out=dfl[:, 32:64], in_=Oh1, axis=AX.X, op=ALU.add), ["Oh1"], ["dfl1"])
    dv_(lambda e: e.tensor_copy(out=dest0_i, in_=dfl[:, 0:32]), ["dfl0"], ["dest0_i"])
    dv_(lambda e: e.tensor_copy(out=dest1_i, in_=dfl[:, 32:64]), ["dfl1"], ["dest1_i"])
    thr = iot_sb[:, 44:140]
    dv_(lambda e: e.tensor_tensor(out=cmp3, in0=pend.unsqueeze(1).to_broadcast([128, NBLK, 32]), in1=thr.unsqueeze(2).to_broadcast([128, NBLK, 32]),
                                  op=ALU.is_le), [pen_n, "iot"], ["cmp3"])
    dv_(lambda e: e.tensor_reduce(out=bexp, in_=cmp3, axis=AX.X, op=ALU.add), ["cmp3"], ["bexp"])
    dv_(lambda e: e.tensor_scalar(out=bexp, in0=bexp, scalar1=31.0, scalar2=None, op0=ALU.min), ["bexp"], ["bexp"])
    chg = idxf[:, 0:NBLK]
    e2 = idxf[:, NBLK:2 * NBLK]
    dv_(lambda e: e.memset(chg[:, 0:1], 1.0), [], ["chg0"])
    dv_(lambda e: e.tensor_tensor(out=chg[:, 1:NBLK], in0=bexp[:, 1:NBLK], in1=bexp[:, 0:NBLK - 1], op=ALU.not_equal), ["bexp"], ["chg1"])
    dv_(lambda e: e.tensor_scalar(out=chg, in0=chg, scalar1=-1.0e7, scalar2=1.0e7, op0=ALU.mult, op1=ALU.add), ["chg0", "chg1"], ["chg"])
    dv_(lambda e: e.tensor_scalar(out=e2, in0=bexp, scalar1=128.0, scalar2=None, op0=ALU.mult), ["bexp"], ["e2"])
    dv_(lambda e: e.tensor_tensor(out=e2, in0=e2, in1=chg, op=ALU.add), ["e2", "chg"], ["e2"])
    dv_(lambda e: e.scalar_tensor_tensor(out=e2, in0=iot_sb[:, 0:1].to_broadcast([128, NBLK]), scalar=1.0, in1=e2, op0=ALU.mult, op1=ALU.add),
        ["e2", "iot"], ["e2"])
    dv_(lambda e: e.tensor_copy(out=idxA, in_=e2), ["e2"], ["idxA"])
    dv_(lambda e: e.tensor_scalar(out=e2, in0=e2, scalar1=1.0, scalar2=None, op0=ALU.add), ["e2", "idxA"], ["e2"])
    dv_(lambda e: e.tensor_copy(out=idxB, in_=e2), ["e2"], ["idxB"])

    if stop < 5:
        S.emit()
        return nc
    S.barrier()
    A.reset(PBASE)
    hrow = [A.alloc("hrow", [128, D], F32) for _ in range(2)]
    for tt in range(NTT):
        b2 = tt % 2
        tsl = slice(tt * 128, (tt + 1) * 128)
        S.dma("sp", lambda e, b2=b2, tsl=tsl: e.dma_start(out=hrow[b2], in_=h_scr[tsl, :]), reads=["h_scr"], writes=["hrow%d" % b2])
        for k, di in enumerate((dest0_i, dest1_i)):
            S.dma("pool", lambda e, b2=b2, tt=tt, di=di: e.indirect_dma_start(
                out=xs_scr, out_offset=bass.IndirectOffsetOnAxis(ap=di[:, tt:tt + 1], axis=0), in_=hrow[b2], in_offset=None),
                reads=["hrow%d" % b2, "dest0_i", "dest1_i"], writes=["xs_scr%d" % k])

    if stop < 6:
        S.emit()
        return nc
    S.barrier()
    xb = [A.alloc("xb", [128, D], BF16) for _ in range(2)]
    xTk = [A.alloc("xTk", [128, 8, 128], BF16) for _ in range(2)]
    Wg = A.alloc("Wg", [128, 8, 512], BF16)
    Wu = A.alloc("Wu", [128, 8, 512], BF16)
    Wd = A.alloc("Wd", [128, 4, 1024], BF16)
    Wgs = A.alloc("Wgs", [128, 4096], F32)
    Wus = A.alloc("Wus", [128, 4096], F32)
    Wds = A.alloc("Wds", [128, 4096], F32)
    sil = A.alloc("sil", [128, 512], F32)
    hdn = A.alloc("hdn", [128, 512], BF16)
    hdT = A.alloc("hdT", [128, 4, 128], BF16)
    yb = [A.alloc("yb", [128, D], F32) for _ in range(2)]
    breg = {}

    def bound_reg(e):
        if "r" not in breg:
            r = e.alloc_register("wbound")
            e.reg_mov(r, 32 * 128 - 1)
            breg["r"] = r
        return breg["r"]

    PTb = PS[0].bitcast(BF16)
    PGa, PUa = PS[1], PS[2]
    PTh = PS[3].bitcast(BF16)
    PY = [PS[4], PS[5]]
    def wload(b, which):
        for (wt, ws, wn, src) in which:
            wflat = wt.rearrange("p a b -> p (a b)")
            S.dma("pool", lambda e, ws=ws, src=src: e.indirect_dma_start(
                out=ws, out_offset=None, in_=src,
                in_offset=bass.IndirectOffsetOnAxis(ap=idxA[:, b:b + 1], axis=0), bounds_check=bound_reg(e), oob_is_err=False),
                reads=["idxA"], writes=[wn + "s"])
            S.op("act", lambda e, wflat=wflat, ws=ws: e.activation(out=wflat[:, 0:2048], in_=ws[:, 0:2048], func=AF.Copy),
                 reads=[wn + "s"], writes=[wn + "a"])
            S.op("dve", lambda e, wflat=wflat, ws=ws: e.tensor_copy(out=wflat[:, 2048:4096], in_=ws[:, 2048:4096]),
                 reads=[wn + "s"], writes=[wn + "b"])

    WG = ((Wg, Wgs, "Wg", w_gate),)
    WU = ((Wu, Wus, "Wu", w_up),)
    WD = ((Wd, Wds, "Wd", w_down),)

    def stA(b):
        b2 = b % 2
        rsl = slice(b * 128, (b + 1) * 128)
        S.dma("sp", lambda e: e.dma_start(out=xb[b2], in_=xs_scr[rsl, :]), reads=["xs_scr0", "xs_scr1"], writes=["xb%d" % b2])
        for c in range(8):
            S.op("pe", lambda e, c=c: e.transpose(out=PTb[:, c * 128:(c + 1) * 128], in_=xb[b2][:, c * 128:(c + 1) * 128], identity=c_bf[:, IDENT, :]),
                 reads=["xb%d" % b2, "c_bf"], writes=["PTb"])
        S.op("dve", lambda e: e.tensor_copy(out=xTk[b2].rearrange("p a b -> p (a b)"), in_=PTb), reads=["PTb"], writes=["xTk%d" % b2])

    def stG(b):
        b2 = b % 2
        for c in range(8):
            S.op("pe", lambda e, c=c: e.matmul(PGa, lhsT=xTk[b2][:, c, :], rhs=Wg[:, c, :], start=(c == 0), stop=(c == 7)),
                 reads=["xTk%d" % b2, "Wga", "Wgb"], writes=["PGa"])
        S.op("act", lambda e: e.activation(out=sil, in_=PGa, func=AF.Silu), reads=["PGa"], writes=["sil"])

    def stU(b):
        b2 = b % 2
        for c in range(8):
            S.op("pe", lambda e, c=c: e.matmul(PUa, lhsT=xTk[b2][:, c, :], rhs=Wu[:, c, :], start=(c == 0), stop=(c == 7)),
                 reads=["xTk%d" % b2, "Wua", "Wub"], writes=["PUa"])
        S.op("dve", lambda e: e.tensor_tensor(out=hdn, in0=sil, in1=PUa, op=ALU.mult), reads=["sil", "PUa"], writes=["hdn"])

    def stC1(b):
        for c in range(4):
            S.op("pe", lambda e, c=c: e.transpose(out=PTh[:, c * 128:(c + 1) * 128], in_=hdn[:, c * 128:(c + 1) * 128], identity=c_bf[:, IDENT, :]),
                 reads=["hdn", "c_bf"], writes=["PTh"])
        S.op("dve", lambda e: e.tensor_copy(out=hdT.rearrange("p a b -> p (a b)"), in_=PTh[:, 0:512]), reads=["PTh"], writes=["hdT"])

    def stC2(b):
        b2 = b % 2
        rsl = slice(b * 128, (b + 1) * 128)
        for hf in range(2):
            for c in range(4):
                S.op("pe", lambda e, hf=hf, c=c: e.matmul(PY[hf], lhsT=hdT[:, c, :], rhs=Wd[:, c, hf * 512:(hf + 1) * 512],
                                                         start=(c == 0), stop=(c == 3)), reads=["hdT", "Wda", "Wdb"], writes=["PY%d" % hf])
            evac(yb[b2][:, hf * 512:(hf + 1) * 512], PY[hf], ["PY%d" % hf], ["yb%d_%d" % (b2, hf)])
        S.dma("sp", lambda e: e.dma_start(out=ys_scr[rsl, :], in_=yb[b2]), reads=["yb%d_0" % b2, "yb%d_1" % b2], writes=["ys_scr"])

    stA(0)
    wload(0, WG)
    wload(0, WU)
    for b in range(NBLK):
        if b + 1 < NBLK:
            stA(b + 1)
        if b > 0:
            stC1(b - 1)
        stG(b)
        if b + 1 < NBLK:
            wload(b + 1, WG)
        if b > 0:
            stC2(b - 1)
        wload(b, WD)
        stU(b)
        if b + 1 < NBLK:
            wload(b + 1, WU)
    stC1(NBLK - 1)
    stC2(NBLK - 1)

    if stop < 7:
        S.emit()
        return nc
    S.barrier()
    A.reset(PBASE)
    g2b = A.alloc("g2b", [128, D], F32); b2b = A.alloc("b2b", [128, D], F32)
    NB3 = 3
    y0 = [A.alloc("y0", [128, D], F32) for _ in range(NB3)]
    y1 = [A.alloc("y1", [128, D], F32) for _ in range(NB3)]
    pr = [A.alloc("pr", [128, D], F32) for _ in range(NB3)]
    ot = [A.alloc("ot", [128, D], F32) for _ in range(2)]
    st7 = [A.alloc("st7", [128, 12], F32) for _ in range(2)]
    mv7 = [A.alloc("mv7", [128, 4], F32) for _ in range(2)]
    S.dma("sp", lambda e: e.dma_start(out=g2b, in_=ln2g.partition_broadcast(128)), writes=["g2b"])
    S.dma("sp", lambda e: e.dma_start(out=b2b, in_=ln2b.partition_broadcast(128)), writes=["b2b"])

    def ld7(tt):
        b3 = tt % NB3
        tsl = slice(tt * 128, (tt + 1) * 128)
        S.dma("sp", lambda e: e.dma_start(out=pr[b3], in_=pre_scr[tsl, :]), reads=["pre_scr"], writes=["pr%d" % b3])
        S.dma("pool", lambda e: e.indirect_dma_start(
            out=y0[b3], out_offset=None, in_=ys_scr, in_offset=bass.IndirectOffsetOnAxis(ap=dest0_i[:, tt:tt + 1], axis=0)),
            reads=["ys_scr", "dest0_i"], writes=["y0_%d" % b3])
        S.dma("pool", lambda e: e.indirect_dma_start(
            out=y1[b3], out_offset=None, in_=ys_scr, in_offset=bass.IndirectOffsetOnAxis(ap=dest1_i[:, tt:tt + 1], axis=0)),
            reads=["ys_scr", "dest1_i"], writes=["y1_%d" % b3])

    def cmb7(tt):
        b3 = tt % NB3; m2 = tt % 2
        S.op("dve", lambda e: e.scalar_tensor_tensor(out=pr[b3], in0=y0[b3], scalar=g0_all[:, tt:tt + 1], in1=pr[b3], op0=ALU.mult, op1=ALU.add),
             reads=["y0_%d" % b3, "pr%d" % b3, "g0_all"], writes=["pr%d" % b3])
        S.op("dve", lambda e: e.scalar_tensor_tensor(out=pr[b3], in0=y1[b3], scalar=g1_all[:, tt:tt + 1], in1=pr[b3], op0=ALU.mult, op1=ALU.add),
             reads=["y1_%d" % b3, "pr%d" % b3, "g1_all"], writes=["pr%d" % b3])
        for hf in range(2):
            S.op("dve", lambda e, hf=hf: e.bn_stats(out=st7[m2][:, hf * 6:(hf + 1) * 6], in_=pr[b3][:, hf * 512:(hf + 1) * 512]),
                 reads=["pr%d" % b3], writes=["st7_%d_%d" % (m2, hf)])
        S.op("dve", lambda e: e.bn_aggr(out=mv7[m2][:, 0:2], in_=st7[m2]), reads=["st7_%d_0" % m2, "st7_%d_1" % m2], writes=["mv7a%d" % m2])
        S.op("act", lambda e: e.activation(out=mv7[m2][:, 2:3], in_=mv7[m2][:, 1:2], func=AF.Ln, bias=eps_c), reads=["mv7a%d" % m2, "eps_c"], writes=["mv7b%d" % m2])
        S.op("act", lambda e: e.activation(out=mv7[m2][:, 3:4], in_=mv7[m2][:, 2:3], func=AF.Exp, scale=-0.5), reads=["mv7b%d" % m2], writes=["mv7c%d" % m2])

    def fin7(tt):
        b3 = tt % NB3; m2 = tt % 2
        tsl = slice(tt * 128, (tt + 1) * 128)
        S.op("dve", lambda e: e.tensor_scalar(out=ot[m2], in0=pr[b3], scalar1=mv7[m2][:, 0:1], scalar2=mv7[m2][:, 3:4], op0=ALU.subtract, op1=ALU.mult),
             reads=["pr%d" % b3, "mv7a%d" % m2, "mv7c%d" % m2], writes=["ot%d" % m2])
        S.op("dve", lambda e: e.tensor_tensor(out=ot[m2], in0=ot[m2], in1=g2b, op=ALU.mult), reads=["ot%d" % m2, "g2b"], writes=["ot%d" % m2])
        S.op("dve", lambda e: e.tensor_tensor(out=ot[m2], in0=ot[m2], in1=b2b, op=ALU.add), reads=["ot%d" % m2, "b2b"], writes=["ot%d" % m2])
        S.dma("sp", lambda e: e.dma_start(out=out[tsl, :], in_=ot[m2]), reads=["ot%d" % m2], writes=["out"], is_output=True)

    ld7(0)
    ld7(1)
    for tt in range(NTT):
        cmb7(tt)
        if tt > 0:
            fin7(tt - 1)
        if tt + 2 < NTT:
            ld7(tt + 2)
    fin7(NTT - 1)

    S.emit()
    return nc


_CACHE = {}


def _prep_inputs(x, p, w_in, b_forget, w_out, ln_mix_g, ln_mix_b, w_group, b_group, w_router, b_router,
                 w_gate, w_up, w_down, w_ple, w_ple_gate, ln_ffn_g, ln_ffn_b):
    f32 = np.float32
    x = np.asarray(x, f32); p = np.asarray(p, f32)
    w_in = np.asarray(w_in, f32)[0]
    kp_cols, qp_cols = [], []
    for h in range(8):
        kp_cols += list(range(512 + 64 * h, 512 + 64 * h + 64)) + list(range(2048 + 64 * h, 2048 + 64 * h + 64))
        qp_cols += list(range(0 + 64 * h, 64 * h + 64)) + list(range(1536 + 64 * h, 1536 + 64 * h + 64))
    v_cols = list(range(1024, 1536)) + list(range(2560, 3072))

    def pcl(w):
        return np.ascontiguousarray(w.reshape(8, 128, -1).transpose(1, 0, 2))

    shared = {
        "w_kp": pcl(w_in[:, kp_cols]), "w_qp": pcl(w_in[:, qp_cols]), "w_v": pcl(w_in[:, v_cols]),
        "w_f": pcl(w_in[:, 3072:3080]),
        "bf_t": np.ascontiguousarray(np.tile(np.asarray(b_forget, f32)[0], 64).reshape(1, 512)),
        "w_out": pcl(np.asarray(w_out, f32)[0]),
        "ln1g": np.asarray(ln_mix_g, f32).reshape(1, D), "ln1b": np.asarray(ln_mix_b, f32).reshape(1, D),
        "ln2g": np.asarray(ln_ffn_g, f32).reshape(1, D), "ln2b": np.asarray(ln_ffn_b, f32).reshape(1, D),
        "w_r36": pcl(np.concatenate([np.asarray(w_group, f32)[0], np.asarray(w_router, f32)[0]], axis=1)),
        "b_r36": np.ascontiguousarray(np.tile(np.concatenate([np.asarray(b_group, f32)[0], np.asarray(b_router, f32)[0]]), 32).reshape(1, 32 * 36)),
        "w_gate": np.ascontiguousarray(np.asarray(w_gate, f32)[0].reshape(32, 8, 128, 512).transpose(0, 2, 1, 3).reshape(32 * 128, 4096)),
        "w_up": np.ascontiguousarray(np.asarray(w_up, f32)[0].reshape(32, 8, 128, 512).transpose(0, 2, 1, 3).reshape(32 * 128, 4096)),
        "w_down": np.ascontiguousarray(np.asarray(w_down, f32)[0].reshape(32, 4, 128, 1024).transpose(0, 2, 1, 3).reshape(32 * 128, 4096)),
        "w_ple": np.ascontiguousarray(np.asarray(w_ple, f32)[0].reshape(2, 128, 1024).transpose(1, 0, 2)),
        "w_pg": pcl(np.asarray(w_ple_gate, f32)[0]),
    }
    k = np.arange(128)[:, None]; q = np.arange(128)[None, :]
    cst = np.zeros((128, 7, 128), f32)
    cst[:, 6, :] = -1.0
    cst[:, 0, :] = (k == q)
    cst[:, 1, :] = (k <= q)
    cst[:, 2, :] = (k < q)
    cst[:, 3, :] = 1.0
    cst[:, 4, :] = -(k >= q).astype(f32)
    cst[:, 5, :] = -(k == q).astype(f32)
    shared["consts"] = cst
    iot = np.zeros((128, 140), f32)
    iot[:, 0:12] = np.arange(12)[None, :] * 128 + np.arange(128)[:, None]
    iot[:, 44:140] = np.arange(96)[None, :] * 128.0
    shared["iot"] = iot

    def diag_tiles(strict):
        t = np.zeros((4, 128, 512), f32)
        for i in range(4):
            for jq in range(4):
                blk = t[i, :, jq * 128:(jq + 1) * 128]
                if jq < i:
                    blk[:] = BIG
                elif jq == i:
                    blk[:] = np.where((k < q) if strict else (k <= q), 0.0, BIG)
        return t

    in_maps = []
    for c in range(NCORE):
        b, par = c // 2, c % 2
        G = G_PAR[par]
        loc = np.concatenate([np.arange(g * 512, (g + 1) * 512) for g in G])
        xT = np.ascontiguousarray(x[b].reshape(SEQ, 8, 128).transpose(2, 1, 0))
        m = dict(shared)
        m["xT_all"] = xT
        m["xT_loc"] = np.ascontiguousarray(xT[:, :, loc])
        m["x_loc"] = np.ascontiguousarray(x[b][loc])
        m["pT_loc"] = np.ascontiguousarray(p[0, b][loc].reshape(NLOC, 2, 128).transpose(2, 1, 0))
        mk = np.zeros((2, 2, 8, 128, 512), f32)
        for kind in range(2):
            dt_ = diag_tiles(strict=(kind == 0))
            for sp_ in range(2):
                has_max = (sp_ == 0) if par == 1 else (sp_ == 1)
                if has_max:
                    mk[kind, sp_, 4:8] = dt_
                else:
                    mk[kind, sp_, 0:4] = dt_
                    mk[kind, sp_, 4:8] = BIG
        m["masks"] = np.ascontiguousarray(mk.reshape(32, 128, 512).transpose(1, 0, 2))
        sel = np.zeros((8, 16, 8), f32)
        for s_, g in enumerate(G):
            sel[s_, g, :] = 1.0
        m["selx"] = sel.reshape(1, 1024)
        in_maps.append(m)
    return in_maps


def kernel(**inputs):
    if "nc" not in _CACHE:
        _CACHE["nc"] = build()
    nc = _CACHE["nc"]
    in_maps = _prep_inputs(**inputs)
    res = run_bass_kernel_spmd(nc, in_maps, core_ids=list(range(NCORE)))
    outp = np.zeros((NB, SEQ, D), np.float32)
    for c in range(NCORE):
        b, par = c // 2, c % 2
        loc = np.concatenate([np.arange(g * 512, (g + 1) * 512) for g in G_PAR[par]])
        outp[b, loc] = res.results[c]["out"]
    return outp
```

```python
import contextlib
import os
import numpy as np
import concourse.bass as bass
import concourse.mybir as mybir
from concourse.bass_utils import run_bass_kernel_spmd

F32 = mybir.dt.float32
BF16 = mybir.dt.bfloat16
I32 = mybir.dt.int32
AF = mybir.ActivationFunctionType
ALU = mybir.AluOpType
AX = mybir.AxisListType

D = 1024
SEQ = 8192
NB = 4
NCORE = 8
NLOC = 4096
NTT = 32
NBLK = 96
CAP = NBLK * 128
ALPHA = 2 ** 0.25
LN_EPS = 1e-5
BIG = -30000.0
G_PAR = ([0, 3, 4, 7, 8, 11, 12, 15], [1, 2, 5, 6, 9, 10, 13, 14])


class Sched:
    ENGS = ("pe", "act", "dve", "pool", "sp")

    def __init__(self, nc, n_dma_sems=10):
        self.nc = nc
        self.ops = {e: [] for e in self.ENGS}
        self.cnt = {e: 0 for e in self.ENGS}
        self.last_w = {}
        self.readers = {}
        self.seen = {e: {} for e in self.ENGS}
        self.n_dma_sems = n_dma_sems
        self.dma_used = {}
        self.dma_rr = {"sp": 0, "pool": 0, "act": 0}
        self.out_tokens = []

    def _deps(self, eng, reads, writes):
        deps = {}

        def add(k, v):
            if deps.get(k, 0) < v:
                deps[k] = v

        for k in reads:
            t = self.last_w.get(k)
            if t:
                add(*t)
        for k in writes:
            t = self.last_w.get(k)
            if t:
                add(*t)
            for kk, vv in self.readers.get(k, {}).items():
                add(kk, vv)
        waits = []
        for k, v in deps.items():
            if k == "pe" and eng == "pe":
                continue
            if self.seen[eng].get(k, 0) >= v:
                continue
            self.seen[eng][k] = v
            waits.append((k, v))
        return waits

    def _commit(self, tok, reads, writes):
        for k in reads:
            r = self.readers.setdefault(k, {})
            if r.get(tok[0], 0) < tok[1]:
                r[tok[0]] = tok[1]
        for k in writes:
            self.last_w[k] = tok
            self.readers[k] = {}

    def op(self, eng, fn, reads=(), writes=()):
        waits = self._deps(eng, reads, writes)
        self.cnt[eng] += 1
        tok = (eng, self.cnt[eng])
        self.ops[eng].append((fn, waits, (eng, 1)))
        self._commit(tok, reads, writes)
        return tok

    def dma(self, q, fn, reads=(), writes=(), is_output=False):
        waits = self._deps(q, reads, writes)
        slot = self.dma_rr[q] % self.n_dma_sems
        self.dma_rr[q] += 1
        key = "dma_%s_%d" % (q, slot)
        used = self.dma_used.get(key, 0)
        if used > 0 and self.seen[q].get(key, 0) < 16 * used:
            self.seen[q][key] = 16 * used
            waits.append((key, 16 * used))
        self.dma_used[key] = used + 1
        tok = (key, 16 * (used + 1))
        self.ops[q].append((fn, waits, (key, 16)))
        self._commit(tok, reads, writes)
        if is_output:
            self.out_tokens.append(tok)
        return tok

    def barrier(self):
        toks = [(e, self.cnt[e]) for e in self.ENGS if self.cnt[e] > 0]
        toks += [(k, 16 * u) for k, u in self.dma_used.items()]
        for e in self.ENGS:
            waits = []
            for k, v in toks:
                if k == e and e == "pe":
                    continue
                if self.seen[e].get(k, 0) >= v:
                    continue
                self.seen[e][k] = v
                waits.append((k, v))
            if waits:
                self.ops[e].append((None, waits, None))

    def emit(self):
        nc = self.nc
        fin = []
        for e in self.ENGS:
            if self.cnt[e] > 0 and self.seen["sp"].get(e, 0) < self.cnt[e]:
                fin.append((e, self.cnt[e]))
        for k, u in self.dma_used.items():
            if self.seen["sp"].get(k, 0) < 16 * u:
                fin.append((k, 16 * u))
        self.ops["sp"].append((None, fin, None))
        keys = set()
        for e in self.ENGS:
            for fn, waits, inc in self.ops[e]:
                for k, v in waits:
                    keys.add(k)
                if inc is not None:
                    keys.add(inc[0])
        with contextlib.ExitStack() as st:
            sems = {}
            for k in sorted(keys):
                sems[k] = st.enter_context(nc.semaphore("s_" + k))
            block = st.enter_context(nc.Block())

            def run(engname):
                def body(eng):
                    for fn, waits, inc in self.ops[engname]:
                        for k, v in waits:
                            eng.wait_ge(sems[k], v)
                        if fn is not None:
                            ins = fn(eng)
                            ins.then_inc(sems[inc[0]], inc[1])
                return body

            block.tensor(run("pe"))
            block.scalar(run("act"))
            block.vector(run("dve"))
            block.gpsimd(run("pool"))
            block.sync(run("sp"))


class Arena:
    def __init__(self, nc, limit=229376 - 256):
        self.nc = nc
        self.off = 16640
        self.limit = limit
        self.n = 0

    def alloc(self, name, shape, dtype):
        esz = {F32: 4, BF16: 2, I32: 4}[dtype]
        per = esz
        for s in shape[1:]:
            per *= s
        self.off = (self.off + 63) // 64 * 64
        self.n += 1
        t = self.nc.alloc_sbuf_tensor_at("%s_%d" % (name, self.n), list(shape), dtype, offset=self.off)
        self.off += per
        assert self.off <= self.limit, (name, self.off)
        return t.ap()

    def mark(self):
        return self.off

    def reset(self, m):
        self.off = m


def build(debug=False, stop=99):
    nc = bass.Bass("TRN2", target_bir_lowering=False)
    S = Sched(nc)
    A = Arena(nc)

    def din(name, shape, dt=F32):
        return nc.dram_tensor(name, list(shape), dt, kind="ExternalInput").ap()

    def dscr(name, shape, dt, dbg=False):
        return nc.dram_tensor(name, list(shape), dt, kind=("ExternalOutput" if (dbg and debug) else "Internal")).ap()

    xT_all = din("xT_all", [128, 8, SEQ])
    xT_loc = din("xT_loc", [128, 8, NLOC])
    x_loc = din("x_loc", [NLOC, D])
    pT_loc = din("pT_loc", [128, 2, NLOC])
    w_kp = din("w_kp", [128, 8, 1024])
    w_qp = din("w_qp", [128, 8, 1024])
    w_v = din("w_v", [128, 8, 1024])
    w_f = din("w_f", [128, 8, 8])
    bf_t = din("bf_t", [1, 512])
    w_out = din("w_out", [128, 8, 1024])
    ln1g = din("ln1g", [1, D]); ln1b = din("ln1b", [1, D])
    ln2g = din("ln2g", [1, D]); ln2b = din("ln2b", [1, D])
    w_r36 = din("w_r36", [128, 8, 36])
    b_r36 = din("b_r36", [1, 32 * 36])
    w_gate = din("w_gate", [32 * 128, 4096])
    w_up = din("w_up", [32 * 128, 4096])
    w_down = din("w_down", [32 * 128, 4096])
    w_ple = din("w_ple", [128, 2, 1024])
    w_pg = din("w_pg", [128, 8, 1024])
    masks = din("masks", [128, 32, 512])
    selx = din("selx", [1, 1024])
    consts = din("consts", [128, 7, 128])
    iot = din("iot", [128, 12 + 32 + 96], F32)
    out = nc.dram_tensor("out", [NLOC, D], F32, kind="ExternalOutput").ap()

    kt_scr = dscr("kt_scr", [8, 128, SEQ], BF16)
    v_scr = dscr("v_scr", [8, 128, 64 * 128], BF16)
    qt_scr = dscr("qt_scr", [8, 128, NLOC], BF16)
    h_scr = dscr("h_scr", [NLOC, D], F32, dbg=True)
    pre_scr = dscr("pre_scr", [NLOC, D], F32, dbg=True)
    xs_scr = dscr("xs_scr", [CAP, D], BF16)
    ys_scr = dscr("ys_scr", [CAP, D], F32)
    dbg_r = dscr("dbg_r", [128, 4096], F32, dbg=True)

    PSALL = nc.alloc_psum_tensor("psall", [128, 4096], F32).ap()
    PS = [PSALL[:, i * 512:(i + 1) * 512] for i in range(8)]

    c_f32 = A.alloc("c_f32", [128, 7, 128], F32)
    c_bf = A.alloc("c_bf", [128, 7, 128], BF16)
    IDENT, TRII, STRICT, ONES, NEGTRI, NEGID, NEGONE = range(7)
    cfpos = A.alloc("cfpos", [128, 512], F32)
    cref = A.alloc("cref", [128, 64], F32)
    L_all = A.alloc("L_all", [128, 32 * 36], F32)
    g0_all = A.alloc("g0_all", [128, 32], F32)
    g1_all = A.alloc("g1_all", [128, 32], F32)
    dest0_i = A.alloc("dest0_i", [128, 32], I32)
    dest1_i = A.alloc("dest1_i", [128, 32], I32)
    idxA = A.alloc("idxA", [128, NBLK], I32)
    idxB = A.alloc("idxB", [128, NBLK], I32)
    iot_sb = A.alloc("iot_sb", [128, 140], F32)
    eps_c = A.alloc("eps_c", [128, 1], F32)
    st6 = A.alloc("st6", [128, 12], F32)
    mv = A.alloc("mv", [128, 8], F32)
    PBASE = A.mark()

    S.dma("sp", lambda e: e.dma_start(out=c_f32, in_=consts), writes=["c_f32"])
    S.dma("pool", lambda e: e.dma_start(out=c_bf, in_=consts), writes=["c_bf"])
    S.dma("sp", lambda e: e.dma_start(out=iot_sb, in_=iot), writes=["iot"])
    S.op("dve", lambda e: e.memset(eps_c, LN_EPS), writes=["eps_c"])

    rr = {"ev": 0}

    def evac(out_ap, in_ap, reads, writes, scale=None):
        rr["ev"] += 1
        if rr["ev"] % 2 == 0:
            if scale is None:
                S.op("act", lambda e: e.activation(out=out_ap, in_=in_ap, func=AF.Copy), reads=reads, writes=writes)
            else:
                S.op("act", lambda e: e.activation(out=out_ap, in_=in_ap, func=AF.Copy, scale=scale), reads=reads, writes=writes)
        else:
            if scale is None:
                S.op("dve", lambda e: e.tensor_copy(out=out_ap, in_=in_ap), reads=reads, writes=writes)
            else:
                S.op("dve", lambda e: e.tensor_scalar(out=out_ap, in0=in_ap, scalar1=scale, scalar2=None, op0=ALU.mult),
                     reads=reads, writes=writes)

    Wkp = A.alloc("Wkp", [128, 8, 1024], BF16)
    Wqp = A.alloc("Wqp", [128, 8, 1024], BF16)
    Wv = A.alloc("Wv", [128, 8, 1024], BF16)
    Wf = A.alloc("Wf", [128, 8, 8], F32)
    bF = A.alloc("bF", [128, 512], F32)
    selx_b = A.alloc("selx_b", [128, 1024], F32)
    xTb = [A.alloc("xTb", [128, 8, 512], BF16) for _ in range(2)]
    xTf = [A.alloc("xTf", [128, 8, 512], F32) for _ in range(2)]
    KTst = [A.alloc("KTst", [128, 8, 512], BF16) for _ in range(2)]
    Vst = [A.alloc("Vst", [128, 8, 512], BF16) for _ in range(2)]

    for j in range(8):
        S.dma("pool", lambda e, j=j: e.dma_start(out=Wkp[:, j, :], in_=w_kp[:, j, :]), writes=["Wkp"])
        S.dma("pool", lambda e, j=j: e.dma_start(out=Wv[:, j, :], in_=w_v[:, j, :]), writes=["Wv"])
        S.dma("pool", lambda e, j=j: e.dma_start(out=Wqp[:, j, :], in_=w_qp[:, j, :]), writes=["Wqp"])
    mask_bf = nc.alloc_sbuf_tensor_at("mask_bf_fix", [128, 32, 512], BF16, offset=194560).ap()
    S.dma("sp", lambda e: e.dma_start(out=Wf, in_=w_f), writes=["Wf"])
    S.dma("sp", lambda e: e.dma_start(out=bF, in_=bf_t.partition_broadcast(128)), writes=["bF"])
    S.dma("sp", lambda e: e.dma_start(out=selx_b, in_=selx.partition_broadcast(128)), writes=["selx_b"])

    PF = PS[7]
    bank = {"i": 0}

    def nxt_bank(n=7):
        bank["i"] = (bank["i"] + 1) % n
        return bank["i"]

    for g in range(16):
        b2 = g % 2
        for c in range(8):
            S.dma("pool", lambda e, g=g, b2=b2, c=c: e.dma_start(out=xTb[b2][:, c, :], in_=xT_all[:, c, g * 512:(g + 1) * 512]),
                  writes=["xTb%d" % b2])
        S.dma("sp", lambda e, g=g, b2=b2: e.dma_start(out=xTf[b2], in_=xT_all[:, :, g * 512:(g + 1) * 512]),
              writes=["xTf%d" % b2])
        for hp in range(8):
            bi = nxt_bank()
            for c in range(8):
                S.op("pe", lambda e, bi=bi, hp=hp, c=c, b2=b2: e.matmul(PS[bi], lhsT=Wkp[:, c, hp * 128:(hp + 1) * 128],
                                                                       rhs=xTb[b2][:, c, :], start=(c == 0), stop=(c == 7)),
                     reads=["Wkp", "xTb%d" % b2], writes=["ps%d" % bi])
            evac(KTst[b2][:, hp, :], PS[bi], ["ps%d" % bi], ["KTst%d" % b2])
        S.dma("sp", lambda e, g=g, b2=b2: e.dma_start(out=kt_scr[:, :, g * 512:(g + 1) * 512].rearrange("h p t -> p h t"),
                                                      in_=KTst[b2]),
              reads=["KTst%d" % b2], writes=["kt_scr"])
        for j in range(4):
            for half in range(2):
                bi = nxt_bank()
                for c in range(8):
                    S.op("pe", lambda e, bi=bi, j=j, half=half, c=c, b2=b2: e.matmul(
                        PS[bi], lhsT=xTb[b2][:, c, j * 128:(j + 1) * 128], rhs=Wv[:, c, half * 512:(half + 1) * 512],
                        start=(c == 0), stop=(c == 7)), reads=["Wv", "xTb%d" % b2], writes=["ps%d" % bi])
                dst = Vst[b2].rearrange("p h (j t e) -> p h j t e", j=4, t=2, e=64)[:, :, j, half, :]
                evac(dst, PS[bi].rearrange("p (h e) -> p h e", h=8, e=64), ["ps%d" % bi], ["Vst%d" % b2])
        S.dma("sp", lambda e, g=g, b2=b2: e.dma_start(
            out=v_scr[:, :, g * 512:(g + 1) * 512].rearrange("h p t -> p h t"), in_=Vst[b2]),
            reads=["Vst%d" % b2], writes=["v_scr"])
        for j in range(4):
            kb = 4 * g + j
            for c in range(8):
                S.op("pe", lambda e, kb=kb, j=j, c=c, b2=b2: e.matmul(PF[:, kb * 8:(kb + 1) * 8], lhsT=xTf[b2][:, c, j * 128:(j + 1) * 128],
                                                                     rhs=Wf[:, c, :], start=(c == 0), stop=(c == 7)),
                     reads=["Wf", "xTf%d" % b2], writes=["PF"])
    for j in range(32):
        S.dma("pool", lambda e, j=j: e.dma_start(out=mask_bf[:, j, :], in_=masks[:, j, :]), writes=["mask_bf"])
    m1 = A.mark()
    lf = A.alloc("lf", [128, 512], F32)
    lfn = A.alloc("lfn", [128, 512], F32)
    Ta = A.alloc("Ta", [128, 512], F32)
    Tb = A.alloc("Tb", [128, 512], F32)
    T0 = A.alloc("T0", [128, 512], F32)
    prod = A.alloc("prod", [128, 128], F32)
    S.op("dve", lambda e: e.tensor_tensor(out=lf, in0=PF, in1=bF, op=ALU.add), reads=["PF", "bF"], writes=["lf"])
    S.op("act", lambda e: e.activation(out=lfn, in_=lf, func=AF.Exp, scale=-1.0), reads=["lf"], writes=["lfn"])
    S.op("act", lambda e: e.activation(out=lf, in_=lfn, func=AF.Ln, bias=1.0), reads=["lfn"], writes=["lf"])
    S.op("pe", lambda e: e.matmul(PS[5], lhsT=c_f32[:, TRII, :], rhs=lf, start=True, stop=True), reads=["c_f32", "lf"], writes=["ps5"])
    S.op("pe", lambda e: e.matmul(PS[6], lhsT=c_f32[:, ONES, :], rhs=lf, start=True, stop=True), reads=["c_f32", "lf"], writes=["ps6"])
    S.op("dve", lambda e: e.tensor_copy(out=T0, in_=PS[6]), reads=["ps6"], writes=["T0"])
    S.op("dve", lambda e: e.tensor_copy(out=Ta, in_=PS[6]), reads=["ps6"], writes=["Ta"])
    cur, oth, cn, on = Ta, Tb, "Ta", "Tb"
    for dsh in (1, 2, 4, 8, 16, 32):
        w_ = 8 * dsh
        S.op("dve", lambda e, cur=cur, oth=oth, w_=w_: e.tensor_tensor(out=oth[:, w_:], in0=cur[:, w_:], in1=cur[:, :512 - w_], op=ALU.add),
             reads=[cn], writes=[on])
        S.op("dve", lambda e, cur=cur, oth=oth, w_=w_: e.tensor_copy(out=oth[:, :w_], in_=cur[:, :w_]), reads=[cn, on], writes=[on])
        cur, oth, cn, on = oth, cur, on, cn
    Tincl, tin = cur, cn
    S.op("dve", lambda e: e.tensor_tensor(out=lfn, in0=Tincl, in1=T0, op=ALU.subtract), reads=[tin, "T0"], writes=["lfn"])
    S.op("dve", lambda e: e.tensor_tensor(out=cfpos, in0=PS[5], in1=lfn, op=ALU.add), reads=["ps5", "lfn"], writes=["cfpos"])
    Tlast = Tincl.rearrange("p (g j h) -> p g j h", g=16, j=4, h=8)[:, :, 3, :]
    for s in range(8):
        S.op("dve", lambda e, s=s: e.tensor_tensor(out=prod.rearrange("p (h g) -> p g h", h=8, g=16), in0=Tlast,
                                                  in1=selx_b[:, s * 128:(s + 1) * 128].rearrange("p (g h) -> p g h", g=16, h=8),
                                                  op=ALU.mult), reads=[tin, "selx_b"], writes=["prod"])
        S.op("dve", lambda e, s=s: e.tensor_reduce(out=cref[:, s * 8:(s + 1) * 8], in_=prod.rearrange("p (h g) -> p h g", h=8, g=16),
                                                  axis=AX.X, op=ALU.add), reads=["prod"], writes=["cref"])

    for s in range(8):
        b2 = s % 2
        S.dma("sp", lambda e, s=s, b2=b2: e.dma_start(out=xTf[b2], in_=xT_loc[:, :, s * 512:(s + 1) * 512]),
              writes=["xTf%d" % b2])
        S.op("act", lambda e, b2=b2: e.activation(out=xTb[b2][:, 0:4, :], in_=xTf[b2][:, 0:4, :], func=AF.Copy),
             reads=["xTf%d" % b2], writes=["xTb%d" % b2])
        S.op("dve", lambda e, b2=b2: e.tensor_copy(out=xTb[b2][:, 4:8, :], in_=xTf[b2][:, 4:8, :]),
             reads=["xTf%d" % b2, "xTb%d" % b2], writes=["xTb%d" % b2])
        for hp in range(8):
            bi = nxt_bank(5)
            for c in range(8):
                S.op("pe", lambda e, bi=bi, hp=hp, c=c, b2=b2: e.matmul(PS[bi], lhsT=Wqp[:, c, hp * 128:(hp + 1) * 128],
                                                                       rhs=xTb[b2][:, c, :], start=(c == 0), stop=(c == 7)),
                     reads=["Wqp", "xTb%d" % b2], writes=["ps%d" % bi])
            evac(KTst[b2][:, hp, :], PS[bi], ["ps%d" % bi], ["KTst%d" % b2], scale=0.125)
        S.dma("sp", lambda e, s=s, b2=b2: e.dma_start(out=qt_scr[:, :, s * 512:(s + 1) * 512].rearrange("h p t -> p h t"),
                                                      in_=KTst[b2]),
              reads=["KTst%d" % b2], writes=["qt_scr"])

    if stop < 2:
        S.emit()
        return nc
    S.barrier()
    A.reset(PBASE)
    mixT = A.alloc("mixT", [128, 8, NLOC], BF16)
    M3 = A.mark()
    KT = A.alloc("KT", [128, SEQ], BF16)
    Vt = A.alloc("Vt", [128, 64 * 192], BF16)
    QT = A.alloc("QT", [128, NLOC], BF16)
    Et2 = [A.alloc("Et2", [128, 1024], F32) for _ in range(2)]
    SP2 = [A.alloc("SP2", [128, 1024], BF16) for _ in range(2)]
    Wt2 = [A.alloc("Wt2", [128, 1024], BF16) for _ in range(2)]
    Ybf = [A.alloc("Ybf", [128, 512], BF16) for _ in range(2)]
    Pt = [A.alloc("Pt", [128, 512], BF16) for _ in range(2)]
    biasF = [A.alloc("biasF", [128, 64], F32) for _ in range(2)]
    rinv = A.alloc("rinv", [128, 512], F32)
    XSA = [PSALL[:, 0:1024], PSALL[:, 1024:2048]]
    XF1 = PS[4]
    YB, OB, OFB = PS[5], PS[6], PS[7]
    Vt4 = Vt.rearrange("p (k t e) -> p k t e", k=64, t=3, e=64)
    S.op("dve", lambda e: e.memset(Vt4[:, :, 2, :], 1.0), writes=["Vones"])
    cf3 = cfpos.rearrange("p (k h) -> p k h", k=64, h=8)

    tiles = []
    for hp in range(8):
        for s in range(8):
            nkb = 8 * s + 8
            for kb in range(nkb - 1, -1, -1):
                tiles.append(dict(hp=hp, s=s, kb=kb, first=(kb == nkb - 1), last=(kb == 0), j=kb - 8 * s,
                                  newhp=(s == 0 and kb == nkb - 1), news=(kb == nkb - 1)))
    NT = len(tiles)
    NP = NT // 2

    def st_load(t):
        hp = t["hp"]
        S.dma("sp", lambda e: e.dma_start(out=KT, in_=kt_scr[hp]), reads=["kt_scr"], writes=["KT"])
        S.dma("sp", lambda e: e.dma_start(out=QT, in_=qt_scr[hp]), reads=["qt_scr"], writes=["QT"])
        S.dma("sp", lambda e: e.dma_start(out=Vt4[:, :, 0:2, :], in_=v_scr[hp].rearrange("p (k t e) -> p k t e", k=64, t=2, e=64)),
              reads=["v_scr"], writes=["Vt"])

    def st_bias(t):
        hp, s = t["hp"], t["s"]
        nkb = 8 * s + 8
        bb = s % 2
        S.op("dve", lambda e: e.tensor_scalar(out=biasF[bb][:, 0:nkb], in0=cf3[:, 0:nkb, hp], scalar1=cref[:, s * 8 + hp:s * 8 + hp + 1],
                                              scalar2=None, op0=ALU.subtract), reads=["cfpos", "cref"], writes=["biasF%d" % bb])

    def s1p(n):
        for h in range(2):
            t = tiles[2 * n + h]
            hp, s, kb, j = t["hp"], t["s"], t["kb"], t["j"]
            if t["newhp"]:
                st_load(t)
            if t["news"]:
                st_bias(t)
            xs = XSA[n % 2][:, h * 512:(h + 1) * 512]
            key = "XS%d_%d" % (n % 2, h)
            S.op("pe", lambda e, xs=xs, kb=kb, s=s, j=j: e.matmul(xs, lhsT=KT[0:64, kb * 128:(kb + 1) * 128], rhs=QT[0:64, s * 512:(s + 1) * 512],
                                                                 start=True, stop=False), reads=["KT", "QT"], writes=[key])
            if j >= 0:
                mi = (0 * 2 + s % 2) * 8 + j
                S.op("pe", lambda e, xs=xs, mi=mi: e.matmul(xs, lhsT=c_bf[:, IDENT, :], rhs=mask_bf[:, mi, :], start=False, stop=False),
                     reads=["c_bf", "mask_bf"], writes=[key])

    def f1(n, h):
        t = tiles[2 * n + h]
        hp, s, kb, j = t["hp"], t["s"], t["kb"], t["j"]
        S.op("pe", lambda e: e.matmul(XF1, lhsT=KT[64:128, kb * 128:(kb + 1) * 128], rhs=QT[64:128, s * 512:(s + 1) * 512],
                                      start=True, stop=(j < 0)), reads=["KT", "QT"], writes=["XF"])
        if j >= 0:
            mi = (1 * 2 + s % 2) * 8 + j
            S.op("pe", lambda e: e.matmul(XF1, lhsT=c_bf[:, IDENT, :], rhs=mask_bf[:, mi, :], start=False, stop=True),
                 reads=["c_bf", "mask_bf"], writes=["XF"])

    def s2p(n):
        q = n % 2
        S.op("act", lambda e: e.activation(out=Et2[q], in_=XSA[q], func=AF.Exp), reads=["XS%d_0" % q, "XS%d_1" % q], writes=["Et2_%d" % q])

    def s3p(n):
        q = n % 2
        S.op("act", lambda e: e.activation(out=SP2[q], in_=Et2[q], func=AF.Ln, bias=1.0), reads=["Et2_%d" % q], writes=["SP2_%d" % q])

    def s4p(n):
        ta, tb = tiles[2 * n], tiles[2 * n + 1]
        q = n % 2
        xa = XSA[q][:, 0:512]; xb_ = XSA[q][:, 512:1024]
        spa = SP2[q][:, 0:512]; spb = SP2[q][:, 512:1024]
        ka, kb_ = "XS%d_0" % q, "XS%d_1" % q
        spk = "SP2_%d" % q
        yprev = Ybf[1 - q]; ypk = "Ybf%d" % (1 - q)
        S.op("pe", lambda e: e.matmul(xa, lhsT=c_bf[:, NEGTRI, :], rhs=spa, start=False, stop=ta["first"]), reads=["c_bf", spk], writes=[ka])
        if not ta["first"]:
            S.op("pe", lambda e: e.matmul(xa, lhsT=c_bf[:, NEGID, :], rhs=yprev, start=False, stop=True), reads=["c_bf", ypk], writes=[ka])
        S.op("pe", lambda e: e.matmul(xb_, lhsT=c_bf[:, NEGTRI, :], rhs=spb, start=False, stop=False), reads=["c_bf", spk], writes=[kb_])
        if not ta["first"]:
            S.op("pe", lambda e: e.matmul(xb_, lhsT=c_bf[:, NEGID, :], rhs=yprev, start=False, stop=False), reads=["c_bf", ypk], writes=[kb_])
        S.op("pe", lambda e: e.matmul(xb_, lhsT=c_bf[:, NEGONE, :], rhs=spa, start=False, stop=True), reads=["c_bf", spk], writes=[kb_])
        S.op("pe", lambda e: e.matmul(YB, lhsT=c_bf[:, ONES, :], rhs=spa, start=ta["first"], stop=False), reads=["c_bf", spk], writes=["Y"])
        S.op("pe", lambda e: e.matmul(YB, lhsT=c_bf[:, ONES, :], rhs=spb, start=False, stop=tb["last"]), reads=["c_bf", spk], writes=["Y"])
        if not tb["last"]:
            S.op("dve", lambda e: e.tensor_copy(out=Ybf[q], in_=YB), reads=["Y"], writes=["Ybf%d" % q])

    def s6p(n):
        q = n % 2
        S.op("act", lambda e: e.activation(out=Wt2[q], in_=XSA[q], func=AF.Exp), reads=["XS%d_0" % q, "XS%d_1" % q], writes=["Wt2_%d" % q])

    def s7(n, h):
        t = tiles[2 * n + h]
        q = n % 2
        hp, s, kb = t["hp"], t["s"], t["kb"]
        S.op("pe", lambda e: e.matmul(OB[0:64, :], lhsT=Vt4[:, kb, 0, :], rhs=Wt2[q][:, h * 512:(h + 1) * 512], start=t["first"], stop=t["last"]),
             reads=["Vt", "Wt2_%d" % q], writes=["O"])
        if t["last"]:
            po = (hp % 2) * 64
            S.op("dve", lambda e: e.tensor_copy(out=mixT[po:po + 64, hp // 2, s * 512:(s + 1) * 512], in_=OB[0:64, :]),
                 reads=["O"], writes=["mixT"])

    def f2(n, h):
        t = tiles[2 * n + h]
        bb = t["s"] % 2; kb = t["kb"]
        S.op("act", lambda e: e.activation(out=Pt[h], in_=XF1, func=AF.Exp, bias=biasF[bb][:, kb:kb + 1]),
             reads=["XF", "biasF%d" % bb], writes=["Pt%d" % h])

    def f3(n, h):
        t = tiles[2 * n + h]
        hp, s, kb = t["hp"], t["s"], t["kb"]
        S.op("pe", lambda e: e.matmul(OFB, lhsT=Vt4[:, kb, 1:3, :].rearrange("p a b -> p (a b)"), rhs=Pt[h], start=t["first"], stop=t["last"]),
             reads=["Vt", "Vones", "Pt%d" % h], writes=["OF"])
        if t["last"]:
            po = (hp % 2) * 64
            S.op("dve", lambda e: e.reciprocal(out=rinv[0:64, :], in_=OFB[64:128, :]), reads=["OF"], writes=["rinv"])
            S.op("dve", lambda e: e.tensor_tensor(out=mixT[po:po + 64, 4 + hp // 2, s * 512:(s + 1) * 512], in0=OFB[0:64, :],
                                                  in1=rinv[0:64, :], op=ALU.mult), reads=["OF", "rinv"], writes=["mixT"])

    s1p(0)
    f1(0, 0)
    flushed = True
    for n in range(NP):
        boundary = (n + 1 < NP) and tiles[2 * (n + 1)]["newhp"]
        s2p(n)
        if not flushed:
            s6p(n - 1)
            s7(n - 1, 0)
            s7(n - 1, 1)
        flushed = False
        if n + 1 < NP and not boundary:
            s1p(n + 1)
        f2(n, 0)
        f1(n, 1)
        s3p(n)
        s4p(n)
        f3(n, 0)
        f2(n, 1)
        f3(n, 1)
        if n + 1 < NP and not boundary:
            f1(n + 1, 0)
        if boundary or n + 1 == NP:
            s6p(n)
            s7(n, 0)
            s7(n, 1)
            flushed = True
            if boundary:
                s1p(n + 1)
                f1(n + 1, 0)

    if stop < 3:
        S.emit()
        return nc
    S.barrier()
    A.reset(M3)
    Wout = A.alloc("Wout", [128, 8, 1024], BF16)
    Wpg = A.alloc("Wpg", [128, 8, 1024], BF16)
    Wple = A.alloc("Wple", [128, 2, 1024], BF16)
    Wr = A.alloc("Wr", [128, 8, 36], F32)
    g1b = A.alloc("g1b", [128, D], F32); b1b = A.alloc("b1b", [128, D], F32)
    b36 = A.alloc("b36", [128, 32 * 36], F32)
    xt = [A.alloc("xt", [128, D], F32) for _ in range(2)]
    pTb = [A.alloc("pTb", [128, 2, 128], BF16) for _ in range(2)]
    rt = A.alloc("rt", [128, D], F32)
    ht = [A.alloc("ht", [128, D], F32) for _ in range(2)]
    hhi = A.alloc("hhi", [128, D], BF16)
    hlo = A.alloc("hlo", [128, D], BF16)
    hTlo = A.alloc("hTlo", [128, 8, 128], BF16)
    Wrh = A.alloc("Wrh", [128, 8, 36], BF16)
    Wrl = A.alloc("Wrl", [128, 8, 36], BF16)
    hTb = A.alloc("hTb", [128, 8, 128], BF16)
    sg = A.alloc("sg", [128, D], F32)
    pre = [A.alloc("pre", [128, D], F32) for _ in range(2)]
    for j in range(8):
        S.dma("pool", lambda e, j=j: e.dma_start(out=Wout[:, j, :], in_=w_out[:, j, :]), writes=["Wout"])
        S.dma("pool", lambda e, j=j: e.dma_start(out=Wpg[:, j, :], in_=w_pg[:, j, :]), writes=["Wpg"])
    for j in range(2):
        S.dma("pool", lambda e, j=j: e.dma_start(out=Wple[:, j, :], in_=w_ple[:, j, :]), writes=["Wple"])
    S.dma("sp", lambda e: e.dma_start(out=Wr, in_=w_r36), writes=["Wr"])
    S.dma("sp", lambda e: e.dma_start(out=g1b, in_=ln1g.partition_broadcast(128)), writes=["g1b"])
    S.dma("sp", lambda e: e.dma_start(out=b1b, in_=ln1b.partition_broadcast(128)), writes=["b1b"])
    S.dma("sp", lambda e: e.dma_start(out=b36, in_=b_r36.partition_broadcast(128)), writes=["b36"])

    def layer_norm(src, dst, gb, bb, rk, wk, gk):
        for hf in range(2):
            S.op("dve", lambda e, hf=hf: e.bn_stats(out=st6[:, hf * 6:(hf + 1) * 6], in_=src[:, hf * 512:(hf + 1) * 512]),
                 reads=[rk], writes=["st6_%d" % hf])
        S.op("dve", lambda e: e.bn_aggr(out=mv[:, 0:2], in_=st6), reads=["st6_0", "st6_1"], writes=["mv"])
        S.op("act", lambda e: e.activation(out=mv[:, 2:3], in_=mv[:, 1:2], func=AF.Ln, bias=eps_c), reads=["mv", "eps_c"], writes=["mv2"])
        S.op("act", lambda e: e.activation(out=mv[:, 3:4], in_=mv[:, 2:3], func=AF.Exp, scale=-0.5), reads=["mv2"], writes=["mv3"])
        S.op("dve", lambda e: e.tensor_scalar(out=dst, in0=src, scalar1=mv[:, 0:1], scalar2=mv[:, 3:4], op0=ALU.subtract, op1=ALU.mult),
             reads=[rk, "mv", "mv3"], writes=[wk])
        S.op("dve", lambda e: e.tensor_tensor(out=dst, in0=dst, in1=gb, op=ALU.mult), reads=[wk] + gk, writes=[wk])
        S.op("dve", lambda e: e.tensor_tensor(out=dst, in0=dst, in1=bb, op=ALU.add), reads=[wk] + gk, writes=[wk])

    PA = [PS[0], PS[1]]; PTr = [PS[2], PS[3]]; PG = [PS[4], PS[5]]; PR = PS[6]
    PThi = PS[2].bitcast(BF16); PTlo = PS[3].bitcast(BF16)
    S.op("dve", lambda e: e.tensor_copy(out=Wrh, in_=Wr), reads=["Wr"], writes=["Wrh"])
    S.op("dve", lambda e: e.tensor_tensor(out=Wrl, in0=Wr, in1=Wrh, op=ALU.subtract), reads=["Wr", "Wrh"], writes=["Wrl"])
    hTb2 = [hTb, A.alloc("hTb_b", [128, 8, 128], BF16)]
    hTlo2 = [hTlo, A.alloc("hTlo_b", [128, 8, 128], BF16)]
    ple_sb = [A.alloc("ple_sb", [128, D], F32) for _ in range(2)]
    PP = PS[7]

    def p1(tt):
        b2 = tt % 2
        tsl = slice(tt * 128, (tt + 1) * 128)
        S.dma("sp", lambda e: e.dma_start(out=xt[b2], in_=x_loc[tsl, :]), writes=["xt%d" % b2])
        S.dma("pool", lambda e: e.dma_start(out=pTb[b2], in_=pT_loc[:, :, tsl]), writes=["pTb%d" % b2])
        for hf in range(2):
            for c in range(8):
                S.op("pe", lambda e, hf=hf, c=c: e.matmul(PA[hf], lhsT=mixT[:, c, tsl], rhs=Wout[:, c, hf * 512:(hf + 1) * 512],
                                                         start=(c == 0), stop=(c == 7)), reads=["mixT", "Wout"], writes=["PA%d" % hf])
            S.op("dve", lambda e, hf=hf: e.scalar_tensor_tensor(out=rt[:, hf * 512:(hf + 1) * 512], in0=xt[b2][:, hf * 512:(hf + 1) * 512],
                                                               scalar=ALPHA, in1=PA[hf], op0=ALU.mult, op1=ALU.add),
                 reads=["xt%d" % b2, "PA%d" % hf], writes=["rt"])
        for hf in range(2):
            for c in range(2):
                S.op("pe", lambda e, hf=hf, c=c: e.matmul(PP, lhsT=pTb[b2][:, c, :], rhs=Wple[:, c, hf * 512:(hf + 1) * 512],
                                                         start=(c == 0), stop=(c == 1)), reads=["pTb%d" % b2, "Wple"], writes=["PP"])
            S.op("act", lambda e, hf=hf: e.activation(out=ple_sb[b2][:, hf * 512:(hf + 1) * 512], in_=PP, func=AF.Copy),
                 reads=["PP"], writes=["ple%d_%d" % (b2, hf)])

    def ln_a(src, rk):
        for hf in range(2):
            S.op("dve", lambda e, hf=hf: e.bn_stats(out=st6[:, hf * 6:(hf + 1) * 6], in_=src[:, hf * 512:(hf + 1) * 512]),
                 reads=[rk], writes=["st6_%d" % hf])
        S.op("dve", lambda e: e.bn_aggr(out=mv[:, 0:2], in_=st6), reads=["st6_0", "st6_1"], writes=["mv"])
        S.op("act", lambda e: e.activation(out=mv[:, 2:3], in_=mv[:, 1:2], func=AF.Ln, bias=eps_c), reads=["mv", "eps_c"], writes=["mv2"])
        S.op("act", lambda e: e.activation(out=mv[:, 3:4], in_=mv[:, 2:3], func=AF.Exp, scale=-0.5), reads=["mv2"], writes=["mv3"])

    def ln_b(src, dst, gb, bb, rk, wk, gk):
        S.op("dve", lambda e: e.tensor_scalar(out=mv[:, 4:5], in0=mv[:, 0:1], scalar1=mv[:, 3:4], scalar2=-1.0, op0=ALU.mult, op1=ALU.mult),
             reads=["mv", "mv3"], writes=["mv4"])
        S.op("act", lambda e: e.activation(out=dst, in_=src, func=AF.Identity, scale=mv[:, 3:4], bias=mv[:, 4:5]),
             reads=[rk, "mv3", "mv4"], writes=[wk])
        S.op("pool", lambda e: e.tensor_tensor(out=dst, in0=dst, in1=gb, op=ALU.mult), reads=[wk] + gk, writes=[wk])
        S.op("dve", lambda e: e.tensor_tensor(out=dst, in0=dst, in1=bb, op=ALU.add), reads=[wk] + gk, writes=[wk])

    def p2a(tt):
        ln_a(rt, "rt")

    def p2(tt):
        b2 = tt % 2
        tsl = slice(tt * 128, (tt + 1) * 128)
        ln_b(rt, ht[b2], g1b, b1b, "rt", "ht%d" % b2, ["g1b", "b1b"])
        S.dma("sp", lambda e: e.dma_start(out=h_scr[tsl, :], in_=ht[b2]), reads=["ht%d" % b2], writes=["h_scr"])
        S.op("dve", lambda e: e.tensor_copy(out=hhi, in_=ht[b2]), reads=["ht%d" % b2], writes=["hhi"])
        S.op("dve", lambda e: e.tensor_tensor(out=hlo, in0=ht[b2], in1=hhi, op=ALU.subtract), reads=["ht%d" % b2, "hhi"], writes=["hlo"])

    def p3(tt):
        b2 = tt % 2
        for c in range(8):
            S.op("pe", lambda e, c=c: e.transpose(out=PThi[:, c * 128:(c + 1) * 128], in_=hhi[:, c * 128:(c + 1) * 128],
                                                 identity=c_bf[:, IDENT, :]), reads=["hhi", "c_bf"], writes=["PTr0"])
        for c in range(8):
            S.op("pe", lambda e, c=c: e.transpose(out=PTlo[:, c * 128:(c + 1) * 128], in_=hlo[:, c * 128:(c + 1) * 128],
                                                 identity=c_bf[:, IDENT, :]), reads=["hlo", "c_bf"], writes=["PTr1"])
        S.op("act", lambda e: e.activation(out=hTb2[b2].rearrange("p a b -> p (a b)"), in_=PThi, func=AF.Copy), reads=["PTr0"], writes=["hTb%d" % b2])
        S.op("dve", lambda e: e.tensor_copy(out=hTlo2[b2].rearrange("p a b -> p (a b)"), in_=PTlo), reads=["PTr1"], writes=["hTlo%d" % b2])

    def q_pe(tt):
        b2 = tt % 2
        k3 = 0
        for c in range(8):
            for (lt, ln_, rt_, rn_) in ((hTb2[b2], "hTb%d" % b2, Wrh, "Wrh"), (hTb2[b2], "hTb%d" % b2, Wrl, "Wrl"), (hTlo2[b2], "hTlo%d" % b2, Wrh, "Wrh")):
                S.op("pe", lambda e, c=c, lt=lt, rt_=rt_, k3=k3: e.matmul(PR[:, 0:36], lhsT=lt[:, c, :], rhs=rt_[:, c, :], start=(k3 == 0), stop=(k3 == 23)),
                     reads=[ln_, rn_], writes=["PR"])
                k3 += 1
        for hf in range(2):
            for c in range(8):
                S.op("pe", lambda e, hf=hf, c=c: e.matmul(PG[hf], lhsT=hTb2[b2][:, c, :], rhs=Wpg[:, c, hf * 512:(hf + 1) * 512],
                                                         start=(c == 0), stop=(c == 7)), reads=["hTb%d" % b2, "Wpg"], writes=["PG%d" % hf])
            S.op("act", lambda e, hf=hf: e.activation(out=sg[:, hf * 512:(hf + 1) * 512], in_=PG[hf], func=AF.Sigmoid),
                 reads=["PG%d" % hf], writes=["sg%d" % hf])

    def q_dve(tt):
        b2 = tt % 2
        tsl = slice(tt * 128, (tt + 1) * 128)
        S.op("dve", lambda e: e.tensor_tensor(out=L_all[:, tt * 36:(tt + 1) * 36], in0=PR[:, 0:36], in1=b36[:, tt * 36:(tt + 1) * 36], op=ALU.add),
             reads=["PR", "b36"], writes=["L_all"])
        for hf in range(2):
            S.op("dve", lambda e, hf=hf: e.tensor_tensor(out=sg[:, hf * 512:(hf + 1) * 512], in0=sg[:, hf * 512:(hf + 1) * 512],
                                                        in1=ple_sb[b2][:, hf * 512:(hf + 1) * 512], op=ALU.mult),
                 reads=["sg%d" % hf, "ple%d_%d" % (b2, hf)], writes=["sg%d" % hf])
        S.op("dve", lambda e: e.scalar_tensor_tensor(out=pre[b2], in0=ht[b2], scalar=ALPHA, in1=sg, op0=ALU.mult, op1=ALU.add),
             reads=["ht%d" % b2, "sg0", "sg1"], writes=["pre%d" % b2])
        S.dma("sp", lambda e: e.dma_start(out=pre_scr[tsl, :], in_=pre[b2]), reads=["pre%d" % b2], writes=["pre_scr"])

    p1(0)
    p2a(0)
    p2(0)
    p3(0)
    for tt in range(NTT):
        if tt + 1 < NTT:
            p1(tt + 1)
        q_pe(tt)
        if tt + 1 < NTT:
            p2a(tt + 1)
        q_dve(tt)
        if tt + 1 < NTT:
            p2(tt + 1)
            p3(tt + 1)

    if stop < 4:
        S.emit()
        return nc
    S.barrier()
    A.reset(PBASE)
    L3 = L_all.rearrange("p (t c) -> p t c", t=32, c=36)
    Lg = L3[:, :, 0:4]
    Le = L3[:, :, 4:36]
    gmax = A.alloc("gmax", [128, 32], F32)
    gm4 = A.alloc("gm4", [128, 32, 4], F32)
    eg = A.alloc("eg", [128, 32, 4], F32)
    gsum = A.alloc("gsum", [128, 32], F32)
    gp = A.alloc("gp", [128, 32], F32)
    Lm = A.alloc("Lm", [128, 32, 32], F32)
    top8 = A.alloc("top8", [128, 32, 8], F32)
    Oh0 = A.alloc("Oh0", [128, 32, 32], F32)
    Oh1 = A.alloc("Oh1", [128, 32, 32], F32)
    Oh2b = A.alloc("Oh2b", [128, 32 * 32], BF16)
    dv = A.alloc("dv", [128, 32], F32)
    ev = A.alloc("ev", [128, 32], F32)
    Ra = A.alloc("Ra", [128, 1024], F32)
    Rb = A.alloc("Rb", [128, 1024], F32)
    R0 = A.alloc("R0", [128, 1024], F32)
    cnt = A.alloc("cnt", [128, 32], F32)
    nb = A.alloc("nb", [128, 32], F32)
    pa = A.alloc("pa", [128, 32], F32)
    pb = A.alloc("pb", [128, 32], F32)
    pstart = A.alloc("pstart", [128, 32], F32)
    dfl = A.alloc("dfl", [128, 64], F32)
    cmp3 = A.alloc("cmp3", [128, NBLK, 32], F32)
    bexp = A.alloc("bexp", [128, NBLK], F32)
    idxf = A.alloc("idxf", [128, NBLK * 8], F32)

    def dv_(fn, reads, writes):
        S.op("dve", fn, reads=reads, writes=writes)

    dv_(lambda e: e.tensor_reduce(out=gmax, in_=Lg, axis=AX.X, op=ALU.max), ["L_all"], ["gmax"])
    gmax_b = gmax.unsqueeze(2).to_broadcast([128, 32, 4])
    dv_(lambda e: e.tensor_tensor(out=gm4, in0=Lg, in1=gmax_b, op=ALU.is_ge), ["L_all", "gmax"], ["gm4"])
    dv_(lambda e: e.tensor_tensor(out=eg, in0=Lg, in1=gmax_b, op=ALU.subtract), ["L_all", "gmax"], ["eg"])
    S.op("act", lambda e: e.activation(out=eg, in_=eg, func=AF.Exp), reads=["eg"], writes=["eg"])
    dv_(lambda e: e.tensor_reduce(out=gsum, in_=eg, axis=AX.X, op=ALU.add), ["eg"], ["gsum"])
    dv_(lambda e: e.reciprocal(out=gp, in_=gsum), ["gsum"], ["gp"])
    dv_(lambda e: e.tensor_scalar(out=gm4, in0=gm4, scalar1=1.0, scalar2=1e30, op0=ALU.subtract, op1=ALU.mult), ["gm4"], ["gm4"])
    dv_(lambda e: e.tensor_tensor(out=Lm.rearrange("p t (g k) -> p t g k", g=4, k=8), in0=Le.rearrange("p t (g k) -> p t g k", g=4, k=8),
                                  in1=gm4.unsqueeze(3).to_broadcast([128, 32, 4, 8]), op=ALU.add), ["L_all", "gm4"], ["Lm"])
    for t in range(32):
        dv_(lambda e, t=t: e.max(out=top8[:, t, :], in_=Lm[:, t, :]), ["Lm"], ["top8"])
    v0 = top8[:, :, 0]
    v1 = top8[:, :, 1]
    dv_(lambda e: e.tensor_tensor(out=Oh0, in0=Lm, in1=top8[:, :, 0:1].to_broadcast([128, 32, 32]), op=ALU.is_equal), ["Lm", "top8"], ["Oh0"])
    dv_(lambda e: e.tensor_tensor(out=Oh1, in0=Lm, in1=top8[:, :, 1:2].to_broadcast([128, 32, 32]), op=ALU.is_equal), ["Lm", "top8"], ["Oh1"])
    dv_(lambda e: e.tensor_tensor(out=dv, in0=v1, in1=v0, op=ALU.subtract), ["top8"], ["dv"])
    S.op("act", lambda e: e.activation(out=ev, in_=dv, func=AF.Exp), reads=["dv"], writes=["ev"])
    dv_(lambda e: e.tensor_scalar(out=dv, in0=ev, scalar1=1.0, scalar2=None, op0=ALU.add), ["ev"], ["dv"])
    dv_(lambda e: e.reciprocal(out=gsum, in_=dv), ["dv"], ["gsum"])
    dv_(lambda e: e.tensor_tensor(out=g0_all, in0=gsum, in1=gp, op=ALU.mult), ["gsum", "gp"], ["g0_all"])
    dv_(lambda e: e.tensor_tensor(out=g1_all, in0=g0_all, in1=ev, op=ALU.mult), ["g0_all", "ev"], ["g1_all"])
    dv_(lambda e: e.tensor_tensor(out=Oh2b.rearrange("p (t c) -> p t c", t=32, c=32), in0=Oh0, in1=Oh1, op=ALU.add), ["Oh0", "Oh1"], ["Oh2b"])
    for hf in range(2):
        S.op("pe", lambda e, hf=hf: e.matmul(PS[hf], lhsT=c_bf[:, STRICT, :], rhs=Oh2b[:, hf * 512:(hf + 1) * 512], start=True, stop=True),
             reads=["c_bf", "Oh2b"], writes=["ps%d" % hf])
        S.op("pe", lambda e, hf=hf: e.matmul(PS[2 + hf], lhsT=c_bf[:, ONES, :], rhs=Oh2b[:, hf * 512:(hf + 1) * 512], start=True, stop=True),
             reads=["c_bf", "Oh2b"], writes=["ps%d" % (2 + hf)])
        dv_(lambda e, hf=hf: e.tensor_copy(out=R0[:, hf * 512:(hf + 1) * 512], in_=PS[2 + hf]), ["ps%d" % (2 + hf)], ["R0"])
        dv_(lambda e, hf=hf: e.tensor_copy(out=Ra[:, hf * 512:(hf + 1) * 512], in_=PS[2 + hf]), ["ps%d" % (2 + hf)], ["Ra"])
    cur, oth, cn, on = Ra, Rb, "Ra", "Rb"
    for dsh in (1, 2, 4, 8, 16):
        w_ = 32 * dsh
        dv_(lambda e, cur=cur, oth=oth, w_=w_: e.tensor_tensor(out=oth[:, w_:], in0=cur[:, w_:], in1=cur[:, :1024 - w_], op=ALU.add), [cn], [on])
        dv_(lambda e, cur=cur, oth=oth, w_=w_: e.tensor_copy(out=oth[:, :w_], in_=cur[:, :w_]), [cn, on], [on])
        cur, oth, cn, on = oth, cur, on, cn
    Rincl, rin = cur, cn
    Rk, rkn = oth, on
    dv_(lambda e: e.tensor_copy(out=cnt, in_=Rincl[:, 31 * 32:32 * 32]), [rin], ["cnt"])
    dv_(lambda e: e.tensor_tensor(out=Rk, in0=Rincl, in1=R0, op=ALU.subtract), [rin, "R0"], [rkn])
    for hf in range(2):
        dv_(lambda e, hf=hf: e.tensor_tensor(out=Rk[:, hf * 512:(hf + 1) * 512], in0=Rk[:, hf * 512:(hf + 1) * 512], in1=PS[hf], op=ALU.add),
            [rkn, "ps%d" % hf], [rkn])
    dv_(lambda e: e.memset(nb, 0.0), [], ["nb"])
    for j in range(32):
        dv_(lambda e, j=j: e.scalar_tensor_tensor(out=nb, in0=cnt, scalar=float(128 * j), in1=nb, op0=ALU.is_gt, op1=ALU.add), ["cnt", "nb"], ["nb"])
    dv_(lambda e: e.tensor_scalar(out=nb, in0=nb, scalar1=128.0, scalar2=None, op0=ALU.mult), ["nb"], ["nb"])
    dv_(lambda e: e.tensor_copy(out=pa, in_=nb), ["nb"], ["pa"])
    cur, oth, cn, on = pa, pb, "pa", "pb"
    for dsh in (1, 2, 4, 8, 16):
        dv_(lambda e, cur=cur, oth=oth, dsh=dsh: e.tensor_tensor(out=oth[:, dsh:], in0=cur[:, dsh:], in1=cur[:, :32 - dsh], op=ALU.add), [cn], [on])
        dv_(lambda e, cur=cur, oth=oth, dsh=dsh: e.tensor_copy(out=oth[:, :dsh], in_=cur[:, :dsh]), [cn, on], [on])
        cur, oth, cn, on = oth, cur, on, cn
    pend, pen_n = cur, cn
    dv_(lambda e: e.tensor_tensor(out=pstart, in0=pend, in1=nb, op=ALU.subtract), [pen_n, "nb"], ["pstart"])
    Rk3 = Rk.rearrange("p (t c) -> p t c", t=32, c=32)
    dv_(lambda e: e.tensor_tensor(out=Rk3, in0=Rk3, in1=pstart.unsqueeze(1).to_broadcast([128, 32, 32]), op=ALU.add), [rkn, "pstart"], [rkn])
    dv_(lambda e: e.tensor_tensor(out=Oh0, in0=Oh0, in1=Rk3, op=ALU.mult), ["Oh0", rkn], ["Oh0"])
    dv_(lambda e: e.tensor_tensor(out=Oh1, in0=Oh1, in1=Rk3, op=ALU.mult), ["Oh1", rkn], ["Oh1"])
    dv_(lambda e: e.tensor_reduce(out=dfl[:, 0:32], in_=Oh0, axis=AX.X, op=ALU.add), ["Oh0"], ["dfl0"])
    dv_(lambda e: e.tensor_reduce(out=dfl[:, 32:64], in_=Oh1, axis=AX.X, op=ALU.add), ["Oh1"], ["dfl1"])
    dv_(lambda e: e.tensor_copy(out=dest0_i, in_=dfl[:, 0:32]), ["dfl0"], ["dest0_i"])
    dv_(lambda e: e.tensor_copy(out=dest1_i, in_=dfl[:, 32:64]), ["dfl1"], ["dest1_i"])
    thr = iot_sb[:, 44:140]
    dv_(lambda e: e.tensor_tensor(out=cmp3, in0=pend.unsqueeze(1).to_broadcast([128, NBLK, 32]), in1=thr.unsqueeze(2).to_broadcast([128, NBLK, 32]),
                                  op=ALU.is_le), [pen_n, "iot"], ["cmp3"])
    dv_(lambda e: e.tensor_reduce(out=bexp, in_=cmp3, axis=AX.X, op=ALU.add), ["cmp3"], ["bexp"])
    dv_(lambda e: e.tensor_scalar(out=bexp, in0=bexp, scalar1=31.0, scalar2=None, op0=ALU.min), ["bexp"], ["bexp"])
    chg = idxf[:, 0:NBLK]
    e2 = idxf[:, NBLK:2 * NBLK]
    dv_(lambda e: e.memset(chg[:, 0:1], 1.0), [], ["chg0"])
    dv_(lambda e: e.tensor_tensor(out=chg[:, 1:NBLK], in0=bexp[:, 1:NBLK], in1=bexp[:, 0:NBLK - 1], op=ALU.not_equal), ["bexp"], ["chg1"])
    dv_(lambda e: e.tensor_scalar(out=chg, in0=chg, scalar1=-1.0e7, scalar2=1.0e7, op0=ALU.mult, op1=ALU.add), ["chg0", "chg1"], ["chg"])
    dv_(lambda e: e.tensor_scalar(out=e2, in0=bexp, scalar1=128.0, scalar2=None, op0=ALU.mult), ["bexp"], ["e2"])
    dv_(lambda e: e.tensor_tensor(out=e2, in0=e2, in1=chg, op=ALU.add), ["e2", "chg"], ["e2"])
    dv_(lambda e: e.scalar_tensor_tensor(out=e2, in0=iot_sb[:, 0:1].to_broadcast([128, NBLK]), scalar=1.0, in1=e2, op0=ALU.mult, op1=ALU.add),
        ["e2", "iot"], ["e2"])
    dv_(lambda e: e.tensor_copy(out=idxA, in_=e2), ["e2"], ["idxA"])
    dv_(lambda e: e.tensor_scalar(out=e2, in0=e2, scalar1=1.0, scalar2=None, op0=ALU.add), ["e2", "idxA"], ["e2"])
    dv_(lambda e: e.tensor_copy(out=idxB, in_=e2), ["e2"], ["idxB"])

    if stop < 5:
        S.emit()
        return nc
    S.barrier()
    A.reset(PBASE)
    hrow = [A.alloc("hrow", [128, D], F32) for _ in range(2)]
    for tt in range(NTT):
        b2 = tt % 2
        tsl = slice(tt * 128, (tt + 1) * 128)
        S.dma("sp", lambda e, b2=b2, tsl=tsl: e.dma_start(out=hrow[b2], in_=h_scr[tsl, :]), reads=["h_scr"], writes=["hrow%d" % b2])
        for k, di in enumerate((dest0_i, dest1_i)):
            S.dma("pool", lambda e, b2=b2, tt=tt, di=di: e.indirect_dma_start(
                out=xs_scr, out_offset=bass.IndirectOffsetOnAxis(ap=di[:, tt:tt + 1], axis=0), in_=hrow[b2], in_offset=None),
                reads=["hrow%d" % b2, "dest0_i", "dest1_i"], writes=["xs_scr%d" % k])

    if stop < 6:
        S.emit()
        return nc
    S.barrier()
    xb = [A.alloc("xb", [128, D], BF16) for _ in range(2)]
    xTk = [A.alloc("xTk", [128, 8, 128], BF16) for _ in range(2)]
    Wg = A.alloc("Wg", [128, 8, 512], BF16)
    Wu = A.alloc("Wu", [128, 8, 512], BF16)
    Wd = A.alloc("Wd", [128, 4, 1024], BF16)
    Wgs = A.alloc("Wgs", [128, 4096], F32)
    Wus = A.alloc("Wus", [128, 4096], F32)
    Wds = A.alloc("Wds", [128, 4096], F32)
    sil = A.alloc("sil", [128, 512], F32)
    hdn = A.alloc("hdn", [128, 512], BF16)
    hdT = A.alloc("hdT", [128, 4, 128], BF16)
    yb = [A.alloc("yb", [128, D], F32) for _ in range(2)]
    breg = {}

    def bound_reg(e):
        if "r" not in breg:
            r = e.alloc_register("wbound")
            e.reg_mov(r, 32 * 128 - 1)
            breg["r"] = r
        return breg["r"]

    PTb = PS[0].bitcast(BF16)
    PGa, PUa = PS[1], PS[2]
    PTh = PS[3].bitcast(BF16)
    PY = [PS[4], PS[5]]
    def wload(b, which):
        for (wt, ws, wn, src) in which:
            wflat = wt.rearrange("p a b -> p (a b)")
            S.dma("pool", lambda e, ws=ws, src=src: e.indirect_dma_start(
                out=ws, out_offset=None, in_=src,
                in_offset=bass.IndirectOffsetOnAxis(ap=idxA[:, b:b + 1], axis=0), bounds_check=bound_reg(e), oob_is_err=False),
                reads=["idxA"], writes=[wn + "s"])
            S.op("act", lambda e, wflat=wflat, ws=ws: e.activation(out=wflat[:, 0:2048], in_=ws[:, 0:2048], func=AF.Copy),
                 reads=[wn + "s"], writes=[wn + "a"])
            S.op("dve", lambda e, wflat=wflat, ws=ws: e.tensor_copy(out=wflat[:, 2048:4096], in_=ws[:, 2048:4096]),
                 reads=[wn + "s"], writes=[wn + "b"])

    WG = ((Wg, Wgs, "Wg", w_gate),)
    WU = ((Wu, Wus, "Wu", w_up),)
    WD = ((Wd, Wds, "Wd", w_down),)

    def stA(b):
        b2 = b % 2
        rsl = slice(b * 128, (b + 1) * 128)
        S.dma("sp", lambda e: e.dma_start(out=xb[b2], in_=xs_scr[rsl, :]), reads=["xs_scr0", "xs_scr1"], writes=["xb%d" % b2])
        for c in range(8):
            S.op("pe", lambda e, c=c: e.transpose(out=PTb[:, c * 128:(c + 1) * 128], in_=xb[b2][:, c * 128:(c + 1) * 128], identity=c_bf[:, IDENT, :]),
                 reads=["xb%d" % b2, "c_bf"], writes=["PTb"])
        S.op("dve", lambda e: e.tensor_copy(out=xTk[b2].rearrange("p a b -> p (a b)"), in_=PTb), reads=["PTb"], writes=["xTk%d" % b2])

    def stG(b):
        b2 = b % 2
        for c in range(8):
            S.op("pe", lambda e, c=c: e.matmul(PGa, lhsT=xTk[b2][:, c, :], rhs=Wg[:, c, :], start=(c == 0), stop=(c == 7)),
                 reads=["xTk%d" % b2, "Wga", "Wgb"], writes=["PGa"])
        S.op("act", lambda e: e.activation(out=sil, in_=PGa, func=AF.Silu), reads=["PGa"], writes=["sil"])

    def stU(b):
        b2 = b % 2
        for c in range(8):
            S.op("pe", lambda e, c=c: e.matmul(PUa, lhsT=xTk[b2][:, c, :], rhs=Wu[:, c, :], start=(c == 0), stop=(c == 7)),
                 reads=["xTk%d" % b2, "Wua", "Wub"], writes=["PUa"])
        S.op("dve", lambda e: e.tensor_tensor(out=hdn, in0=sil, in1=PUa, op=ALU.mult), reads=["sil", "PUa"], writes=["hdn"])

    def stC1(b):
        for c in range(4):
            S.op("pe", lambda e, c=c: e.transpose(out=PTh[:, c * 128:(c + 1) * 128], in_=hdn[:, c * 128:(c + 1) * 128], identity=c_bf[:, IDENT, :]),
                 reads=["hdn", "c_bf"], writes=["PTh"])
        S.op("dve", lambda e: e.tensor_copy(out=hdT.rearrange("p a b -> p (a b)"), in_=PTh[:, 0:512]), reads=["PTh"], writes=["hdT"])

    def stC2(b):
        b2 = b % 2
        rsl = slice(b * 128, (b + 1) * 128)
        for hf in range(2):
            for c in range(4):
                S.op("pe", lambda e, hf=hf, c=c: e.matmul(PY[hf], lhsT=hdT[:, c, :], rhs=Wd[:, c, hf * 512:(hf + 1) * 512],
                                                         start=(c == 0), stop=(c == 3)), reads=["hdT", "Wda", "Wdb"], writes=["PY%d" % hf])
            evac(yb[b2][:, hf * 512:(hf + 1) * 512], PY[hf], ["PY%d" % hf], ["yb%d_%d" % (b2, hf)])
        S.dma("sp", lambda e: e.dma_start(out=ys_scr[rsl, :], in_=yb[b2]), reads=["yb%d_0" % b2, "yb%d_1" % b2], writes=["ys_scr"])

    stA(0)
    wload(0, WG)
    wload(0, WU)
    for b in range(NBLK):
        if b + 1 < NBLK:
            stA(b + 1)
        if b > 0:
            stC1(b - 1)
        stG(b)
        if b + 1 < NBLK:
            wload(b + 1, WG)
        if b > 0:
            stC2(b - 1)
        wload(b, WD)
        stU(b)
        if b + 1 < NBLK:
            wload(b + 1, WU)
    stC1(NBLK - 1)
    stC2(NBLK - 1)

    if stop < 7:
        S.emit()
        return nc
    S.barrier()
    A.reset(PBASE)
    g2b = A.alloc("g2b", [128, D], F32); b2b = A.alloc("b2b", [128, D], F32)
    NB3 = 3
    y0 = [A.alloc("y0", [128, D], F32) for _ in range(NB3)]
    y1 = [A.alloc("y1", [128, D], F32) for _ in range(NB3)]
    pr = [A.alloc("pr", [128, D], F32) for _ in range(NB3)]
    ot = [A.alloc("ot", [128, D], F32) for _ in range(2)]
    st7 = [A.alloc("st7", [128, 12], F32) for _ in range(2)]
    mv7 = [A.alloc("mv7", [128, 8], F32) for _ in range(2)]
    S.dma("sp", lambda e: e.dma_start(out=g2b, in_=ln2g.partition_broadcast(128)), writes=["g2b"])
    S.dma("sp", lambda e: e.dma_start(out=b2b, in_=ln2b.partition_broadcast(128)), writes=["b2b"])

    def ld7(tt):
        b3 = tt % NB3
        tsl = slice(tt * 128, (tt + 1) * 128)
        S.dma("sp", lambda e: e.dma_start(out=pr[b3], in_=pre_scr[tsl, :]), reads=["pre_scr"], writes=["pr%d" % b3])
        S.dma("pool", lambda e: e.indirect_dma_start(
            out=y0[b3], out_offset=None, in_=ys_scr, in_offset=bass.IndirectOffsetOnAxis(ap=dest0_i[:, tt:tt + 1], axis=0)),
            reads=["ys_scr", "dest0_i"], writes=["y0_%d" % b3])
        S.dma("pool", lambda e: e.indirect_dma_start(
            out=y1[b3], out_offset=None, in_=ys_scr, in_offset=bass.IndirectOffsetOnAxis(ap=dest1_i[:, tt:tt + 1], axis=0)),
            reads=["ys_scr", "dest1_i"], writes=["y1_%d" % b3])

    def cmb7(tt):
        b3 = tt % NB3; m2 = tt % 2
        S.op("dve", lambda e: e.scalar_tensor_tensor(out=pr[b3], in0=y0[b3], scalar=g0_all[:, tt:tt + 1], in1=pr[b3], op0=ALU.mult, op1=ALU.add),
             reads=["y0_%d" % b3, "pr%d" % b3, "g0_all"], writes=["pr%d" % b3])
        S.op("dve", lambda e: e.scalar_tensor_tensor(out=pr[b3], in0=y1[b3], scalar=g1_all[:, tt:tt + 1], in1=pr[b3], op0=ALU.mult, op1=ALU.add),
             reads=["y1_%d" % b3, "pr%d" % b3, "g1_all"], writes=["pr%d" % b3])
        for hf in range(2):
            S.op("dve", lambda e, hf=hf: e.bn_stats(out=st7[m2][:, hf * 6:(hf + 1) * 6], in_=pr[b3][:, hf * 512:(hf + 1) * 512]),
                 reads=["pr%d" % b3], writes=["st7_%d_%d" % (m2, hf)])
        S.op("dve", lambda e: e.bn_aggr(out=mv7[m2][:, 0:2], in_=st7[m2]), reads=["st7_%d_0" % m2, "st7_%d_1" % m2], writes=["mv7a%d" % m2])
        S.op("act", lambda e: e.activation(out=mv7[m2][:, 2:3], in_=mv7[m2][:, 1:2], func=AF.Ln, bias=eps_c), reads=["mv7a%d" % m2, "eps_c"], writes=["mv7b%d" % m2])
        S.op("act", lambda e: e.activation(out=mv7[m2][:, 3:4], in_=mv7[m2][:, 2:3], func=AF.Exp, scale=-0.5), reads=["mv7b%d" % m2], writes=["mv7c%d" % m2])

    def fin7(tt):
        b3 = tt % NB3; m2 = tt % 2
        tsl = slice(tt * 128, (tt + 1) * 128)
        S.op("dve", lambda e: e.tensor_scalar(out=mv7[m2][:, 4:5], in0=mv7[m2][:, 0:1], scalar1=mv7[m2][:, 3:4], scalar2=-1.0, op0=ALU.mult, op1=ALU.mult),
             reads=["mv7a%d" % m2, "mv7c%d" % m2], writes=["mv7d%d" % m2])
        S.op("act", lambda e: e.activation(out=ot[m2], in_=pr[b3], func=AF.Identity, scale=mv7[m2][:, 3:4], bias=mv7[m2][:, 4:5]),
             reads=["pr%d" % b3, "mv7c%d" % m2, "mv7d%d" % m2], writes=["ot%d" % m2])
        S.op("pool", lambda e: e.tensor_tensor(out=ot[m2], in0=ot[m2], in1=g2b, op=ALU.mult), reads=["ot%d" % m2, "g2b"], writes=["ot%d" % m2])
        S.op("dve", lambda e: e.tensor_tensor(out=ot[m2], in0=ot[m2], in1=b2b, op=ALU.add), reads=["ot%d" % m2, "b2b"], writes=["ot%d" % m2])
        S.dma("sp", lambda e: e.dma_start(out=out[tsl, :], in_=ot[m2]), reads=["ot%d" % m2], writes=["out"], is_output=True)

    ld7(0)
    ld7(1)
    for tt in range(NTT):
        cmb7(tt)
        if tt > 0:
            fin7(tt - 1)
        if tt + 2 < NTT:
            ld7(tt + 2)
    fin7(NTT - 1)

    S.emit()
    return nc


_CACHE = {}


def _prep_inputs(x, p, w_in, b_forget, w_out, ln_mix_g, ln_mix_b, w_group, b_group, w_router, b_router,
                 w_gate, w_up, w_down, w_ple, w_ple_gate, ln_ffn_g, ln_ffn_b):
    f32 = np.float32
    x = np.asarray(x, f32); p = np.asarray(p, f32)
    w_in = np.asarray(w_in, f32)[0]
    kp_cols, qp_cols = [], []
    for h in range(8):
        kp_cols += list(range(512 + 64 * h, 512 + 64 * h + 64)) + list(range(2048 + 64 * h, 2048 + 64 * h + 64))
        qp_cols += list(range(0 + 64 * h, 64 * h + 64)) + list(range(1536 + 64 * h, 1536 + 64 * h + 64))
    v_cols = list(range(1024, 1536)) + list(range(2560, 3072))

    def pcl(w):
        return np.ascontiguousarray(w.reshape(8, 128, -1).transpose(1, 0, 2))

    shared = {
        "w_kp": pcl(w_in[:, kp_cols]), "w_qp": pcl(w_in[:, qp_cols]), "w_v": pcl(w_in[:, v_cols]),
        "w_f": pcl(w_in[:, 3072:3080]),
        "bf_t": np.ascontiguousarray(np.tile(np.asarray(b_forget, f32)[0], 64).reshape(1, 512)),
        "w_out": pcl(np.asarray(w_out, f32)[0]),
        "ln1g": np.asarray(ln_mix_g, f32).reshape(1, D), "ln1b": np.asarray(ln_mix_b, f32).reshape(1, D),
        "ln2g": np.asarray(ln_ffn_g, f32).reshape(1, D), "ln2b": np.asarray(ln_ffn_b, f32).reshape(1, D),
        "w_r36": pcl(np.concatenate([np.asarray(w_group, f32)[0], np.asarray(w_router, f32)[0]], axis=1)),
        "b_r36": np.ascontiguousarray(np.tile(np.concatenate([np.asarray(b_group, f32)[0], np.asarray(b_router, f32)[0]]), 32).reshape(1, 32 * 36)),
        "w_gate": np.ascontiguousarray(np.asarray(w_gate, f32)[0].reshape(32, 8, 128, 512).transpose(0, 2, 1, 3).reshape(32 * 128, 4096)),
        "w_up": np.ascontiguousarray(np.asarray(w_up, f32)[0].reshape(32, 8, 128, 512).transpose(0, 2, 1, 3).reshape(32 * 128, 4096)),
        "w_down": np.ascontiguousarray(np.asarray(w_down, f32)[0].reshape(32, 4, 128, 1024).transpose(0, 2, 1, 3).reshape(32 * 128, 4096)),
        "w_ple": np.ascontiguousarray(np.asarray(w_ple, f32)[0].reshape(2, 128, 1024).transpose(1, 0, 2)),
        "w_pg": pcl(np.asarray(w_ple_gate, f32)[0]),
    }
    k = np.arange(128)[:, None]; q = np.arange(128)[None, :]
    cst = np.zeros((128, 7, 128), f32)
    cst[:, 6, :] = -1.0
    cst[:, 0, :] = (k == q)
    cst[:, 1, :] = (k <= q)
    cst[:, 2, :] = (k < q)
    cst[:, 3, :] = 1.0
    cst[:, 4, :] = -(k >= q).astype(f32)
    cst[:, 5, :] = -(k == q).astype(f32)
    shared["consts"] = cst
    iot = np.zeros((128, 140), f32)
    iot[:, 0:12] = np.arange(12)[None, :] * 128 + np.arange(128)[:, None]
    iot[:, 44:140] = np.arange(96)[None, :] * 128.0
    shared["iot"] = iot

    def diag_tiles(strict):
        t = np.zeros((4, 128, 512), f32)
        for i in range(4):
            for jq in range(4):
                blk = t[i, :, jq * 128:(jq + 1) * 128]
                if jq < i:
                    blk[:] = BIG
                elif jq == i:
                    blk[:] = np.where((k < q) if strict else (k <= q), 0.0, BIG)
        return t

    in_maps = []
    for c in range(NCORE):
        b, par = c // 2, c % 2
        G = G_PAR[par]
        loc = np.concatenate([np.arange(g * 512, (g + 1) * 512) for g in G])
        xT = np.ascontiguousarray(x[b].reshape(SEQ, 8, 128).transpose(2, 1, 0))
        m = dict(shared)
        m["xT_all"] = xT
        m["xT_loc"] = np.ascontiguousarray(xT[:, :, loc])
        m["x_loc"] = np.ascontiguousarray(x[b][loc])
        m["pT_loc"] = np.ascontiguousarray(p[0, b][loc].reshape(NLOC, 2, 128).transpose(2, 1, 0))
        mk = np.zeros((2, 2, 8, 128, 512), f32)
        for kind in range(2):
            dt_ = diag_tiles(strict=(kind == 0))
            for sp_ in range(2):
                has_max = (sp_ == 0) if par == 1 else (sp_ == 1)
                if has_max:
                    mk[kind, sp_, 4:8] = dt_
                else:
                    mk[kind, sp_, 0:4] = dt_
                    mk[kind, sp_, 4:8] = BIG
        m["masks"] = np.ascontiguousarray(mk.reshape(32, 128, 512).transpose(1, 0, 2))
        sel = np.zeros((8, 16, 8), f32)
        for s_, g in enumerate(G):
            sel[s_, g, :] = 1.0
        m["selx"] = sel.reshape(1, 1024)
        in_maps.append(m)
    return in_maps


def kernel(**inputs):
    if "nc" not in _CACHE:
        _CACHE["nc"] = build()
    nc = _CACHE["nc"]
    in_maps = _prep_inputs(**inputs)
    res = run_bass_kernel_spmd(nc, in_maps, core_ids=list(range(NCORE)))
    outp = np.zeros((NB, SEQ, D), np.float32)
    for c in range(NCORE):
        b, par = c // 2, c % 2
        loc = np.concatenate([np.arange(g * 512, (g + 1) * 512) for g in G_PAR[par]])
        outp[b, loc] = res.results[c]["out"]
    return outp
```

```python
import contextlib
import os
import numpy as np
import concourse.bass as bass
import concourse.mybir as mybir
from concourse.bass_utils import run_bass_kernel_spmd

F32 = mybir.dt.float32
BF16 = mybir.dt.bfloat16
I32 = mybir.dt.int32
AF = mybir.ActivationFunctionType
ALU = mybir.AluOpType
AX = mybir.AxisListType

D = 1024
SEQ = 8192
NB = 4
NCORE = 8
NLOC = 4096
NTT = 32
NBLK = 96
CAP = NBLK * 128
ALPHA = 2 ** 0.25
LN_EPS = 1e-5
BIG = -30000.0
G_PAR = ([0, 3, 4, 7, 8, 11, 12, 15], [1, 2, 5, 6, 9, 10, 13, 14])


class Sched:
    ENGS = ("pe", "act", "dve", "pool", "sp")

    def __init__(self, nc, n_dma_sems=10):
        self.nc = nc
        self.ops = {e: [] for e in self.ENGS}
        self.cnt = {e: 0 for e in self.ENGS}
        self.last_w = {}
        self.readers = {}
        self.seen = {e: {} for e in self.ENGS}
        self.n_dma_sems = n_dma_sems
        self.dma_used = {}
        self.dma_rr = {"sp": 0, "pool": 0, "act": 0}
        self.out_tokens = []

    def _deps(self, eng, reads, writes):
        deps = {}

        def add(k, v):
            if deps.get(k, 0) < v:
                deps[k] = v

        for k in reads:
            t = self.last_w.get(k)
            if t:
                add(*t)
        for k in writes:
            t = self.last_w.get(k)
            if t:
                add(*t)
            for kk, vv in self.readers.get(k, {}).items():
                add(kk, vv)
        waits = []
        for k, v in deps.items():
            if k == "pe" and eng == "pe":
                continue
            if self.seen[eng].get(k, 0) >= v:
                continue
            self.seen[eng][k] = v
            waits.append((k, v))
        return waits

    def _commit(self, tok, reads, writes):
        for k in reads:
            r = self.readers.setdefault(k, {})
            if r.get(tok[0], 0) < tok[1]:
                r[tok[0]] = tok[1]
        for k in writes:
            self.last_w[k] = tok
            self.readers[k] = {}

    def op(self, eng, fn, reads=(), writes=()):
        waits = self._deps(eng, reads, writes)
        self.cnt[eng] += 1
        tok = (eng, self.cnt[eng])
        self.ops[eng].append((fn, waits, (eng, 1)))
        self._commit(tok, reads, writes)
        return tok

    def dma(self, q, fn, reads=(), writes=(), is_output=False):
        waits = self._deps(q, reads, writes)
        slot = self.dma_rr[q] % self.n_dma_sems
        self.dma_rr[q] += 1
        key = "dma_%s_%d" % (q, slot)
        used = self.dma_used.get(key, 0)
        if used > 0 and self.seen[q].get(key, 0) < 16 * used:
            self.seen[q][key] = 16 * used
            waits.append((key, 16 * used))
        self.dma_used[key] = used + 1
        tok = (key, 16 * (used + 1))
        self.ops[q].append((fn, waits, (key, 16)))
        self._commit(tok, reads, writes)
        if is_output:
            self.out_tokens.append(tok)
        return tok

    def barrier(self):
        toks = [(e, self.cnt[e]) for e in self.ENGS if self.cnt[e] > 0]
        toks += [(k, 16 * u) for k, u in self.dma_used.items()]
        for e in self.ENGS:
            waits = []
            for k, v in toks:
                if k == e and e == "pe":
                    continue
                if self.seen[e].get(k, 0) >= v:
                    continue
                self.seen[e][k] = v
                waits.append((k, v))
            if waits:
                self.ops[e].append((None, waits, None))

    def emit(self):
        nc = self.nc
        fin = []
        for e in self.ENGS:
            if self.cnt[e] > 0 and self.seen["sp"].get(e, 0) < self.cnt[e]:
                fin.append((e, self.cnt[e]))
        for k, u in self.dma_used.items():
            if self.seen["sp"].get(k, 0) < 16 * u:
                fin.append((k, 16 * u))
        self.ops["sp"].append((None, fin, None))
        keys = set()
        for e in self.ENGS:
            for fn, waits, inc in self.ops[e]:
                for k, v in waits:
                    keys.add(k)
                if inc is not None:
                    keys.add(inc[0])
        with contextlib.ExitStack() as st:
            sems = {}
            for k in sorted(keys):
                sems[k] = st.enter_context(nc.semaphore("s_" + k))
            block = st.enter_context(nc.Block())

            def run(engname):
                def body(eng):
                    for fn, waits, inc in self.ops[engname]:
                        for k, v in waits:
                            eng.wait_ge(sems[k], v)
                        if fn is not None:
                            ins = fn(eng)
                            ins.then_inc(sems[inc[0]], inc[1])
                return body

            block.tensor(run("pe"))
            block.scalar(run("act"))
            block.vector(run("dve"))
            block.gpsimd(run("pool"))
            block.sync(run("sp"))


class Arena:
    def __init__(self, nc, limit=229376 - 256):
        self.nc = nc
        self.off = 16640
        self.limit = limit
        self.n = 0

    def alloc(self, name, shape, dtype):
        esz = {F32: 4, BF16: 2, I32: 4}[dtype]
        per = esz
        for s in shape[1:]:
            per *= s
        self.off = (self.off + 63) // 64 * 64
        self.n += 1
        t = self.nc.alloc_sbuf_tensor_at("%s_%d" % (name, self.n), list(shape), dtype, offset=self.off)
        self.off += per
        assert self.off <= self.limit, (name, self.off)
        return t.ap()

    def mark(self):
        return self.off

    def reset(self, m):
        self.off = m


def build(debug=False, stop=99):
    nc = bass.Bass("TRN2", target_bir_lowering=False)
    S = Sched(nc)
    A = Arena(nc)

    def din(name, shape, dt=F32):
        return nc.dram_tensor(name, list(shape), dt, kind="ExternalInput").ap()

    def dscr(name, shape, dt, dbg=False):
        return nc.dram_tensor(name, list(shape), dt, kind=("ExternalOutput" if (dbg and debug) else "Internal")).ap()

    xT_all = din("xT_all", [128, 8, SEQ])
    xT_loc = din("xT_loc", [128, 8, NLOC])
    x_loc = din("x_loc", [NLOC, D])
    pT_loc = din("pT_loc", [128, 2, NLOC])
    w_kp = din("w_kp", [128, 8, 1024])
    w_qp = din("w_qp", [128, 8, 1024])
    w_v = din("w_v", [128, 8, 1024])
    w_f = din("w_f", [128, 8, 8])
    bf_t = din("bf_t", [1, 512])
    w_out = din("w_out", [128, 8, 1024])
    ln1g = din("ln1g", [1, D]); ln1b = din("ln1b", [1, D])
    ln2g = din("ln2g", [1, D]); ln2b = din("ln2b", [1, D])
    w_r36 = din("w_r36", [128, 8, 36])
    b_r36 = din("b_r36", [1, 32 * 36])
    w_gate = din("w_gate", [32 * 128, 4096])
    w_up = din("w_up", [32 * 128, 4096])
    w_down = din("w_down", [32 * 128, 4096])
    w_ple = din("w_ple", [128, 2, 1024])
    w_pg = din("w_pg", [128, 8, 1024])
    masks = din("masks", [128, 32, 512])
    selx = din("selx", [1, 1024])
    consts = din("consts", [128, 7, 128])
    iot = din("iot", [128, 12 + 32 + 96], F32)
    out = nc.dram_tensor("out", [NLOC, D], F32, kind="ExternalOutput").ap()

    kt_scr = dscr("kt_scr", [8, 128, SEQ], BF16)
    v_scr = dscr("v_scr", [8, 128, 64 * 128], BF16)
    qt_scr = dscr("qt_scr", [8, 128, NLOC], BF16)
    h_scr = dscr("h_scr", [NLOC, D], F32, dbg=True)
    pre_scr = dscr("pre_scr", [NLOC, D], F32, dbg=True)
    xs_scr = dscr("xs_scr", [CAP, D], BF16)
    ys_scr = dscr("ys_scr", [CAP, D], F32)
    dbg_r = dscr("dbg_r", [128, 4096], F32, dbg=True)

    PSALL = nc.alloc_psum_tensor("psall", [128, 4096], F32).ap()
    PS = [PSALL[:, i * 512:(i + 1) * 512] for i in range(8)]

    c_f32 = A.alloc("c_f32", [128, 7, 128], F32)
    c_bf = A.alloc("c_bf", [128, 7, 128], BF16)
    IDENT, TRII, STRICT, ONES, NEGTRI, NEGID, NEGONE = range(7)
    cfpos = A.alloc("cfpos", [128, 512], F32)
    cref = A.alloc("cref", [128, 64], F32)
    L_all = A.alloc("L_all", [128, 32 * 36], F32)
    g0_all = A.alloc("g0_all", [128, 32], F32)
    g1_all = A.alloc("g1_all", [128, 32], F32)
    dest0_i = A.alloc("dest0_i", [128, 32], I32)
    dest1_i = A.alloc("dest1_i", [128, 32], I32)
    idxA = A.alloc("idxA", [128, NBLK], I32)
    idxB = A.alloc("idxB", [128, NBLK], I32)
    iot_sb = A.alloc("iot_sb", [128, 140], F32)
    eps_c = A.alloc("eps_c", [128, 1], F32)
    st6 = A.alloc("st6", [128, 12], F32)
    mv = A.alloc("mv", [128, 4], F32)
    PBASE = A.mark()

    S.dma("sp", lambda e: e.dma_start(out=c_f32, in_=consts), writes=["c_f32"])
    S.dma("pool", lambda e: e.dma_start(out=c_bf, in_=consts), writes=["c_bf"])
    S.dma("sp", lambda e: e.dma_start(out=iot_sb, in_=iot), writes=["iot"])
    S.op("dve", lambda e: e.memset(eps_c, LN_EPS), writes=["eps_c"])

    rr = {"ev": 0}

    def evac(out_ap, in_ap, reads, writes, scale=None):
        rr["ev"] += 1
        if rr["ev"] % 2 == 0:
            if scale is None:
                S.op("act", lambda e: e.activation(out=out_ap, in_=in_ap, func=AF.Copy), reads=reads, writes=writes)
            else:
                S.op("act", lambda e: e.activation(out=out_ap, in_=in_ap, func=AF.Copy, scale=scale), reads=reads, writes=writes)
        else:
            if scale is None:
                S.op("dve", lambda e: e.tensor_copy(out=out_ap, in_=in_ap), reads=reads, writes=writes)
            else:
                S.op("dve", lambda e: e.tensor_scalar(out=out_ap, in0=in_ap, scalar1=scale, scalar2=None, op0=ALU.mult),
                     reads=reads, writes=writes)

    Wkp = A.alloc("Wkp", [128, 8, 1024], BF16)
    Wqp = A.alloc("Wqp", [128, 8, 1024], BF16)
    Wv = A.alloc("Wv", [128, 8, 1024], BF16)
    Wf = A.alloc("Wf", [128, 8, 8], F32)
    bF = A.alloc("bF", [128, 512], F32)
    selx_b = A.alloc("selx_b", [128, 1024], F32)
    xTb = [A.alloc("xTb", [128, 8, 512], BF16) for _ in range(2)]
    xTf = [A.alloc("xTf", [128, 8, 512], F32) for _ in range(2)]
    KTst = [A.alloc("KTst", [128, 8, 512], BF16) for _ in range(2)]
    Vst = [A.alloc("Vst", [128, 8, 512], BF16) for _ in range(2)]

    for j in range(8):
        S.dma("pool", lambda e, j=j: e.dma_start(out=Wkp[:, j, :], in_=w_kp[:, j, :]), writes=["Wkp"])
        S.dma("pool", lambda e, j=j: e.dma_start(out=Wv[:, j, :], in_=w_v[:, j, :]), writes=["Wv"])
        S.dma("pool", lambda e, j=j: e.dma_start(out=Wqp[:, j, :], in_=w_qp[:, j, :]), writes=["Wqp"])
    mask_bf = nc.alloc_sbuf_tensor_at("mask_bf_fix", [128, 32, 512], BF16, offset=194560).ap()
    S.dma("sp", lambda e: e.dma_start(out=Wf, in_=w_f), writes=["Wf"])
    S.dma("sp", lambda e: e.dma_start(out=bF, in_=bf_t.partition_broadcast(128)), writes=["bF"])
    S.dma("sp", lambda e: e.dma_start(out=selx_b, in_=selx.partition_broadcast(128)), writes=["selx_b"])

    PF = PS[7]
    bank = {"i": 0}

    def nxt_bank(n=7):
        bank["i"] = (bank["i"] + 1) % n
        return bank["i"]

    for g in range(16):
        b2 = g % 2
        for c in range(8):
            S.dma("pool", lambda e, g=g, b2=b2, c=c: e.dma_start(out=xTb[b2][:, c, :], in_=xT_all[:, c, g * 512:(g + 1) * 512]),
                  writes=["xTb%d" % b2])
        S.dma("sp", lambda e, g=g, b2=b2: e.dma_start(out=xTf[b2], in_=xT_all[:, :, g * 512:(g + 1) * 512]),
              writes=["xTf%d" % b2])
        for hp in range(8):
            bi = nxt_bank()
            for c in range(8):
                S.op("pe", lambda e, bi=bi, hp=hp, c=c, b2=b2: e.matmul(PS[bi], lhsT=Wkp[:, c, hp * 128:(hp + 1) * 128],
                                                                       rhs=xTb[b2][:, c, :], start=(c == 0), stop=(c == 7)),
                     reads=["Wkp", "xTb%d" % b2], writes=["ps%d" % bi])
            evac(KTst[b2][:, hp, :], PS[bi], ["ps%d" % bi], ["KTst%d" % b2])
        S.dma("sp", lambda e, g=g, b2=b2: e.dma_start(out=kt_scr[:, :, g * 512:(g + 1) * 512].rearrange("h p t -> p h t"),
                                                      in_=KTst[b2]),
              reads=["KTst%d" % b2], writes=["kt_scr"])
        for j in range(4):
            for half in range(2):
                bi = nxt_bank()
                for c in range(8):
                    S.op("pe", lambda e, bi=bi, j=j, half=half, c=c, b2=b2: e.matmul(
                        PS[bi], lhsT=xTb[b2][:, c, j * 128:(j + 1) * 128], rhs=Wv[:, c, half * 512:(half + 1) * 512],
                        start=(c == 0), stop=(c == 7)), reads=["Wv", "xTb%d" % b2], writes=["ps%d" % bi])
                dst = Vst[b2].rearrange("p h (j t e) -> p h j t e", j=4, t=2, e=64)[:, :, j, half, :]
                evac(dst, PS[bi].rearrange("p (h e) -> p h e", h=8, e=64), ["ps%d" % bi], ["Vst%d" % b2])
        S.dma("sp", lambda e, g=g, b2=b2: e.dma_start(
            out=v_scr[:, :, g * 512:(g + 1) * 512].rearrange("h p t -> p h t"), in_=Vst[b2]),
            reads=["Vst%d" % b2], writes=["v_scr"])
        for j in range(4):
            kb = 4 * g + j
            for c in range(8):
                S.op("pe", lambda e, kb=kb, j=j, c=c, b2=b2: e.matmul(PF[:, kb * 8:(kb + 1) * 8], lhsT=xTf[b2][:, c, j * 128:(j + 1) * 128],
                                                                     rhs=Wf[:, c, :], start=(c == 0), stop=(c == 7)),
                     reads=["Wf", "xTf%d" % b2], writes=["PF"])
    for j in range(32):
        S.dma("pool", lambda e, j=j: e.dma_start(out=mask_bf[:, j, :], in_=masks[:, j, :]), writes=["mask_bf"])
    m1 = A.mark()
    lf = A.alloc("lf", [128, 512], F32)
    lfn = A.alloc("lfn", [128, 512], F32)
    Ta = A.alloc("Ta", [128, 512], F32)
    Tb = A.alloc("Tb", [128, 512], F32)
    T0 = A.alloc("T0", [128, 512], F32)
    prod = A.alloc("prod", [128, 128], F32)
    S.op("dve", lambda e: e.tensor_tensor(out=lf, in0=PF, in1=bF, op=ALU.add), reads=["PF", "bF"], writes=["lf"])
    S.op("act", lambda e: e.activation(out=lfn, in_=lf, func=AF.Exp, scale=-1.0), reads=["lf"], writes=["lfn"])
    S.op("act", lambda e: e.activation(out=lf, in_=lfn, func=AF.Ln, bias=1.0), reads=["lfn"], writes=["lf"])
    S.op("pe", lambda e: e.matmul(PS[5], lhsT=c_f32[:, TRII, :], rhs=lf, start=True, stop=True), reads=["c_f32", "lf"], writes=["ps5"])
    S.op("pe", lambda e: e.matmul(PS[6], lhsT=c_f32[:, ONES, :], rhs=lf, start=True, stop=True), reads=["c_f32", "lf"], writes=["ps6"])
    S.op("dve", lambda e: e.tensor_copy(out=T0, in_=PS[6]), reads=["ps6"], writes=["T0"])
    S.op("dve", lambda e: e.tensor_copy(out=Ta, in_=PS[6]), reads=["ps6"], writes=["Ta"])
    cur, oth, cn, on = Ta, Tb, "Ta", "Tb"
    for dsh in (1, 2, 4, 8, 16, 32):
        w_ = 8 * dsh
        S.op("dve", lambda e, cur=cur, oth=oth, w_=w_: e.tensor_tensor(out=oth[:, w_:], in0=cur[:, w_:], in1=cur[:, :512 - w_], op=ALU.add),
             reads=[cn], writes=[on])
        S.op("dve", lambda e, cur=cur, oth=oth, w_=w_: e.tensor_copy(out=oth[:, :w_], in_=cur[:, :w_]), reads=[cn, on], writes=[on])
        cur, oth, cn, on = oth, cur, on, cn
    Tincl, tin = cur, cn
    S.op("dve", lambda e: e.tensor_tensor(out=lfn, in0=Tincl, in1=T0, op=ALU.subtract), reads=[tin, "T0"], writes=["lfn"])
    S.op("dve", lambda e: e.tensor_tensor(out=cfpos, in0=PS[5], in1=lfn, op=ALU.add), reads=["ps5", "lfn"], writes=["cfpos"])
    Tlast = Tincl.rearrange("p (g j h) -> p g j h", g=16, j=4, h=8)[:, :, 3, :]
    for s in range(8):
        S.op("dve", lambda e, s=s: e.tensor_tensor(out=prod.rearrange("p (h g) -> p g h", h=8, g=16), in0=Tlast,
                                                  in1=selx_b[:, s * 128:(s + 1) * 128].rearrange("p (g h) -> p g h", g=16, h=8),
                                                  op=ALU.mult), reads=[tin, "selx_b"], writes=["prod"])
        S.op("dve", lambda e, s=s: e.tensor_reduce(out=cref[:, s * 8:(s + 1) * 8], in_=prod.rearrange("p (h g) -> p h g", h=8, g=16),
                                                  axis=AX.X, op=ALU.add), reads=["prod"], writes=["cref"])

    for s in range(8):
        b2 = s % 2
        S.dma("sp", lambda e, s=s, b2=b2: e.dma_start(out=xTf[b2], in_=xT_loc[:, :, s * 512:(s + 1) * 512]),
              writes=["xTf%d" % b2])
        S.op("act", lambda e, b2=b2: e.activation(out=xTb[b2][:, 0:4, :], in_=xTf[b2][:, 0:4, :], func=AF.Copy),
             reads=["xTf%d" % b2], writes=["xTb%d" % b2])
        S.op("dve", lambda e, b2=b2: e.tensor_copy(out=xTb[b2][:, 4:8, :], in_=xTf[b2][:, 4:8, :]),
             reads=["xTf%d" % b2, "xTb%d" % b2], writes=["xTb%d" % b2])
        for hp in range(8):
            bi = nxt_bank(5)
            for c in range(8):
                S.op("pe", lambda e, bi=bi, hp=hp, c=c, b2=b2: e.matmul(PS[bi], lhsT=Wqp[:, c, hp * 128:(hp + 1) * 128],
                                                                       rhs=xTb[b2][:, c, :], start=(c == 0), stop=(c == 7)),
                     reads=["Wqp", "xTb%d" % b2], writes=["ps%d" % bi])
            evac(KTst[b2][:, hp, :], PS[bi], ["ps%d" % bi], ["KTst%d" % b2], scale=0.125)
        S.dma("sp", lambda e, s=s, b2=b2: e.dma_start(out=qt_scr[:, :, s * 512:(s + 1) * 512].rearrange("h p t -> p h t"),
                                                      in_=KTst[b2]),
              reads=["KTst%d" % b2], writes=["qt_scr"])

    if stop < 2:
        S.emit()
        return nc
    S.barrier()
    A.reset(PBASE)
    mixT = A.alloc("mixT", [128, 8, NLOC], BF16)
    M3 = A.mark()
    KT = A.alloc("KT", [128, SEQ], BF16)
    Vt = A.alloc("Vt", [128, 64 * 192], BF16)
    QT = A.alloc("QT", [128, NLOC], BF16)
    Et2 = [A.alloc("Et2", [128, 1024], F32) for _ in range(2)]
    SP2 = [A.alloc("SP2", [128, 1024], BF16) for _ in range(2)]
    Wt2 = [A.alloc("Wt2", [128, 1024], BF16) for _ in range(2)]
    Ybf = [A.alloc("Ybf", [128, 512], BF16) for _ in range(2)]
    Pt = [A.alloc("Pt", [128, 512], BF16) for _ in range(2)]
    biasF = [A.alloc("biasF", [128, 64], F32) for _ in range(2)]
    rinv = A.alloc("rinv", [128, 512], F32)
    XSA = [PSALL[:, 0:1024], PSALL[:, 1024:2048]]
    XF1 = PS[4]
    YB, OB, OFB = PS[5], PS[6], PS[7]
    Vt4 = Vt.rearrange("p (k t e) -> p k t e", k=64, t=3, e=64)
    S.op("dve", lambda e: e.memset(Vt4[:, :, 2, :], 1.0), writes=["Vones"])
    cf3 = cfpos.rearrange("p (k h) -> p k h", k=64, h=8)

    tiles = []
    for hp in range(8):
        for s in range(8):
            nkb = 8 * s + 8
            for kb in range(nkb - 1, -1, -1):
                tiles.append(dict(hp=hp, s=s, kb=kb, first=(kb == nkb - 1), last=(kb == 0), j=kb - 8 * s,
                                  newhp=(s == 0 and kb == nkb - 1), news=(kb == nkb - 1)))
    NT = len(tiles)
    NP = NT // 2

    def st_load(t):
        hp = t["hp"]
        S.dma("sp", lambda e: e.dma_start(out=KT, in_=kt_scr[hp]), reads=["kt_scr"], writes=["KT"])
        S.dma("sp", lambda e: e.dma_start(out=QT, in_=qt_scr[hp]), reads=["qt_scr"], writes=["QT"])
        S.dma("sp", lambda e: e.dma_start(out=Vt4[:, :, 0:2, :], in_=v_scr[hp].rearrange("p (k t e) -> p k t e", k=64, t=2, e=64)),
              reads=["v_scr"], writes=["Vt"])

    def st_bias(t):
        hp, s = t["hp"], t["s"]
        nkb = 8 * s + 8
        bb = s % 2
        S.op("dve", lambda e: e.tensor_scalar(out=biasF[bb][:, 0:nkb], in0=cf3[:, 0:nkb, hp], scalar1=cref[:, s * 8 + hp:s * 8 + hp + 1],
                                              scalar2=None, op0=ALU.subtract), reads=["cfpos", "cref"], writes=["biasF%d" % bb])

    def s1p(n):
        for h in range(2):
            t = tiles[2 * n + h]
            hp, s, kb, j = t["hp"], t["s"], t["kb"], t["j"]
            if t["newhp"]:
                st_load(t)
            if t["news"]:
                st_bias(t)
            xs = XSA[n % 2][:, h * 512:(h + 1) * 512]
            key = "XS%d_%d" % (n % 2, h)
            S.op("pe", lambda e, xs=xs, kb=kb, s=s, j=j: e.matmul(xs, lhsT=KT[0:64, kb * 128:(kb + 1) * 128], rhs=QT[0:64, s * 512:(s + 1) * 512],
                                                                 start=True, stop=False), reads=["KT", "QT"], writes=[key])
            if j >= 0:
                mi = (0 * 2 + s % 2) * 8 + j
                S.op("pe", lambda e, xs=xs, mi=mi: e.matmul(xs, lhsT=c_bf[:, IDENT, :], rhs=mask_bf[:, mi, :], start=False, stop=False),
                     reads=["c_bf", "mask_bf"], writes=[key])

    def f1(n, h):
        t = tiles[2 * n + h]
        hp, s, kb, j = t["hp"], t["s"], t["kb"], t["j"]
        S.op("pe", lambda e: e.matmul(XF1, lhsT=KT[64:128, kb * 128:(kb + 1) * 128], rhs=QT[64:128, s * 512:(s + 1) * 512],
                                      start=True, stop=(j < 0)), reads=["KT", "QT"], writes=["XF"])
        if j >= 0:
            mi = (1 * 2 + s % 2) * 8 + j
            S.op("pe", lambda e: e.matmul(XF1, lhsT=c_bf[:, IDENT, :], rhs=mask_bf[:, mi, :], start=False, stop=True),
                 reads=["c_bf", "mask_bf"], writes=["XF"])

    def s2p(n):
        q = n % 2
        S.op("act", lambda e: e.activation(out=Et2[q], in_=XSA[q], func=AF.Exp), reads=["XS%d_0" % q, "XS%d_1" % q], writes=["Et2_%d" % q])

    def s3p(n):
        q = n % 2
        S.op("act", lambda e: e.activation(out=SP2[q], in_=Et2[q], func=AF.Ln, bias=1.0), reads=["Et2_%d" % q], writes=["SP2_%d" % q])

    def s4p(n):
        ta, tb = tiles[2 * n], tiles[2 * n + 1]
        q = n % 2
        xa = XSA[q][:, 0:512]; xb_ = XSA[q][:, 512:1024]
        spa = SP2[q][:, 0:512]; spb = SP2[q][:, 512:1024]
        ka, kb_ = "XS%d_0" % q, "XS%d_1" % q
        spk = "SP2_%d" % q
        yprev = Ybf[1 - q]; ypk = "Ybf%d" % (1 - q)
        S.op("pe", lambda e: e.matmul(xa, lhsT=c_bf[:, NEGTRI, :], rhs=spa, start=False, stop=ta["first"]), reads=["c_bf", spk], writes=[ka])
        if not ta["first"]:
            S.op("pe", lambda e: e.matmul(xa, lhsT=c_bf[:, NEGID, :], rhs=yprev, start=False, stop=True), reads=["c_bf", ypk], writes=[ka])
        S.op("pe", lambda e: e.matmul(xb_, lhsT=c_bf[:, NEGTRI, :], rhs=spb, start=False, stop=False), reads=["c_bf", spk], writes=[kb_])
        if not ta["first"]:
            S.op("pe", lambda e: e.matmul(xb_, lhsT=c_bf[:, NEGID, :], rhs=yprev, start=False, stop=False), reads=["c_bf", ypk], writes=[kb_])
        S.op("pe", lambda e: e.matmul(xb_, lhsT=c_bf[:, NEGONE, :], rhs=spa, start=False, stop=True), reads=["c_bf", spk], writes=[kb_])
        S.op("pe", lambda e: e.matmul(YB, lhsT=c_bf[:, ONES, :], rhs=spa, start=ta["first"], stop=False), reads=["c_bf", spk], writes=["Y"])
        S.op("pe", lambda e: e.matmul(YB, lhsT=c_bf[:, ONES, :], rhs=spb, start=False, stop=tb["last"]), reads=["c_bf", spk], writes=["Y"])
        if not tb["last"]:
            S.op("dve", lambda e: e.tensor_copy(out=Ybf[q], in_=YB), reads=["Y"], writes=["Ybf%d" % q])

    def s6p(n):
        q = n % 2
        S.op("act", lambda e: e.activation(out=Wt2[q], in_=XSA[q], func=AF.Exp), reads=["XS%d_0" % q, "XS%d_1" % q], writes=["Wt2_%d" % q])

    def s7(n, h):
        t = tiles[2 * n + h]
        q = n % 2
        hp, s, kb = t["hp"], t["s"], t["kb"]
        S.op("pe", lambda e: e.matmul(OB[0:64, :], lhsT=Vt4[:, kb, 0, :], rhs=Wt2[q][:, h * 512:(h + 1) * 512], start=t["first"], stop=t["last"]),
             reads=["Vt", "Wt2_%d" % q], writes=["O"])
        if t["last"]:
            po = (hp % 2) * 64
            S.op("dve", lambda e: e.tensor_copy(out=mixT[po:po + 64, hp // 2, s * 512:(s + 1) * 512], in_=OB[0:64, :]),
                 reads=["O"], writes=["mixT"])

    def f2(n, h):
        t = tiles[2 * n + h]
        bb = t["s"] % 2; kb = t["kb"]
        S.op("act", lambda e: e.activation(out=Pt[h], in_=XF1, func=AF.Exp, bias=biasF[bb][:, kb:kb + 1]),
             reads=["XF", "biasF%d" % bb], writes=["Pt%d" % h])

    def f3(n, h):
        t = tiles[2 * n + h]
        hp, s, kb = t["hp"], t["s"], t["kb"]
        S.op("pe", lambda e: e.matmul(OFB, lhsT=Vt4[:, kb, 1:3, :].rearrange("p a b -> p (a b)"), rhs=Pt[h], start=t["first"], stop=t["last"]),
             reads=["Vt", "Vones", "Pt%d" % h], writes=["OF"])
        if t["last"]:
            po = (hp % 2) * 64
            S.op("dve", lambda e: e.reciprocal(out=rinv[0:64, :], in_=OFB[64:128, :]), reads=["OF"], writes=["rinv"])
            S.op("dve", lambda e: e.tensor_tensor(out=mixT[po:po + 64, 4 + hp // 2, s * 512:(s + 1) * 512], in0=OFB[0:64, :],
                                                  in1=rinv[0:64, :], op=ALU.mult), reads=["OF", "rinv"], writes=["mixT"])

    s1p(0)
    f1(0, 0)
    flushed = True
    for n in range(NP):
        boundary = (n + 1 < NP) and tiles[2 * (n + 1)]["newhp"]
        s2p(n)
        if not flushed:
            s6p(n - 1)
            s7(n - 1, 0)
            s7(n - 1, 1)
        flushed = False
        if n + 1 < NP and not boundary:
            s1p(n + 1)
        f2(n, 0)
        f1(n, 1)
        s3p(n)
        s4p(n)
        f3(n, 0)
        f2(n, 1)
        f3(n, 1)
        if n + 1 < NP and not boundary:
            f1(n + 1, 0)
        if boundary or n + 1 == NP:
            s6p(n)
            s7(n, 0)
            s7(n, 1)
            flushed = True
            if boundary:
                s1p(n + 1)
                f1(n + 1, 0)

    if stop < 3:
        S.emit()
        return nc
    S.barrier()
    A.reset(M3)
    Wout = A.alloc("Wout", [128, 8, 1024], BF16)
    Wpg = A.alloc("Wpg", [128, 8, 1024], BF16)
    Wple = A.alloc("Wple", [128, 2, 1024], BF16)
    Wr = A.alloc("Wr", [128, 8, 36], F32)
    g1b = A.alloc("g1b", [128, D], F32); b1b = A.alloc("b1b", [128, D], F32)
    b36 = A.alloc("b36", [128, 32 * 36], F32)
    xt = [A.alloc("xt", [128, D], F32) for _ in range(2)]
    pTb = [A.alloc("pTb", [128, 2, 128], BF16) for _ in range(2)]
    rt = A.alloc("rt", [128, D], F32)
    ht = [A.alloc("ht", [128, D], F32) for _ in range(2)]
    hhi = A.alloc("hhi", [128, D], BF16)
    hlo = A.alloc("hlo", [128, D], BF16)
    hTlo = A.alloc("hTlo", [128, 8, 128], BF16)
    Wrh = A.alloc("Wrh", [128, 8, 36], BF16)
    Wrl = A.alloc("Wrl", [128, 8, 36], BF16)
    hTb = A.alloc("hTb", [128, 8, 128], BF16)
    sg = A.alloc("sg", [128, D], F32)
    pre = [A.alloc("pre", [128, D], F32) for _ in range(2)]
    for j in range(8):
        S.dma("pool", lambda e, j=j: e.dma_start(out=Wout[:, j, :], in_=w_out[:, j, :]), writes=["Wout"])
        S.dma("pool", lambda e, j=j: e.dma_start(out=Wpg[:, j, :], in_=w_pg[:, j, :]), writes=["Wpg"])
    for j in range(2):
        S.dma("pool", lambda e, j=j: e.dma_start(out=Wple[:, j, :], in_=w_ple[:, j, :]), writes=["Wple"])
    S.dma("sp", lambda e: e.dma_start(out=Wr, in_=w_r36), writes=["Wr"])
    S.dma("sp", lambda e: e.dma_start(out=g1b, in_=ln1g.partition_broadcast(128)), writes=["g1b"])
    S.dma("sp", lambda e: e.dma_start(out=b1b, in_=ln1b.partition_broadcast(128)), writes=["b1b"])
    S.dma("sp", lambda e: e.dma_start(out=b36, in_=b_r36.partition_broadcast(128)), writes=["b36"])

    def layer_norm(src, dst, gb, bb, rk, wk, gk):
        for hf in range(2):
            S.op("dve", lambda e, hf=hf: e.bn_stats(out=st6[:, hf * 6:(hf + 1) * 6], in_=src[:, hf * 512:(hf + 1) * 512]),
                 reads=[rk], writes=["st6_%d" % hf])
        S.op("dve", lambda e: e.bn_aggr(out=mv[:, 0:2], in_=st6), reads=["st6_0", "st6_1"], writes=["mv"])
        S.op("act", lambda e: e.activation(out=mv[:, 2:3], in_=mv[:, 1:2], func=AF.Ln, bias=eps_c), reads=["mv", "eps_c"], writes=["mv2"])
        S.op("act", lambda e: e.activation(out=mv[:, 3:4], in_=mv[:, 2:3], func=AF.Exp, scale=-0.5), reads=["mv2"], writes=["mv3"])
        S.op("dve", lambda e: e.tensor_scalar(out=dst, in0=src, scalar1=mv[:, 0:1], scalar2=mv[:, 3:4], op0=ALU.subtract, op1=ALU.mult),
             reads=[rk, "mv", "mv3"], writes=[wk])
        S.op("dve", lambda e: e.tensor_tensor(out=dst, in0=dst, in1=gb, op=ALU.mult), reads=[wk] + gk, writes=[wk])
        S.op("dve", lambda e: e.tensor_tensor(out=dst, in0=dst, in1=bb, op=ALU.add), reads=[wk] + gk, writes=[wk])

    PA = [PS[0], PS[1]]; PTr = [PS[2], PS[3]]; PG = [PS[4], PS[5]]; PR = PS[6]
    PThi = PS[2].bitcast(BF16); PTlo = PS[3].bitcast(BF16)
    S.op("dve", lambda e: e.tensor_copy(out=Wrh, in_=Wr), reads=["Wr"], writes=["Wrh"])
    S.op("dve", lambda e: e.tensor_tensor(out=Wrl, in0=Wr, in1=Wrh, op=ALU.subtract), reads=["Wr", "Wrh"], writes=["Wrl"])
    hTb2 = [hTb, A.alloc("hTb_b", [128, 8, 128], BF16)]
    hTlo2 = [hTlo, A.alloc("hTlo_b", [128, 8, 128], BF16)]
    ple_sb = [A.alloc("ple_sb", [128, D], F32) for _ in range(2)]
    PP = PS[7]

    def p1(tt):
        b2 = tt % 2
        tsl = slice(tt * 128, (tt + 1) * 128)
        S.dma("sp", lambda e: e.dma_start(out=xt[b2], in_=x_loc[tsl, :]), writes=["xt%d" % b2])
        S.dma("pool", lambda e: e.dma_start(out=pTb[b2], in_=pT_loc[:, :, tsl]), writes=["pTb%d" % b2])
        for hf in range(2):
            for c in range(8):
                S.op("pe", lambda e, hf=hf, c=c: e.matmul(PA[hf], lhsT=mixT[:, c, tsl], rhs=Wout[:, c, hf * 512:(hf + 1) * 512],
                                                         start=(c == 0), stop=(c == 7)), reads=["mixT", "Wout"], writes=["PA%d" % hf])
            S.op("dve", lambda e, hf=hf: e.scalar_tensor_tensor(out=rt[:, hf * 512:(hf + 1) * 512], in0=xt[b2][:, hf * 512:(hf + 1) * 512],
                                                               scalar=ALPHA, in1=PA[hf], op0=ALU.mult, op1=ALU.add),
                 reads=["xt%d" % b2, "PA%d" % hf], writes=["rt"])
        for hf in range(2):
            for c in range(2):
                S.op("pe", lambda e, hf=hf, c=c: e.matmul(PP, lhsT=pTb[b2][:, c, :], rhs=Wple[:, c, hf * 512:(hf + 1) * 512],
                                                         start=(c == 0), stop=(c == 1)), reads=["pTb%d" % b2, "Wple"], writes=["PP"])
            S.op("act", lambda e, hf=hf: e.activation(out=ple_sb[b2][:, hf * 512:(hf + 1) * 512], in_=PP, func=AF.Copy),
                 reads=["PP"], writes=["ple%d_%d" % (b2, hf)])

    def ln_a(src, rk):
        for hf in range(2):
            S.op("dve", lambda e, hf=hf: e.bn_stats(out=st6[:, hf * 6:(hf + 1) * 6], in_=src[:, hf * 512:(hf + 1) * 512]),
                 reads=[rk], writes=["st6_%d" % hf])
        S.op("dve", lambda e: e.bn_aggr(out=mv[:, 0:2], in_=st6), reads=["st6_0", "st6_1"], writes=["mv"])
        S.op("act", lambda e: e.activation(out=mv[:, 2:3], in_=mv[:, 1:2], func=AF.Ln, bias=eps_c), reads=["mv", "eps_c"], writes=["mv2"])
        S.op("act", lambda e: e.activation(out=mv[:, 3:4], in_=mv[:, 2:3], func=AF.Exp, scale=-0.5), reads=["mv2"], writes=["mv3"])

    def ln_b(src, dst, gb, bb, rk, wk, gk):
        S.op("dve", lambda e: e.tensor_scalar(out=dst, in0=src, scalar1=mv[:, 0:1], scalar2=mv[:, 3:4], op0=ALU.subtract, op1=ALU.mult),
             reads=[rk, "mv", "mv3"], writes=[wk])
        S.op("dve", lambda e: e.tensor_tensor(out=dst, in0=dst, in1=gb, op=ALU.mult), reads=[wk] + gk, writes=[wk])
        S.op("dve", lambda e: e.tensor_tensor(out=dst, in0=dst, in1=bb, op=ALU.add), reads=[wk] + gk, writes=[wk])

    def p2a(tt):
        ln_a(rt, "rt")

    def p2(tt):
        b2 = tt % 2
        tsl = slice(tt * 128, (tt + 1) * 128)
        ln_b(rt, ht[b2], g1b, b1b, "rt", "ht%d" % b2, ["g1b", "b1b"])
        S.dma("sp", lambda e: e.dma_start(out=h_scr[tsl, :], in_=ht[b2]), reads=["ht%d" % b2], writes=["h_scr"])
        S.op("dve", lambda e: e.tensor_copy(out=hhi, in_=ht[b2]), reads=["ht%d" % b2], writes=["hhi"])
        S.op("dve", lambda e: e.tensor_tensor(out=hlo, in0=ht[b2], in1=hhi, op=ALU.subtract), reads=["ht%d" % b2, "hhi"], writes=["hlo"])

    def p3(tt):
        b2 = tt % 2
        for c in range(8):
            S.op("pe", lambda e, c=c: e.transpose(out=PThi[:, c * 128:(c + 1) * 128], in_=hhi[:, c * 128:(c + 1) * 128],
                                                 identity=c_bf[:, IDENT, :]), reads=["hhi", "c_bf"], writes=["PTr0"])
        for c in range(8):
            S.op("pe", lambda e, c=c: e.transpose(out=PTlo[:, c * 128:(c + 1) * 128], in_=hlo[:, c * 128:(c + 1) * 128],
                                                 identity=c_bf[:, IDENT, :]), reads=["hlo", "c_bf"], writes=["PTr1"])
        S.op("act", lambda e: e.activation(out=hTb2[b2].rearrange("p a b -> p (a b)"), in_=PThi, func=AF.Copy), reads=["PTr0"], writes=["hTb%d" % b2])
        S.op("dve", lambda e: e.tensor_copy(out=hTlo2[b2].rearrange("p a b -> p (a b)"), in_=PTlo), reads=["PTr1"], writes=["hTlo%d" % b2])

    def q_pe(tt):
        b2 = tt % 2
        k3 = 0
        for c in range(8):
            for (lt, ln_, rt_, rn_) in ((hTb2[b2], "hTb%d" % b2, Wrh, "Wrh"), (hTb2[b2], "hTb%d" % b2, Wrl, "Wrl"), (hTlo2[b2], "hTlo%d" % b2, Wrh, "Wrh")):
                S.op("pe", lambda e, c=c, lt=lt, rt_=rt_, k3=k3: e.matmul(PR[:, 0:36], lhsT=lt[:, c, :], rhs=rt_[:, c, :], start=(k3 == 0), stop=(k3 == 23)),
                     reads=[ln_, rn_], writes=["PR"])
                k3 += 1
        for hf in range(2):
            for c in range(8):
                S.op("pe", lambda e, hf=hf, c=c: e.matmul(PG[hf], lhsT=hTb2[b2][:, c, :], rhs=Wpg[:, c, hf * 512:(hf + 1) * 512],
                                                         start=(c == 0), stop=(c == 7)), reads=["hTb%d" % b2, "Wpg"], writes=["PG%d" % hf])
            S.op("act", lambda e, hf=hf: e.activation(out=sg[:, hf * 512:(hf + 1) * 512], in_=PG[hf], func=AF.Sigmoid),
                 reads=["PG%d" % hf], writes=["sg%d" % hf])

    def q_dve(tt):
        b2 = tt % 2
        tsl = slice(tt * 128, (tt + 1) * 128)
        S.op("dve", lambda e: e.tensor_tensor(out=L_all[:, tt * 36:(tt + 1) * 36], in0=PR[:, 0:36], in1=b36[:, tt * 36:(tt + 1) * 36], op=ALU.add),
             reads=["PR", "b36"], writes=["L_all"])
        for hf in range(2):
            S.op("dve", lambda e, hf=hf: e.tensor_tensor(out=sg[:, hf * 512:(hf + 1) * 512], in0=sg[:, hf * 512:(hf + 1) * 512],
                                                        in1=ple_sb[b2][:, hf * 512:(hf + 1) * 512], op=ALU.mult),
                 reads=["sg%d" % hf, "ple%d_%d" % (b2, hf)], writes=["sg%d" % hf])
        S.op("dve", lambda e: e.scalar_tensor_tensor(out=pre[b2], in0=ht[b2], scalar=ALPHA, in1=sg, op0=ALU.mult, op1=ALU.add),
             reads=["ht%d" % b2, "sg0", "sg1"], writes=["pre%d" % b2])
        S.dma("sp", lambda e: e.dma_start(out=pre_scr[tsl, :], in_=pre[b2]), reads=["pre%d" % b2], writes=["pre_scr"])

    p1(0)
    p2a(0)
    p2(0)
    p3(0)
    for tt in range(NTT):
        if tt + 1 < NTT:
            p1(tt + 1)
        q_pe(tt)
        if tt + 1 < NTT:
            p2a(tt + 1)
        q_dve(tt)
        if tt + 1 < NTT:
            p2(tt + 1)
            p3(tt + 1)

    if stop < 4:
        S.emit()
        return nc
    S.barrier()
    A.reset(PBASE)
    L3 = L_all.rearrange("p (t c) -> p t c", t=32, c=36)
    Lg = L3[:, :, 0:4]
    Le = L3[:, :, 4:36]
    gmax = A.alloc("gmax", [128, 32], F32)
    gm4 = A.alloc("gm4", [128, 32, 4], F32)
    eg = A.alloc("eg", [128, 32, 4], F32)
    gsum = A.alloc("gsum", [128, 32], F32)
    gp = A.alloc("gp", [128, 32], F32)
    Lm = A.alloc("Lm", [128, 32, 32], F32)
    top8 = A.alloc("top8", [128, 32, 8], F32)
    Oh0 = A.alloc("Oh0", [128, 32, 32], F32)
    Oh1 = A.alloc("Oh1", [128, 32, 32], F32)
    Oh2b = A.alloc("Oh2b", [128, 32 * 32], BF16)
    dv = A.alloc("dv", [128, 32], F32)
    ev = A.alloc("ev", [128, 32], F32)
    Ra = A.alloc("Ra", [128, 1024], F32)
    Rb = A.alloc("Rb", [128, 1024], F32)
    R0 = A.alloc("R0", [128, 1024], F32)
    cnt = A.alloc("cnt", [128, 32], F32)
    nb = A.alloc("nb", [128, 32], F32)
    pa = A.alloc("pa", [128, 32], F32)
    pb = A.alloc("pb", [128, 32], F32)
    pstart = A.alloc("pstart", [128, 32], F32)
    dfl = A.alloc("dfl", [128, 64], F32)
    cmp3 = A.alloc("cmp3", [128, NBLK, 32], F32)
    bexp = A.alloc("bexp", [128, NBLK], F32)
    idxf = A.alloc("idxf", [128, NBLK * 8], F32)

    def dv_(fn, reads, writes):
        S.op("dve", fn, reads=reads, writes=writes)

    dv_(lambda e: e.tensor_reduce(out=gmax, in_=Lg, axis=AX.X, op=ALU.max), ["L_all"], ["gmax"])
    gmax_b = gmax.unsqueeze(2).to_broadcast([128, 32, 4])
    dv_(lambda e: e.tensor_tensor(out=gm4, in0=Lg, in1=gmax_b, op=ALU.is_ge), ["L_all", "gmax"], ["gm4"])
    dv_(lambda e: e.tensor_tensor(out=eg, in0=Lg, in1=gmax_b, op=ALU.subtract), ["L_all", "gmax"], ["eg"])
    S.op("act", lambda e: e.activation(out=eg, in_=eg, func=AF.Exp), reads=["eg"], writes=["eg"])
    dv_(lambda e: e.tensor_reduce(out=gsum, in_=eg, axis=AX.X, op=ALU.add), ["eg"], ["gsum"])
    dv_(lambda e: e.reciprocal(out=gp, in_=gsum), ["gsum"], ["gp"])
    dv_(lambda e: e.tensor_scalar(out=gm4, in0=gm4, scalar1=1.0, scalar2=1e30, op0=ALU.subtract, op1=ALU.mult), ["gm4"], ["gm4"])
    dv_(lambda e: e.tensor_tensor(out=Lm.rearrange("p t (g k) -> p t g k", g=4, k=8), in0=Le.rearrange("p t (g k) -> p t g k", g=4, k=8),
                                  in1=gm4.unsqueeze(3).to_broadcast([128, 32, 4, 8]), op=ALU.add), ["L_all", "gm4"], ["Lm"])
    for t in range(32):
        dv_(lambda e, t=t: e.max(out=top8[:, t, :], in_=Lm[:, t, :]), ["Lm"], ["top8"])
    v0 = top8[:, :, 0]
    v1 = top8[:, :, 1]
    dv_(lambda e: e.tensor_tensor(out=Oh0, in0=Lm, in1=top8[:, :, 0:1].to_broadcast([128, 32, 32]), op=ALU.is_equal), ["Lm", "top8"], ["Oh0"])
    dv_(lambda e: e.tensor_tensor(out=Oh1, in0=Lm, in1=top8[:, :, 1:2].to_broadcast([128, 32, 32]), op=ALU.is_equal), ["Lm", "top8"], ["Oh1"])
    dv_(lambda e: e.tensor_tensor(out=dv, in0=v1, in1=v0, op=ALU.subtract), ["top8"], ["dv"])
    S.op("act", lambda e: e.activation(out=ev, in_=dv, func=AF.Exp), reads=["dv"], writes=["ev"])
    dv_(lambda e: e.tensor_scalar(out=dv, in0=ev, scalar1=1.0, scalar2=None, op0=ALU.add), ["ev"], ["dv"])
    dv_(lambda e: e.reciprocal(out=gsum, in_=dv), ["dv"], ["gsum"])
    dv_(lambda e: e.tensor_tensor(out=g0_all, in0=gsum, in1=gp, op=ALU.mult), ["gsum", "gp"], ["g0_all"])
    dv_(lambda e: e.tensor_tensor(out=g1_all, in0=g0_all, in1=ev, op=ALU.mult), ["g0_all", "ev"], ["g1_all"])
    dv_(lambda e: e.tensor_tensor(out=Oh2b.rearrange("p (t c) -> p t c", t=32, c=32), in0=Oh0, in1=Oh1, op=ALU.add), ["Oh0", "Oh1"], ["Oh2b"])
    for hf in range(2):
        S.op("pe", lambda e, hf=hf: e.matmul(PS[hf], lhsT=c_bf[:, STRICT, :], rhs=Oh2b[:, hf * 512:(hf + 1) * 512], start=True, stop=True),
             reads=["c_bf", "Oh2b"], writes=["ps%d" % hf])
        S.op("pe", lambda e, hf=hf: e.matmul(PS[2 + hf], lhsT=c_bf[:, ONES, :], rhs=Oh2b[:, hf * 512:(hf + 1) * 512], start=True, stop=True),
             reads=["c_bf", "Oh2b"], writes=["ps%d" % (2 + hf)])
        dv_(lambda e, hf=hf: e.tensor_copy(out=R0[:, hf * 512:(hf + 1) * 512], in_=PS[2 + hf]), ["ps%d" % (2 + hf)], ["R0"])
        dv_(lambda e, hf=hf: e.tensor_copy(out=Ra[:, hf * 512:(hf + 1) * 512], in_=PS[2 + hf]), ["ps%d" % (2 + hf)], ["Ra"])
    cur, oth, cn, on = Ra, Rb, "Ra", "Rb"
    for dsh in (1, 2, 4, 8, 16):
        w_ = 32 * dsh
        dv_(lambda e, cur=cur, oth=oth, w_=w_: e.tensor_tensor(out=oth[:, w_:], in0=cur[:, w_:], in1=cur[:, :1024 - w_], op=ALU.add), [cn], [on])
        dv_(lambda e, cur=cur, oth=oth, w_=w_: e.tensor_copy(out=oth[:, :w_], in_=cur[:, :w_]), [cn, on], [on])
        cur, oth, cn, on = oth, cur, on, cn
    Rincl, rin = cur, cn
    Rk, rkn = oth, on
    dv_(lambda e: e.tensor_copy(out=cnt, in_=Rincl[:, 31 * 32:32 * 32]), [rin], ["cnt"])
    dv_(lambda e: e.tensor_tensor(out=Rk, in0=Rincl, in1=R0, op=ALU.subtract), [rin, "R0"], [rkn])
    for hf in range(2):
        dv_(lambda e, hf=hf: e.tensor_tensor(out=Rk[:, hf * 512:(hf + 1) * 512], in0=Rk[:, hf * 512:(hf + 1) * 512], in1=PS[hf], op=ALU.add),
            [rkn, "ps%d" % hf], [rkn])
    dv_(lambda e: e.memset(nb, 0.0), [], ["nb"])
    for j in range(32):
        dv_(lambda e, j=j: e.scalar_tensor_tensor(out=nb, in0=cnt, scalar=float(128 * j), in1=nb, op0=ALU.is_gt, op1=ALU.add), ["cnt", "nb"], ["nb"])
    dv_(lambda e: e.tensor_scalar(out=nb, in0=nb, scalar1=128.0, scalar2=None, op0=ALU.mult), ["nb"], ["nb"])
    dv_(lambda e: e.tensor_copy(out=pa, in_=nb), ["nb"], ["pa"])
    cur, oth, cn, on = pa, pb, "pa", "pb"
    for dsh in (1, 2, 4, 8, 16):
        dv_(lambda e, cur=cur, oth=oth, dsh=dsh: e.tensor_tensor(out=oth[:, dsh:], in0=cur[:, dsh:], in1=cur[:, :32 - dsh], op=ALU.add), [cn], [on])
        dv_(lambda e, cur=cur, oth=oth, dsh=dsh: e.tensor_copy(out=oth[:, :dsh], in_=cur[:, :dsh]), [cn, on], [on])
        cur, oth, cn, on = oth, cur, on, cn
    pend, pen_n = cur, cn
    dv_(lambda e: e.tensor_tensor(out=pstart, in0=pend, in1=nb, op=ALU.subtract), [pen_n, "nb"], ["pstart"])
    Rk3 = Rk.rearrange("p (t c) -> p t c", t=32, c=32)
    dv_(lambda e: e.tensor_tensor(out=Rk3, in0=Rk3, in1=pstart.unsqueeze(1).to_broadcast([128, 32, 32]), op=ALU.add), [rkn, "pstart"], [rkn])
    dv_(lambda e: e.tensor_tensor(out=Oh0, in0=Oh0, in1=Rk3, op=ALU.mult), ["Oh0", rkn], ["Oh0"])
    dv_(lambda e: e.tensor_tensor(out=Oh1, in0=Oh1, in1=Rk3, op=ALU.mult), ["Oh1", rkn], ["Oh1"])
    dv_(lambda e: e.tensor_reduce(out=dfl[:, 0:32], in_=Oh0, axis=AX.X, op=ALU.add), ["Oh0"], ["dfl0"])
    dv_(lambda e: e.tensor_reduce(out=dfl[:, 32:64], in_=Oh1, axis=AX.X, op=ALU.add), ["Oh1"], ["dfl1"])
    dv_(lambda e: e.tensor_copy(out=dest0_i, in_=dfl[:, 0:32]), ["dfl0"], ["dest0_i"])
    dv_(lambda e: e.tensor_copy(out=dest1_i, in_=dfl[:, 32:64]), ["dfl1"], ["dest1_i"])
    hrow = [A.alloc("hrow", [128, D], F32) for _ in range(4)]
    for tt in range(NTT):
        b2 = tt % 4
        tsl = slice(tt * 128, (tt + 1) * 128)
        S.dma("sp", lambda e, b2=b2, tsl=tsl: e.dma_start(out=hrow[b2], in_=h_scr[tsl, :]), reads=["h_scr"], writes=["hrow%d" % b2])
        for k, di in enumerate((dest0_i, dest1_i)):
            S.dma("pool", lambda e, b2=b2, tt=tt, di=di: e.indirect_dma_start(
                out=xs_scr, out_offset=bass.IndirectOffsetOnAxis(ap=di[:, tt:tt + 1], axis=0), in_=hrow[b2], in_offset=None),
                reads=["hrow%d" % b2, "dest0_i", "dest1_i"], writes=["xs_scr%d" % k])
    thr = iot_sb[:, 44:140]
    dv_(lambda e: e.tensor_tensor(out=cmp3, in0=pend.unsqueeze(1).to_broadcast([128, NBLK, 32]), in1=thr.unsqueeze(2).to_broadcast([128, NBLK, 32]),
                                  op=ALU.is_le), [pen_n, "iot"], ["cmp3"])
    dv_(lambda e: e.tensor_reduce(out=bexp, in_=cmp3, axis=AX.X, op=ALU.add), ["cmp3"], ["bexp"])
    dv_(lambda e: e.tensor_scalar(out=bexp, in0=bexp, scalar1=31.0, scalar2=None, op0=ALU.min), ["bexp"], ["bexp"])
    chg = idxf[:, 0:NBLK]
    e2 = idxf[:, NBLK:2 * NBLK]
    dv_(lambda e: e.memset(chg[:, 0:1], 1.0), [], ["chg0"])
    dv_(lambda e: e.tensor_tensor(out=chg[:, 1:NBLK], in0=bexp[:, 1:NBLK], in1=bexp[:, 0:NBLK - 1], op=ALU.not_equal), ["bexp"], ["chg1"])
    dv_(lambda e: e.tensor_scalar(out=chg, in0=chg, scalar1=-1.0e7, scalar2=1.0e7, op0=ALU.mult, op1=ALU.add), ["chg0", "chg1"], ["chg"])
    dv_(lambda e: e.tensor_scalar(out=e2, in0=bexp, scalar1=128.0, scalar2=None, op0=ALU.mult), ["bexp"], ["e2"])
    dv_(lambda e: e.tensor_tensor(out=e2, in0=e2, in1=chg, op=ALU.add), ["e2", "chg"], ["e2"])
    dv_(lambda e: e.scalar_tensor_tensor(out=e2, in0=iot_sb[:, 0:1].to_broadcast([128, NBLK]), scalar=1.0, in1=e2, op0=ALU.mult, op1=ALU.add),
        ["e2", "iot"], ["e2"])
    dv_(lambda e: e.tensor_copy(out=idxA, in_=e2), ["e2"], ["idxA"])
    dv_(lambda e: e.tensor_scalar(out=e2, in0=e2, scalar1=1.0, scalar2=None, op0=ALU.add), ["e2", "idxA"], ["e2"])
    dv_(lambda e: e.tensor_copy(out=idxB, in_=e2), ["e2"], ["idxB"])

    if stop < 5:
        S.emit()
        return nc

    if stop < 6:
        S.emit()
        return nc
    S.barrier()
    xb = [A.alloc("xb", [128, D], BF16) for _ in range(2)]
    xTk = [A.alloc("xTk", [128, 8, 128], BF16) for _ in range(2)]
    Wg = A.alloc("Wg", [128, 8, 512], BF16)
    Wu = A.alloc("Wu", [128, 8, 512], BF16)
    Wd = A.alloc("Wd", [128, 4, 1024], BF16)
    Wgs = A.alloc("Wgs", [128, 4096], F32)
    Wus = A.alloc("Wus", [128, 4096], F32)
    Wds = A.alloc("Wds", [128, 4096], F32)
    sil = A.alloc("sil", [128, 512], F32)
    hdn = A.alloc("hdn", [128, 512], BF16)
    hdT = A.alloc("hdT", [128, 4, 128], BF16)
    yb = [A.alloc("yb", [128, D], F32) for _ in range(2)]
    breg = {}

    def bound_reg(e):
        if "r" not in breg:
            r = e.alloc_register("wbound")
            e.reg_mov(r, 32 * 128 - 1)
            breg["r"] = r
        return breg["r"]

    PTb = PS[0].bitcast(BF16)
    PGa, PUa = PS[1], PS[2]
    PTh = PS[3].bitcast(BF16)
    PY = [PS[4], PS[5]]
    def wload(b, which):
        for (wt, ws, wn, src) in which:
            wflat = wt.rearrange("p a b -> p (a b)")
            S.dma("pool", lambda e, ws=ws, src=src: e.indirect_dma_start(
                out=ws, out_offset=None, in_=src,
                in_offset=bass.IndirectOffsetOnAxis(ap=idxA[:, b:b + 1], axis=0), bounds_check=bound_reg(e), oob_is_err=False),
                reads=["idxA"], writes=[wn + "s"])
            S.op("act", lambda e, wflat=wflat, ws=ws: e.activation(out=wflat[:, 0:2048], in_=ws[:, 0:2048], func=AF.Copy),
                 reads=[wn + "s"], writes=[wn + "a"])
            S.op("dve", lambda e, wflat=wflat, ws=ws: e.tensor_copy(out=wflat[:, 2048:4096], in_=ws[:, 2048:4096]),
                 reads=[wn + "s"], writes=[wn + "b"])

    WG = ((Wg, Wgs, "Wg", w_gate),)
    WU = ((Wu, Wus, "Wu", w_up),)
    WD = ((Wd, Wds, "Wd", w_down),)

    def stA(b):
        b2 = b % 2
        rsl = slice(b * 128, (b + 1) * 128)
        S.dma("sp", lambda e: e.dma_start(out=xb[b2], in_=xs_scr[rsl, :]), reads=["xs_scr0", "xs_scr1"], writes=["xb%d" % b2])
        for c in range(8):
            S.op("pe", lambda e, c=c: e.transpose(out=PTb[:, c * 128:(c + 1) * 128], in_=xb[b2][:, c * 128:(c + 1) * 128], identity=c_bf[:, IDENT, :]),
                 reads=["xb%d" % b2, "c_bf"], writes=["PTb"])
        S.op("dve", lambda e: e.tensor_copy(out=xTk[b2].rearrange("p a b -> p (a b)"), in_=PTb), reads=["PTb"], writes=["xTk%d" % b2])

    def stG(b):
        b2 = b % 2
        for c in range(8):
            S.op("pe", lambda e, c=c: e.matmul(PGa, lhsT=xTk[b2][:, c, :], rhs=Wg[:, c, :], start=(c == 0), stop=(c == 7)),
                 reads=["xTk%d" % b2, "Wga", "Wgb"], writes=["PGa"])
        S.op("act", lambda e: e.activation(out=sil, in_=PGa, func=AF.Silu), reads=["PGa"], writes=["sil"])

    def stU(b):
        b2 = b % 2
        for c in range(8):
            S.op("pe", lambda e, c=c: e.matmul(PUa, lhsT=xTk[b2][:, c, :], rhs=Wu[:, c, :], start=(c == 0), stop=(c == 7)),
                 reads=["xTk%d" % b2, "Wua", "Wub"], writes=["PUa"])
        S.op("dve", lambda e: e.tensor_tensor(out=hdn, in0=sil, in1=PUa, op=ALU.mult), reads=["sil", "PUa"], writes=["hdn"])

    def stC1(b):
        for c in range(4):
            S.op("pe", lambda e, c=c: e.transpose(out=PTh[:, c * 128:(c + 1) * 128], in_=hdn[:, c * 128:(c + 1) * 128], identity=c_bf[:, IDENT, :]),
                 reads=["hdn", "c_bf"], writes=["PTh"])
        S.op("dve", lambda e: e.tensor_copy(out=hdT.rearrange("p a b -> p (a b)"), in_=PTh[:, 0:512]), reads=["PTh"], writes=["hdT"])

    def stC2(b):
        b2 = b % 2
        rsl = slice(b * 128, (b + 1) * 128)
        for hf in range(2):
            for c in range(4):
                S.op("pe", lambda e, hf=hf, c=c: e.matmul(PY[hf], lhsT=hdT[:, c, :], rhs=Wd[:, c, hf * 512:(hf + 1) * 512],
                                                         start=(c == 0), stop=(c == 3)), reads=["hdT", "Wda", "Wdb"], writes=["PY%d" % hf])
            evac(yb[b2][:, hf * 512:(hf + 1) * 512], PY[hf], ["PY%d" % hf], ["yb%d_%d" % (b2, hf)])
        S.dma("sp", lambda e: e.dma_start(out=ys_scr[rsl, :], in_=yb[b2]), reads=["yb%d_0" % b2, "yb%d_1" % b2], writes=["ys_scr"])

    stA(0)
    wload(0, WG)
    wload(0, WU)
    for b in range(NBLK):
        if b + 1 < NBLK:
            stA(b + 1)
        if b > 0:
            stC1(b - 1)
        stG(b)
        if b + 1 < NBLK:
            wload(b + 1, WG)
        if b > 0:
            stC2(b - 1)
        wload(b, WD)
        stU(b)
        if b + 1 < NBLK:
            wload(b + 1, WU)
    stC1(NBLK - 1)
    stC2(NBLK - 1)

    if stop < 7:
        S.emit()
        return nc
    S.barrier()
    A.reset(PBASE)
    g2b = A.alloc("g2b", [128, D], F32); b2b = A.alloc("b2b", [128, D], F32)
    NB3 = 3
    y0 = [A.alloc("y0", [128, D], F32) for _ in range(NB3)]
    y1 = [A.alloc("y1", [128, D], F32) for _ in range(NB3)]
    pr = [A.alloc("pr", [128, D], F32) for _ in range(NB3)]
    ot = [A.alloc("ot", [128, D], F32) for _ in range(2)]
    st7 = [A.alloc("st7", [128, 12], F32) for _ in range(2)]
    mv7 = [A.alloc("mv7", [128, 4], F32) for _ in range(2)]
    S.dma("sp", lambda e: e.dma_start(out=g2b, in_=ln2g.partition_broadcast(128)), writes=["g2b"])
    S.dma("sp", lambda e: e.dma_start(out=b2b, in_=ln2b.partition_broadcast(128)), writes=["b2b"])

    def ld7(tt):
        b3 = tt % NB3
        tsl = slice(tt * 128, (tt + 1) * 128)
        S.dma("sp", lambda e: e.dma_start(out=pr[b3], in_=pre_scr[tsl, :]), reads=["pre_scr"], writes=["pr%d" % b3])
        S.dma("pool", lambda e: e.indirect_dma_start(
            out=y0[b3], out_offset=None, in_=ys_scr, in_offset=bass.IndirectOffsetOnAxis(ap=dest0_i[:, tt:tt + 1], axis=0)),
            reads=["ys_scr", "dest0_i"], writes=["y0_%d" % b3])
        S.dma("pool", lambda e: e.indirect_dma_start(
            out=y1[b3], out_offset=None, in_=ys_scr, in_offset=bass.IndirectOffsetOnAxis(ap=dest1_i[:, tt:tt + 1], axis=0)),
            reads=["ys_scr", "dest1_i"], writes=["y1_%d" % b3])

    def cmb7(tt):
        b3 = tt % NB3; m2 = tt % 2
        S.op("dve", lambda e: e.scalar_tensor_tensor(out=pr[b3], in0=y0[b3], scalar=g0_all[:, tt:tt + 1], in1=pr[b3], op0=ALU.mult, op1=ALU.add),
             reads=["y0_%d" % b3, "pr%d" % b3, "g0_all"], writes=["pr%d" % b3])
        S.op("dve", lambda e: e.scalar_tensor_tensor(out=pr[b3], in0=y1[b3], scalar=g1_all[:, tt:tt + 1], in1=pr[b3], op0=ALU.mult, op1=ALU.add),
             reads=["y1_%d" % b3, "pr%d" % b3, "g1_all"], writes=["pr%d" % b3])
        for hf in range(2):
            S.op("dve", lambda e, hf=hf: e.bn_stats(out=st7[m2][:, hf * 6:(hf + 1) * 6], in_=pr[b3][:, hf * 512:(hf + 1) * 512]),
                 reads=["pr%d" % b3], writes=["st7_%d_%d" % (m2, hf)])
        S.op("dve", lambda e: e.bn_aggr(out=mv7[m2][:, 0:2], in_=st7[m2]), reads=["st7_%d_0" % m2, "st7_%d_1" % m2], writes=["mv7a%d" % m2])
        S.op("act", lambda e: e.activation(out=mv7[m2][:, 2:3], in_=mv7[m2][:, 1:2], func=AF.Ln, bias=eps_c), reads=["mv7a%d" % m2, "eps_c"], writes=["mv7b%d" % m2])
        S.op("act", lambda e: e.activation(out=mv7[m2][:, 3:4], in_=mv7[m2][:, 2:3], func=AF.Exp, scale=-0.5), reads=["mv7b%d" % m2], writes=["mv7c%d" % m2])

    def fin7(tt):
        b3 = tt % NB3; m2 = tt % 2
        tsl = slice(tt * 128, (tt + 1) * 128)
        S.op("dve", lambda e: e.tensor_scalar(out=ot[m2], in0=pr[b3], scalar1=mv7[m2][:, 0:1], scalar2=mv7[m2][:, 3:4], op0=ALU.subtract, op1=ALU.mult),
             reads=["pr%d" % b3, "mv7a%d" % m2, "mv7c%d" % m2], writes=["ot%d" % m2])
        S.op("dve", lambda e: e.tensor_tensor(out=ot[m2], in0=ot[m2], in1=g2b, op=ALU.mult), reads=["ot%d" % m2, "g2b"], writes=["ot%d" % m2])
        S.op("dve", lambda e: e.tensor_tensor(out=ot[m2], in0=ot[m2], in1=b2b, op=ALU.add), reads=["ot%d" % m2, "b2b"], writes=["ot%d" % m2])
        S.dma("sp", lambda e: e.dma_start(out=out[tsl, :], in_=ot[m2]), reads=["ot%d" % m2], writes=["out"], is_output=True)

    ld7(0)
    ld7(1)
    for tt in range(NTT):
        cmb7(tt)
        if tt > 0:
            fin7(tt - 1)
        if tt + 2 < NTT:
            ld7(tt + 2)
    fin7(NTT - 1)

    S.emit()
    return nc


_CACHE = {}


def _prep_inputs(x, p, w_in, b_forget, w_out, ln_mix_g, ln_mix_b, w_group, b_group, w_router, b_router,
                 w_gate, w_up, w_down, w_ple, w_ple_gate, ln_ffn_g, ln_ffn_b):
    f32 = np.float32
    x = np.asarray(x, f32); p = np.asarray(p, f32)
    w_in = np.asarray(w_in, f32)[0]
    kp_cols, qp_cols = [], []
    for h in range(8):
        kp_cols += list(range(512 + 64 * h, 512 + 64 * h + 64)) + list(range(2048 + 64 * h, 2048 + 64 * h + 64))
        qp_cols += list(range(0 + 64 * h, 64 * h + 64)) + list(range(1536 + 64 * h, 1536 + 64 * h + 64))
    v_cols = list(range(1024, 1536)) + list(range(2560, 3072))

    def pcl(w):
        return np.ascontiguousarray(w.reshape(8, 128, -1).transpose(1, 0, 2))

    shared = {
        "w_kp": pcl(w_in[:, kp_cols]), "w_qp": pcl(w_in[:, qp_cols]), "w_v": pcl(w_in[:, v_cols]),
        "w_f": pcl(w_in[:, 3072:3080]),
        "bf_t": np.ascontiguousarray(np.tile(np.asarray(b_forget, f32)[0], 64).reshape(1, 512)),
        "w_out": pcl(np.asarray(w_out, f32)[0]),
        "ln1g": np.asarray(ln_mix_g, f32).reshape(1, D), "ln1b": np.asarray(ln_mix_b, f32).reshape(1, D),
        "ln2g": np.asarray(ln_ffn_g, f32).reshape(1, D), "ln2b": np.asarray(ln_ffn_b, f32).reshape(1, D),
        "w_r36": pcl(np.concatenate([np.asarray(w_group, f32)[0], np.asarray(w_router, f32)[0]], axis=1)),
        "b_r36": np.ascontiguousarray(np.tile(np.concatenate([np.asarray(b_group, f32)[0], np.asarray(b_router, f32)[0]]), 32).reshape(1, 32 * 36)),
        "w_gate": np.ascontiguousarray(np.asarray(w_gate, f32)[0].reshape(32, 8, 128, 512).transpose(0, 2, 1, 3).reshape(32 * 128, 4096)),
        "w_up": np.ascontiguousarray(np.asarray(w_up, f32)[0].reshape(32, 8, 128, 512).transpose(0, 2, 1, 3).reshape(32 * 128, 4096)),
        "w_down": np.ascontiguousarray(np.asarray(w_down, f32)[0].reshape(32, 4, 128, 1024).transpose(0, 2, 1, 3).reshape(32 * 128, 4096)),
        "w_ple": np.ascontiguousarray(np.asarray(w_ple, f32)[0].reshape(2, 128, 1024).transpose(1, 0, 2)),
        "w_pg": pcl(np.asarray(w_ple_gate, f32)[0]),
    }
    k = np.arange(128)[:, None]; q = np.arange(128)[None, :]
    cst = np.zeros((128, 7, 128), f32)
    cst[:, 6, :] = -1.0
    cst[:, 0, :] = (k == q)
    cst[:, 1, :] = (k <= q)
    cst[:, 2, :] = (k < q)
    cst[:, 3, :] = 1.0
    cst[:, 4, :] = -(k >= q).astype(f32)
    cst[:, 5, :] = -(k == q).astype(f32)
    shared["consts"] = cst
    iot = np.zeros((128, 140), f32)
    iot[:, 0:12] = np.arange(12)[None, :] * 128 + np.arange(128)[:, None]
    iot[:, 44:140] = np.arange(96)[None, :] * 128.0
    shared["iot"] = iot

    def diag_tiles(strict):
        t = np.zeros((4, 128, 512), f32)
        for i in range(4):
            for jq in range(4):
                blk = t[i, :, jq * 128:(jq + 1) * 128]
                if jq < i:
                    blk[:] = BIG
                elif jq == i:
                    blk[:] = np.where((k < q) if strict else (k <= q), 0.0, BIG)
        return t

    in_maps = []
    for c in range(NCORE):
        b, par = c // 2, c % 2
        G = G_PAR[par]
        loc = np.concatenate([np.arange(g * 512, (g + 1) * 512) for g in G])
        xT = np.ascontiguousarray(x[b].reshape(SEQ, 8, 128).transpose(2, 1, 0))
        m = dict(shared)
        m["xT_all"] = xT
        m["xT_loc"] = np.ascontiguousarray(xT[:, :, loc])
        m["x_loc"] = np.ascontiguousarray(x[b][loc])
        m["pT_loc"] = np.ascontiguousarray(p[0, b][loc].reshape(NLOC, 2, 128).transpose(2, 1, 0))
        mk = np.zeros((2, 2, 8, 128, 512), f32)
        for kind in range(2):
            dt_ = diag_tiles(strict=(kind == 0))
            for sp_ in range(2):
                has_max = (sp_ == 0) if par == 1 else (sp_ == 1)
                if has_max:
                    mk[kind, sp_, 4:8] = dt_
                else:
                    mk[kind, sp_, 0:4] = dt_
                    mk[kind, sp_, 4:8] = BIG
        m["masks"] = np.ascontiguousarray(mk.reshape(32, 128, 512).transpose(1, 0, 2))
        sel = np.zeros((8, 16, 8), f32)
        for s_, g in enumerate(G):
            sel[s_, g, :] = 1.0
        m["selx"] = sel.reshape(1, 1024)
        in_maps.append(m)
    return in_maps


def kernel(**inputs):
    if "nc" not in _CACHE:
        _CACHE["nc"] = build()
    nc = _CACHE["nc"]
    in_maps = _prep_inputs(**inputs)
    res = run_bass_kernel_spmd(nc, in_maps, core_ids=list(range(NCORE)))
    outp = np.zeros((NB, SEQ, D), np.float32)
    for c in range(NCORE):
        b, par = c // 2, c % 2
        loc = np.concatenate([np.arange(g * 512, (g + 1) * 512) for g in G_PAR[par]])
        outp[b, loc] = res.results[c]["out"]
    return outp
```

```python
import contextlib
import os
import numpy as np
import concourse.bass as bass
import concourse.mybir as mybir
from concourse.bass_utils import run_bass_kernel_spmd

F32 = mybir.dt.float32
BF16 = mybir.dt.bfloat16
I32 = mybir.dt.int32
AF = mybir.ActivationFunctionType
ALU = mybir.AluOpType
AX = mybir.AxisListType

D = 1024
SEQ = 8192
NB = 4
NCORE = 8
NLOC = 4096
NTT = 32
NBLK = 96
CAP = NBLK * 128
ALPHA = 2 ** 0.25
LN_EPS = 1e-5
BIG = -30000.0
G_PAR = ([0, 3, 4, 7, 8, 11, 12, 15], [1, 2, 5, 6, 9, 10, 13, 14])


class Sched:
    ENGS = ("pe", "act", "dve", "pool", "sp")

    def __init__(self, nc, n_dma_sems=10):
        self.nc = nc
        self.ops = {e: [] for e in self.ENGS}
        self.cnt = {e: 0 for e in self.ENGS}
        self.last_w = {}
        self.readers = {}
        self.seen = {e: {} for e in self.ENGS}
        self.n_dma_sems = n_dma_sems
        self.dma_used = {}
        self.dma_rr = {"sp": 0, "pool": 0, "act": 0}
        self.out_tokens = []

    def _deps(self, eng, reads, writes):
        deps = {}

        def add(k, v):
            if deps.get(k, 0) < v:
                deps[k] = v

        for k in reads:
            t = self.last_w.get(k)
            if t:
                add(*t)
        for k in writes:
            t = self.last_w.get(k)
            if t:
                add(*t)
            for kk, vv in self.readers.get(k, {}).items():
                add(kk, vv)
        waits = []
        for k, v in deps.items():
            if k == "pe" and eng == "pe":
                continue
            if self.seen[eng].get(k, 0) >= v:
                continue
            self.seen[eng][k] = v
            waits.append((k, v))
        return waits

    def _commit(self, tok, reads, writes):
        for k in reads:
            r = self.readers.setdefault(k, {})
            if r.get(tok[0], 0) < tok[1]:
                r[tok[0]] = tok[1]
        for k in writes:
            self.last_w[k] = tok
            self.readers[k] = {}

    def op(self, eng, fn, reads=(), writes=()):
        waits = self._deps(eng, reads, writes)
        self.cnt[eng] += 1
        tok = (eng, self.cnt[eng])
        self.ops[eng].append((fn, waits, (eng, 1)))
        self._commit(tok, reads, writes)
        return tok

    def dma(self, q, fn, reads=(), writes=(), is_output=False):
        waits = self._deps(q, reads, writes)
        slot = self.dma_rr[q] % self.n_dma_sems
        self.dma_rr[q] += 1
        key = "dma_%s_%d" % (q, slot)
        used = self.dma_used.get(key, 0)
        if used > 0 and self.seen[q].get(key, 0) < 16 * used:
            self.seen[q][key] = 16 * used
            waits.append((key, 16 * used))
        self.dma_used[key] = used + 1
        tok = (key, 16 * (used + 1))
        self.ops[q].append((fn, waits, (key, 16)))
        self._commit(tok, reads, writes)
        if is_output:
            self.out_tokens.append(tok)
        return tok

    def barrier(self):
        toks = [(e, self.cnt[e]) for e in self.ENGS if self.cnt[e] > 0]
        toks += [(k, 16 * u) for k, u in self.dma_used.items()]
        for e in self.ENGS:
            waits = []
            for k, v in toks:
                if k == e and e == "pe":
                    continue
                if self.seen[e].get(k, 0) >= v:
                    continue
                self.seen[e][k] = v
                waits.append((k, v))
            if waits:
                self.ops[e].append((None, waits, None))

    def emit(self):
        nc = self.nc
        fin = []
        for e in self.ENGS:
            if self.cnt[e] > 0 and self.seen["sp"].get(e, 0) < self.cnt[e]:
                fin.append((e, self.cnt[e]))
        for k, u in self.dma_used.items():
            if self.seen["sp"].get(k, 0) < 16 * u:
                fin.append((k, 16 * u))
        self.ops["sp"].append((None, fin, None))
        keys = set()
        for e in self.ENGS:
            for fn, waits, inc in self.ops[e]:
                for k, v in waits:
                    keys.add(k)
                if inc is not None:
                    keys.add(inc[0])
        with contextlib.ExitStack() as st:
            sems = {}
            for k in sorted(keys):
                sems[k] = st.enter_context(nc.semaphore("s_" + k))
            block = st.enter_context(nc.Block())

            def run(engname):
                def body(eng):
                    for fn, waits, inc in self.ops[engname]:
                        for k, v in waits:
                            eng.wait_ge(sems[k], v)
                        if fn is not None:
                            ins = fn(eng)
                            ins.then_inc(sems[inc[0]], inc[1])
                return body

            block.tensor(run("pe"))
            block.scalar(run("act"))
            block.vector(run("dve"))
            block.gpsimd(run("pool"))
            block.sync(run("sp"))


class Arena:
    def __init__(self, nc, limit=229376 - 256):
        self.nc = nc
        self.off = 16640
        self.limit = limit
        self.n = 0

    def alloc(self, name, shape, dtype):
        esz = {F32: 4, BF16: 2, I32: 4}[dtype]
        per = esz
        for s in shape[1:]:
            per *= s
        self.off = (self.off + 63) // 64 * 64
        self.n += 1
        t = self.nc.alloc_sbuf_tensor_at("%s_%d" % (name, self.n), list(shape), dtype, offset=self.off)
        self.off += per
        assert self.off <= self.limit, (name, self.off)
        return t.ap()

    def mark(self):
        return self.off

    def reset(self, m):
        self.off = m


def build(debug=False, stop=99):
    nc = bass.Bass("TRN2", target_bir_lowering=False)
    S = Sched(nc)
    A = Arena(nc)

    def din(name, shape, dt=F32):
        return nc.dram_tensor(name, list(shape), dt, kind="ExternalInput").ap()

    def dscr(name, shape, dt, dbg=False):
        return nc.dram_tensor(name, list(shape), dt, kind=("ExternalOutput" if (dbg and debug) else "Internal")).ap()

    xT_all = din("xT_all", [128, 8, SEQ])
    xT_loc = din("xT_loc", [128, 8, NLOC])
    x_loc = din("x_loc", [NLOC, D])
    pT_loc = din("pT_loc", [128, 2, NLOC])
    w_kp = din("w_kp", [128, 8, 1024])
    w_qp = din("w_qp", [128, 8, 1024])
    w_v = din("w_v", [128, 8, 1024])
    w_f = din("w_f", [128, 8, 8])
    bf_t = din("bf_t", [1, 512])
    w_out = din("w_out", [128, 8, 1024])
    ln1g = din("ln1g", [1, D]); ln1b = din("ln1b", [1, D])
    ln2g = din("ln2g", [1, D]); ln2b = din("ln2b", [1, D])
    w_r36 = din("w_r36", [128, 8, 36])
    b_r36 = din("b_r36", [1, 32 * 36])
    w_gate = din("w_gate", [32 * 128, 4096])
    w_up = din("w_up", [32 * 128, 4096])
    w_down = din("w_down", [32 * 128, 4096])
    w_ple = din("w_ple", [128, 2, 1024])
    w_pg = din("w_pg", [128, 8, 1024])
    masks = din("masks", [128, 32, 512])
    selx = din("selx", [1, 1024])
    consts = din("consts", [128, 7, 128])
    iot = din("iot", [128, 12 + 32 + 96], F32)
    out = nc.dram_tensor("out", [NLOC, D], F32, kind="ExternalOutput").ap()

    kt_scr = dscr("kt_scr", [8, 128, SEQ], BF16)
    v_scr = dscr("v_scr", [8, 128, 64 * 128], BF16)
    qt_scr = dscr("qt_scr", [8, 128, NLOC], BF16)
    h_scr = dscr("h_scr", [NLOC, D], F32, dbg=True)
    pre_scr = dscr("pre_scr", [NLOC, D], F32, dbg=True)
    xs_scr = dscr("xs_scr", [CAP, D], BF16)
    ys_scr = dscr("ys_scr", [CAP, D], F32)
    dbg_r = dscr("dbg_r", [128, 4096], F32, dbg=True)

    PSALL = nc.alloc_psum_tensor("psall", [128, 4096], F32).ap()
    PS = [PSALL[:, i * 512:(i + 1) * 512] for i in range(8)]

    c_f32 = A.alloc("c_f32", [128, 7, 128], F32)
    c_bf = A.alloc("c_bf", [128, 7, 128], BF16)
    IDENT, TRII, STRICT, ONES, NEGTRI, NEGID, NEGONE = range(7)
    cfpos = A.alloc("cfpos", [128, 512], F32)
    cref = A.alloc("cref", [128, 64], F32)
    L_all = A.alloc("L_all", [128, 32 * 36], F32)
    g0_all = A.alloc("g0_all", [128, 32], F32)
    g1_all = A.alloc("g1_all", [128, 32], F32)
    dest0_i = A.alloc("dest0_i", [128, 32], I32)
    dest1_i = A.alloc("dest1_i", [128, 32], I32)
    idxA = A.alloc("idxA", [128, NBLK], I32)
    idxB = A.alloc("idxB", [128, NBLK], I32)
    iot_sb = A.alloc("iot_sb", [128, 140], F32)
    eps_c = A.alloc("eps_c", [128, 1], F32)
    st6 = A.alloc("st6", [128, 12], F32)
    mv = A.alloc("mv", [128, 4], F32)
    PBASE = A.mark()

    S.dma("sp", lambda e: e.dma_start(out=c_f32, in_=consts), writes=["c_f32"])
    S.dma("pool", lambda e: e.dma_start(out=c_bf, in_=consts), writes=["c_bf"])
    S.dma("sp", lambda e: e.dma_start(out=iot_sb, in_=iot), writes=["iot"])
    S.op("dve", lambda e: e.memset(eps_c, LN_EPS), writes=["eps_c"])

    rr = {"ev": 0}

    def evac(out_ap, in_ap, reads, writes, scale=None):
        rr["ev"] += 1
        if rr["ev"] % 2 == 0:
            if scale is None:
                S.op("act", lambda e: e.activation(out=out_ap, in_=in_ap, func=AF.Copy), reads=reads, writes=writes)
            else:
                S.op("act", lambda e: e.activation(out=out_ap, in_=in_ap, func=AF.Copy, scale=scale), reads=reads, writes=writes)
        else:
            if scale is None:
                S.op("dve", lambda e: e.tensor_copy(out=out_ap, in_=in_ap), reads=reads, writes=writes)
            else:
                S.op("dve", lambda e: e.tensor_scalar(out=out_ap, in0=in_ap, scalar1=scale, scalar2=None, op0=ALU.mult),
                     reads=reads, writes=writes)

    Wkp = A.alloc("Wkp", [128, 8, 1024], BF16)
    Wqp = A.alloc("Wqp", [128, 8, 1024], BF16)
    Wv = A.alloc("Wv", [128, 8, 1024], BF16)
    Wf = A.alloc("Wf", [128, 8, 8], F32)
    bF = A.alloc("bF", [128, 512], F32)
    selx_b = A.alloc("selx_b", [128, 1024], F32)
    xTb = [A.alloc("xTb", [128, 8, 512], BF16) for _ in range(2)]
    xTf = [A.alloc("xTf", [128, 8, 512], F32) for _ in range(2)]
    KTst = [A.alloc("KTst", [128, 8, 512], BF16) for _ in range(2)]
    Vst = [A.alloc("Vst", [128, 8, 512], BF16) for _ in range(2)]

    for j in range(8):
        S.dma("pool", lambda e, j=j: e.dma_start(out=Wkp[:, j, :], in_=w_kp[:, j, :]), writes=["Wkp"])
        S.dma("pool", lambda e, j=j: e.dma_start(out=Wv[:, j, :], in_=w_v[:, j, :]), writes=["Wv"])
        S.dma("pool", lambda e, j=j: e.dma_start(out=Wqp[:, j, :], in_=w_qp[:, j, :]), writes=["Wqp"])
    mask_bf = nc.alloc_sbuf_tensor_at("mask_bf_fix", [128, 32, 512], BF16, offset=194560).ap()
    S.dma("sp", lambda e: e.dma_start(out=Wf, in_=w_f), writes=["Wf"])
    S.dma("sp", lambda e: e.dma_start(out=bF, in_=bf_t.partition_broadcast(128)), writes=["bF"])
    S.dma("sp", lambda e: e.dma_start(out=selx_b, in_=selx.partition_broadcast(128)), writes=["selx_b"])

    PF = PS[7]
    bank = {"i": 0}

    def nxt_bank(n=7):
        bank["i"] = (bank["i"] + 1) % n
        return bank["i"]

    for g in range(16):
        b2 = g % 2
        for c in range(8):
            S.dma("pool", lambda e, g=g, b2=b2, c=c: e.dma_start(out=xTb[b2][:, c, :], in_=xT_all[:, c, g * 512:(g + 1) * 512]),
                  writes=["xTb%d" % b2])
        S.dma("sp", lambda e, g=g, b2=b2: e.dma_start(out=xTf[b2], in_=xT_all[:, :, g * 512:(g + 1) * 512]),
              writes=["xTf%d" % b2])
        for hp in range(8):
            bi = nxt_bank()
            for c in range(8):
                S.op("pe", lambda e, bi=bi, hp=hp, c=c, b2=b2: e.matmul(PS[bi], lhsT=Wkp[:, c, hp * 128:(hp + 1) * 128],
                                                                       rhs=xTb[b2][:, c, :], start=(c == 0), stop=(c == 7)),
                     reads=["Wkp", "xTb%d" % b2], writes=["ps%d" % bi])
            evac(KTst[b2][:, hp, :], PS[bi], ["ps%d" % bi], ["KTst%d" % b2])
        S.dma("sp", lambda e, g=g, b2=b2: e.dma_start(out=kt_scr[:, :, g * 512:(g + 1) * 512].rearrange("h p t -> p h t"),
                                                      in_=KTst[b2]),
              reads=["KTst%d" % b2], writes=["kt_scr"])
        for j in range(4):
            for half in range(2):
                bi = nxt_bank()
                for c in range(8):
                    S.op("pe", lambda e, bi=bi, j=j, half=half, c=c, b2=b2: e.matmul(
                        PS[bi], lhsT=xTb[b2][:, c, j * 128:(j + 1) * 128], rhs=Wv[:, c, half * 512:(half + 1) * 512],
                        start=(c == 0), stop=(c == 7)), reads=["Wv", "xTb%d" % b2], writes=["ps%d" % bi])
                dst = Vst[b2].rearrange("p h (j t e) -> p h j t e", j=4, t=2, e=64)[:, :, j, half, :]
                evac(dst, PS[bi].rearrange("p (h e) -> p h e", h=8, e=64), ["ps%d" % bi], ["Vst%d" % b2])
        S.dma("sp", lambda e, g=g, b2=b2: e.dma_start(
            out=v_scr[:, :, g * 512:(g + 1) * 512].rearrange("h p t -> p h t"), in_=Vst[b2]),
            reads=["Vst%d" % b2], writes=["v_scr"])
        for j in range(4):
            kb = 4 * g + j
            for c in range(8):
                S.op("pe", lambda e, kb=kb, j=j, c=c, b2=b2: e.matmul(PF[:, kb * 8:(kb + 1) * 8], lhsT=xTf[b2][:, c, j * 128:(j + 1) * 128],
                                                                     rhs=Wf[:, c, :], start=(c == 0), stop=(c == 7)),
                     reads=["Wf", "xTf%d" % b2], writes=["PF"])
    for j in range(32):
        S.dma("pool", lambda e, j=j: e.dma_start(out=mask_bf[:, j, :], in_=masks[:, j, :]), writes=["mask_bf"])
    m1 = A.mark()
    lf = A.alloc("lf", [128, 512], F32)
    lfn = A.alloc("lfn", [128, 512], F32)
    Ta = A.alloc("Ta", [128, 512], F32)
    Tb = A.alloc("Tb", [128, 512], F32)
    T0 = A.alloc("T0", [128, 512], F32)
    prod = A.alloc("prod", [128, 128], F32)
    S.op("dve", lambda e: e.tensor_tensor(out=lf, in0=PF, in1=bF, op=ALU.add), reads=["PF", "bF"], writes=["lf"])
    S.op("act", lambda e: e.activation(out=lfn, in_=lf, func=AF.Exp, scale=-1.0), reads=["lf"], writes=["lfn"])
    S.op("act", lambda e: e.activation(out=lf, in_=lfn, func=AF.Ln, bias=1.0), reads=["lfn"], writes=["lf"])
    S.op("pe", lambda e: e.matmul(PS[5], lhsT=c_f32[:, TRII, :], rhs=lf, start=True, stop=True), reads=["c_f32", "lf"], writes=["ps5"])
    S.op("pe", lambda e: e.matmul(PS[6], lhsT=c_f32[:, ONES, :], rhs=lf, start=True, stop=True), reads=["c_f32", "lf"], writes=["ps6"])
    S.op("dve", lambda e: e.tensor_copy(out=T0, in_=PS[6]), reads=["ps6"], writes=["T0"])
    S.op("dve", lambda e: e.tensor_copy(out=Ta, in_=PS[6]), reads=["ps6"], writes=["Ta"])
    cur, oth, cn, on = Ta, Tb, "Ta", "Tb"
    for dsh in (1, 2, 4, 8, 16, 32):
        w_ = 8 * dsh
        S.op("dve", lambda e, cur=cur, oth=oth, w_=w_: e.tensor_tensor(out=oth[:, w_:], in0=cur[:, w_:], in1=cur[:, :512 - w_], op=ALU.add),
             reads=[cn], writes=[on])
        S.op("dve", lambda e, cur=cur, oth=oth, w_=w_: e.tensor_copy(out=oth[:, :w_], in_=cur[:, :w_]), reads=[cn, on], writes=[on])
        cur, oth, cn, on = oth, cur, on, cn
    Tincl, tin = cur, cn
    S.op("dve", lambda e: e.tensor_tensor(out=lfn, in0=Tincl, in1=T0, op=ALU.subtract), reads=[tin, "T0"], writes=["lfn"])
    S.op("dve", lambda e: e.tensor_tensor(out=cfpos, in0=PS[5], in1=lfn, op=ALU.add), reads=["ps5", "lfn"], writes=["cfpos"])
    Tlast = Tincl.rearrange("p (g j h) -> p g j h", g=16, j=4, h=8)[:, :, 3, :]
    for s in range(8):
        S.op("dve", lambda e, s=s: e.tensor_tensor(out=prod.rearrange("p (h g) -> p g h", h=8, g=16), in0=Tlast,
                                                  in1=selx_b[:, s * 128:(s + 1) * 128].rearrange("p (g h) -> p g h", g=16, h=8),
                                                  op=ALU.mult), reads=[tin, "selx_b"], writes=["prod"])
        S.op("dve", lambda e, s=s: e.tensor_reduce(out=cref[:, s * 8:(s + 1) * 8], in_=prod.rearrange("p (h g) -> p h g", h=8, g=16),
                                                  axis=AX.X, op=ALU.add), reads=["prod"], writes=["cref"])

    for s in range(8):
        b2 = s % 2
        S.dma("sp", lambda e, s=s, b2=b2: e.dma_start(out=xTf[b2], in_=xT_loc[:, :, s * 512:(s + 1) * 512]),
              writes=["xTf%d" % b2])
        S.op("act", lambda e, b2=b2: e.activation(out=xTb[b2][:, 0:4, :], in_=xTf[b2][:, 0:4, :], func=AF.Copy),
             reads=["xTf%d" % b2], writes=["xTb%d" % b2])
        S.op("dve", lambda e, b2=b2: e.tensor_copy(out=xTb[b2][:, 4:8, :], in_=xTf[b2][:, 4:8, :]),
             reads=["xTf%d" % b2, "xTb%d" % b2], writes=["xTb%d" % b2])
        for hp in range(8):
            bi = nxt_bank(5)
            for c in range(8):
                S.op("pe", lambda e, bi=bi, hp=hp, c=c, b2=b2: e.matmul(PS[bi], lhsT=Wqp[:, c, hp * 128:(hp + 1) * 128],
                                                                       rhs=xTb[b2][:, c, :], start=(c == 0), stop=(c == 7)),
                     reads=["Wqp", "xTb%d" % b2], writes=["ps%d" % bi])
            evac(KTst[b2][:, hp, :], PS[bi], ["ps%d" % bi], ["KTst%d" % b2], scale=0.125)
        S.dma("sp", lambda e, s=s, b2=b2: e.dma_start(out=qt_scr[:, :, s * 512:(s + 1) * 512].rearrange("h p t -> p h t"),
                                                      in_=KTst[b2]),
              reads=["KTst%d" % b2], writes=["qt_scr"])

    if stop < 2:
        S.emit()
        return nc
    S.barrier()
    A.reset(PBASE)
    mixT = A.alloc("mixT", [128, 8, NLOC], BF16)
    M3 = A.mark()
    KT = A.alloc("KT", [128, SEQ], BF16)
    Vt = A.alloc("Vt", [128, 64 * 192], BF16)
    QT = A.alloc("QT", [128, NLOC], BF16)
    Et2 = [A.alloc("Et2", [128, 1024], F32) for _ in range(2)]
    SP2 = [A.alloc("SP2", [128, 1024], BF16) for _ in range(2)]
    Wt2 = [A.alloc("Wt2", [128, 1024], BF16) for _ in range(2)]
    Ybf = [A.alloc("Ybf", [128, 512], BF16) for _ in range(2)]
    Pt = [A.alloc("Pt", [128, 512], BF16) for _ in range(2)]
    biasF = [A.alloc("biasF", [128, 64], F32) for _ in range(2)]
    rinv = A.alloc("rinv", [128, 512], F32)
    XSA = [PSALL[:, 0:1024], PSALL[:, 1024:2048]]
    XF1 = PS[4]
    YB, OB, OFB = PS[5], PS[6], PS[7]
    Vt4 = Vt.rearrange("p (k t e) -> p k t e", k=64, t=3, e=64)
    S.op("dve", lambda e: e.memset(Vt4[:, :, 2, :], 1.0), writes=["Vones"])
    cf3 = cfpos.rearrange("p (k h) -> p k h", k=64, h=8)

    tiles = []
    for hp in range(8):
        for s in range(8):
            nkb = 8 * s + 8
            for kb in range(nkb - 1, -1, -1):
                tiles.append(dict(hp=hp, s=s, kb=kb, first=(kb == nkb - 1), last=(kb == 0), j=kb - 8 * s,
                                  newhp=(s == 0 and kb == nkb - 1), news=(kb == nkb - 1)))
    NT = len(tiles)
    NP = NT // 2

    def st_load(t):
        hp = t["hp"]
        S.dma("sp", lambda e: e.dma_start(out=KT, in_=kt_scr[hp]), reads=["kt_scr"], writes=["KT"])
        S.dma("sp", lambda e: e.dma_start(out=QT, in_=qt_scr[hp]), reads=["qt_scr"], writes=["QT"])
        S.dma("sp", lambda e: e.dma_start(out=Vt4[:, :, 0:2, :], in_=v_scr[hp].rearrange("p (k t e) -> p k t e", k=64, t=2, e=64)),
              reads=["v_scr"], writes=["Vt"])

    def st_bias(t):
        hp, s = t["hp"], t["s"]
        nkb = 8 * s + 8
        bb = s % 2
        S.op("dve", lambda e: e.tensor_scalar(out=biasF[bb][:, 0:nkb], in0=cf3[:, 0:nkb, hp], scalar1=cref[:, s * 8 + hp:s * 8 + hp + 1],
                                              scalar2=None, op0=ALU.subtract), reads=["cfpos", "cref"], writes=["biasF%d" % bb])

    def s1p(n):
        for h in range(2):
            t = tiles[2 * n + h]
            hp, s, kb, j = t["hp"], t["s"], t["kb"], t["j"]
            if t["newhp"]:
                st_load(t)
            if t["news"]:
                st_bias(t)
            xs = XSA[n % 2][:, h * 512:(h + 1) * 512]
            key = "XS%d_%d" % (n % 2, h)
            S.op("pe", lambda e, xs=xs, kb=kb, s=s, j=j: e.matmul(xs, lhsT=KT[0:64, kb * 128:(kb + 1) * 128], rhs=QT[0:64, s * 512:(s + 1) * 512],
                                                                 start=True, stop=(j < 0)), reads=["KT", "QT"], writes=[key])
            if j >= 0:
                mi = (0 * 2 + s % 2) * 8 + j
                S.op("pe", lambda e, xs=xs, mi=mi: e.matmul(xs, lhsT=c_bf[:, IDENT, :], rhs=mask_bf[:, mi, :], start=False, stop=True),
                     reads=["c_bf", "mask_bf"], writes=[key])

    def f1(n, h):
        t = tiles[2 * n + h]
        hp, s, kb, j = t["hp"], t["s"], t["kb"], t["j"]
        S.op("pe", lambda e: e.matmul(XF1, lhsT=KT[64:128, kb * 128:(kb + 1) * 128], rhs=QT[64:128, s * 512:(s + 1) * 512],
                                      start=True, stop=(j < 0)), reads=["KT", "QT"], writes=["XF"])
        if j >= 0:
            mi = (1 * 2 + s % 2) * 8 + j
            S.op("pe", lambda e: e.matmul(XF1, lhsT=c_bf[:, IDENT, :], rhs=mask_bf[:, mi, :], start=False, stop=True),
                 reads=["c_bf", "mask_bf"], writes=["XF"])

    def s2p(n):
        q = n % 2
        S.op("act", lambda e: e.activation(out=Et2[q], in_=XSA[q], func=AF.Exp), reads=["XS%d_0" % q, "XS%d_1" % q], writes=["Et2_%d" % q])

    def s3p(n):
        q = n % 2
        S.op("act", lambda e: e.activation(out=SP2[q], in_=Et2[q], func=AF.Ln, bias=1.0), reads=["Et2_%d" % q], writes=["SP2_%d" % q])

    def s4p(n):
        ta, tb = tiles[2 * n], tiles[2 * n + 1]
        q = n % 2
        xa = XSA[q][:, 0:512]; xb_ = XSA[q][:, 512:1024]
        spa = SP2[q][:, 0:512]; spb = SP2[q][:, 512:1024]
        ka, kb_ = "XS%d_0" % q, "XS%d_1" % q
        spk = "SP2_%d" % q
        yprev = Ybf[1 - q]; ypk = "Ybf%d" % (1 - q)
        S.op("pe", lambda e: e.matmul(xa, lhsT=c_bf[:, NEGTRI, :], rhs=spa, start=False, stop=True, skip_group_check=True), reads=["c_bf", spk], writes=[ka])
        if not ta["first"]:
            S.op("pe", lambda e: e.matmul(xa, lhsT=c_bf[:, NEGID, :], rhs=yprev, start=False, stop=True, skip_group_check=True), reads=["c_bf", ypk], writes=[ka])
        S.op("pe", lambda e: e.matmul(xb_, lhsT=c_bf[:, NEGTRI, :], rhs=spb, start=False, stop=True, skip_group_check=True), reads=["c_bf", spk], writes=[kb_])
        if not ta["first"]:
            S.op("pe", lambda e: e.matmul(xb_, lhsT=c_bf[:, NEGID, :], rhs=yprev, start=False, stop=True, skip_group_check=True), reads=["c_bf", ypk], writes=[kb_])
        S.op("pe", lambda e: e.matmul(xb_, lhsT=c_bf[:, NEGONE, :], rhs=spa, start=False, stop=True, skip_group_check=True), reads=["c_bf", spk], writes=[kb_])
        S.op("pe", lambda e: e.matmul(YB, lhsT=c_bf[:, ONES, :], rhs=spa, start=ta["first"], stop=True, skip_group_check=(not ta["first"])), reads=["c_bf", spk], writes=["Y"])
        S.op("pe", lambda e: e.matmul(YB, lhsT=c_bf[:, ONES, :], rhs=spb, start=False, stop=True, skip_group_check=True), reads=["c_bf", spk], writes=["Y"])
        if not tb["last"]:
            S.op("dve", lambda e: e.tensor_copy(out=Ybf[q], in_=YB), reads=["Y"], writes=["Ybf%d" % q])

    def s6p(n):
        q = n % 2
        S.op("act", lambda e: e.activation(out=Wt2[q], in_=XSA[q], func=AF.Exp), reads=["XS%d_0" % q, "XS%d_1" % q], writes=["Wt2_%d" % q])

    def s7(n, h):
        t = tiles[2 * n + h]
        q = n % 2
        hp, s, kb = t["hp"], t["s"], t["kb"]
        S.op("pe", lambda e: e.matmul(OB[0:64, :], lhsT=Vt4[:, kb, 0, :], rhs=Wt2[q][:, h * 512:(h + 1) * 512], start=t["first"], stop=t["last"]),
             reads=["Vt", "Wt2_%d" % q], writes=["O"])
        if t["last"]:
            po = (hp % 2) * 64
            S.op("dve", lambda e: e.tensor_copy(out=mixT[po:po + 64, hp // 2, s * 512:(s + 1) * 512], in_=OB[0:64, :]),
                 reads=["O"], writes=["mixT"])

    def f2(n, h):
        t = tiles[2 * n + h]
        bb = t["s"] % 2; kb = t["kb"]
        S.op("act", lambda e: e.activation(out=Pt[h], in_=XF1, func=AF.Exp, bias=biasF[bb][:, kb:kb + 1]),
             reads=["XF", "biasF%d" % bb], writes=["Pt%d" % h])

    def f3(n, h):
        t = tiles[2 * n + h]
        hp, s, kb = t["hp"], t["s"], t["kb"]
        S.op("pe", lambda e: e.matmul(OFB, lhsT=Vt4[:, kb, 1:3, :].rearrange("p a b -> p (a b)"), rhs=Pt[h], start=t["first"], stop=t["last"]),
             reads=["Vt", "Vones", "Pt%d" % h], writes=["OF"])
        if t["last"]:
            po = (hp % 2) * 64
            S.op("dve", lambda e: e.reciprocal(out=rinv[0:64, :], in_=OFB[64:128, :]), reads=["OF"], writes=["rinv"])
            S.op("dve", lambda e: e.tensor_tensor(out=mixT[po:po + 64, 4 + hp // 2, s * 512:(s + 1) * 512], in0=OFB[0:64, :],
                                                  in1=rinv[0:64, :], op=ALU.mult), reads=["OF", "rinv"], writes=["mixT"])

    s1p(0)
    f1(0, 0)
    flushed = True
    for n in range(NP):
        boundary = (n + 1 < NP) and tiles[2 * (n + 1)]["newhp"]
        s2p(n)
        if not flushed:
            s6p(n - 1)
            s7(n - 1, 0)
            s7(n - 1, 1)
        flushed = False
        if n + 1 < NP and not boundary:
            s1p(n + 1)
        f2(n, 0)
        f1(n, 1)
        s3p(n)
        s4p(n)
        f3(n, 0)
        f2(n, 1)
        f3(n, 1)
        if n + 1 < NP and not boundary:
            f1(n + 1, 0)
        if boundary or n + 1 == NP:
            s6p(n)
            s7(n, 0)
            s7(n, 1)
            flushed = True
            if boundary:
                s1p(n + 1)
                f1(n + 1, 0)

    if stop < 3:
        S.emit()
        return nc
    S.barrier()
    A.reset(M3)
    Wout = A.alloc("Wout", [128, 8, 1024], BF16)
    Wpg = A.alloc("Wpg", [128, 8, 1024], BF16)
    Wple = A.alloc("Wple", [128, 2, 1024], BF16)
    Wr = A.alloc("Wr", [128, 8, 36], F32)
    g1b = A.alloc("g1b", [128, D], F32); b1b = A.alloc("b1b", [128, D], F32)
    b36 = A.alloc("b36", [128, 32 * 36], F32)
    xt = [A.alloc("xt", [128, D], F32) for _ in range(2)]
    pTb = [A.alloc("pTb", [128, 2, 128], BF16) for _ in range(2)]
    rt = A.alloc("rt", [128, D], F32)
    ht = [A.alloc("ht", [128, D], F32) for _ in range(2)]
    hhi = A.alloc("hhi", [128, D], BF16)
    hlo = A.alloc("hlo", [128, D], BF16)
    hTlo = A.alloc("hTlo", [128, 8, 128], BF16)
    Wrh = A.alloc("Wrh", [128, 8, 36], BF16)
    Wrl = A.alloc("Wrl", [128, 8, 36], BF16)
    hTb = A.alloc("hTb", [128, 8, 128], BF16)
    sg = A.alloc("sg", [128, D], F32)
    pre = [A.alloc("pre", [128, D], F32) for _ in range(2)]
    for j in range(8):
        S.dma("pool", lambda e, j=j: e.dma_start(out=Wout[:, j, :], in_=w_out[:, j, :]), writes=["Wout"])
        S.dma("pool", lambda e, j=j: e.dma_start(out=Wpg[:, j, :], in_=w_pg[:, j, :]), writes=["Wpg"])
    for j in range(2):
        S.dma("pool", lambda e, j=j: e.dma_start(out=Wple[:, j, :], in_=w_ple[:, j, :]), writes=["Wple"])
    S.dma("sp", lambda e: e.dma_start(out=Wr, in_=w_r36), writes=["Wr"])
    S.dma("sp", lambda e: e.dma_start(out=g1b, in_=ln1g.partition_broadcast(128)), writes=["g1b"])
    S.dma("sp", lambda e: e.dma_start(out=b1b, in_=ln1b.partition_broadcast(128)), writes=["b1b"])
    S.dma("sp", lambda e: e.dma_start(out=b36, in_=b_r36.partition_broadcast(128)), writes=["b36"])

    def layer_norm(src, dst, gb, bb, rk, wk, gk):
        for hf in range(2):
            S.op("dve", lambda e, hf=hf: e.bn_stats(out=st6[:, hf * 6:(hf + 1) * 6], in_=src[:, hf * 512:(hf + 1) * 512]),
                 reads=[rk], writes=["st6_%d" % hf])
        S.op("dve", lambda e: e.bn_aggr(out=mv[:, 0:2], in_=st6), reads=["st6_0", "st6_1"], writes=["mv"])
        S.op("act", lambda e: e.activation(out=mv[:, 2:3], in_=mv[:, 1:2], func=AF.Ln, bias=eps_c), reads=["mv", "eps_c"], writes=["mv2"])
        S.op("act", lambda e: e.activation(out=mv[:, 3:4], in_=mv[:, 2:3], func=AF.Exp, scale=-0.5), reads=["mv2"], writes=["mv3"])
        S.op("dve", lambda e: e.tensor_scalar(out=dst, in0=src, scalar1=mv[:, 0:1], scalar2=mv[:, 3:4], op0=ALU.subtract, op1=ALU.mult),
             reads=[rk, "mv", "mv3"], writes=[wk])
        S.op("dve", lambda e: e.tensor_tensor(out=dst, in0=dst, in1=gb, op=ALU.mult), reads=[wk] + gk, writes=[wk])
        S.op("dve", lambda e: e.tensor_tensor(out=dst, in0=dst, in1=bb, op=ALU.add), reads=[wk] + gk, writes=[wk])

    PA = [PS[0], PS[1]]; PTr = [PS[2], PS[3]]; PG = [PS[4], PS[5]]; PR = PS[6]
    PThi = PS[2].bitcast(BF16); PTlo = PS[3].bitcast(BF16)
    S.op("dve", lambda e: e.tensor_copy(out=Wrh, in_=Wr), reads=["Wr"], writes=["Wrh"])
    S.op("dve", lambda e: e.tensor_tensor(out=Wrl, in0=Wr, in1=Wrh, op=ALU.subtract), reads=["Wr", "Wrh"], writes=["Wrl"])
    hTb2 = [hTb, A.alloc("hTb_b", [128, 8, 128], BF16)]
    hTlo2 = [hTlo, A.alloc("hTlo_b", [128, 8, 128], BF16)]
    ple_sb = [A.alloc("ple_sb", [128, D], F32) for _ in range(2)]
    PP = PS[7]

    def p1(tt):
        b2 = tt % 2
        tsl = slice(tt * 128, (tt + 1) * 128)
        S.dma("sp", lambda e: e.dma_start(out=xt[b2], in_=x_loc[tsl, :]), writes=["xt%d" % b2])
        S.dma("pool", lambda e: e.dma_start(out=pTb[b2], in_=pT_loc[:, :, tsl]), writes=["pTb%d" % b2])
        for hf in range(2):
            for c in range(8):
                S.op("pe", lambda e, hf=hf, c=c: e.matmul(PA[hf], lhsT=mixT[:, c, tsl], rhs=Wout[:, c, hf * 512:(hf + 1) * 512],
                                                         start=(c == 0), stop=(c == 7)), reads=["mixT", "Wout"], writes=["PA%d" % hf])
            S.op("dve", lambda e, hf=hf: e.scalar_tensor_tensor(out=rt[:, hf * 512:(hf + 1) * 512], in0=xt[b2][:, hf * 512:(hf + 1) * 512],
                                                               scalar=ALPHA, in1=PA[hf], op0=ALU.mult, op1=ALU.add),
                 reads=["xt%d" % b2, "PA%d" % hf], writes=["rt"])
        for hf in range(2):
            for c in range(2):
                S.op("pe", lambda e, hf=hf, c=c: e.matmul(PP, lhsT=pTb[b2][:, c, :], rhs=Wple[:, c, hf * 512:(hf + 1) * 512],
                                                         start=(c == 0), stop=(c == 1)), reads=["pTb%d" % b2, "Wple"], writes=["PP"])
            S.op("act", lambda e, hf=hf: e.activation(out=ple_sb[b2][:, hf * 512:(hf + 1) * 512], in_=PP, func=AF.Copy),
                 reads=["PP"], writes=["ple%d_%d" % (b2, hf)])

    def ln_a(src, rk):
        for hf in range(2):
            S.op("dve", lambda e, hf=hf: e.bn_stats(out=st6[:, hf * 6:(hf + 1) * 6], in_=src[:, hf * 512:(hf + 1) * 512]),
                 reads=[rk], writes=["st6_%d" % hf])
        S.op("dve", lambda e: e.bn_aggr(out=mv[:, 0:2], in_=st6), reads=["st6_0", "st6_1"], writes=["mv"])
        S.op("act", lambda e: e.activation(out=mv[:, 2:3], in_=mv[:, 1:2], func=AF.Ln, bias=eps_c), reads=["mv", "eps_c"], writes=["mv2"])
        S.op("act", lambda e: e.activation(out=mv[:, 3:4], in_=mv[:, 2:3], func=AF.Exp, scale=-0.5), reads=["mv2"], writes=["mv3"])

    def ln_b(src, dst, gb, bb, rk, wk, gk):
        S.op("dve", lambda e: e.tensor_scalar(out=dst, in0=src, scalar1=mv[:, 0:1], scalar2=mv[:, 3:4], op0=ALU.subtract, op1=ALU.mult),
             reads=[rk, "mv", "mv3"], writes=[wk])
        S.op("dve", lambda e: e.tensor_tensor(out=dst, in0=dst, in1=gb, op=ALU.mult), reads=[wk] + gk, writes=[wk])
        S.op("dve", lambda e: e.tensor_tensor(out=dst, in0=dst, in1=bb, op=ALU.add), reads=[wk] + gk, writes=[wk])

    def p2a(tt):
        ln_a(rt, "rt")

    def p2(tt):
        b2 = tt % 2
        tsl = slice(tt * 128, (tt + 1) * 128)
        ln_b(rt, ht[b2], g1b, b1b, "rt", "ht%d" % b2, ["g1b", "b1b"])
        S.dma("sp", lambda e: e.dma_start(out=h_scr[tsl, :], in_=ht[b2]), reads=["ht%d" % b2], writes=["h_scr"])
        S.op("dve", lambda e: e.tensor_copy(out=hhi, in_=ht[b2]), reads=["ht%d" % b2], writes=["hhi"])
        S.op("dve", lambda e: e.tensor_tensor(out=hlo, in0=ht[b2], in1=hhi, op=ALU.subtract), reads=["ht%d" % b2, "hhi"], writes=["hlo"])

    def p3(tt):
        b2 = tt % 2
        for c in range(8):
            S.op("pe", lambda e, c=c: e.transpose(out=PThi[:, c * 128:(c + 1) * 128], in_=hhi[:, c * 128:(c + 1) * 128],
                                                 identity=c_bf[:, IDENT, :]), reads=["hhi", "c_bf"], writes=["PTr0"])
        for c in range(8):
            S.op("pe", lambda e, c=c: e.transpose(out=PTlo[:, c * 128:(c + 1) * 128], in_=hlo[:, c * 128:(c + 1) * 128],
                                                 identity=c_bf[:, IDENT, :]), reads=["hlo", "c_bf"], writes=["PTr1"])
        S.op("act", lambda e: e.activation(out=hTb2[b2].rearrange("p a b -> p (a b)"), in_=PThi, func=AF.Copy), reads=["PTr0"], writes=["hTb%d" % b2])
        S.op("dve", lambda e: e.tensor_copy(out=hTlo2[b2].rearrange("p a b -> p (a b)"), in_=PTlo), reads=["PTr1"], writes=["hTlo%d" % b2])

    def q_pe(tt):
        b2 = tt % 2
        k3 = 0
        for c in range(8):
            for (lt, ln_, rt_, rn_) in ((hTb2[b2], "hTb%d" % b2, Wrh, "Wrh"), (hTb2[b2], "hTb%d" % b2, Wrl, "Wrl"), (hTlo2[b2], "hTlo%d" % b2, Wrh, "Wrh")):
                S.op("pe", lambda e, c=c, lt=lt, rt_=rt_, k3=k3: e.matmul(PR[:, 0:36], lhsT=lt[:, c, :], rhs=rt_[:, c, :], start=(k3 == 0), stop=(k3 == 23)),
                     reads=[ln_, rn_], writes=["PR"])
                k3 += 1
        for hf in range(2):
            for c in range(8):
                S.op("pe", lambda e, hf=hf, c=c: e.matmul(PG[hf], lhsT=hTb2[b2][:, c, :], rhs=Wpg[:, c, hf * 512:(hf + 1) * 512],
                                                         start=(c == 0), stop=(c == 7)), reads=["hTb%d" % b2, "Wpg"], writes=["PG%d" % hf])
            S.op("act", lambda e, hf=hf: e.activation(out=sg[:, hf * 512:(hf + 1) * 512], in_=PG[hf], func=AF.Sigmoid),
                 reads=["PG%d" % hf], writes=["sg%d" % hf])

    def q_dve(tt):
        b2 = tt % 2
        tsl = slice(tt * 128, (tt + 1) * 128)
        S.op("dve", lambda e: e.tensor_tensor(out=L_all[:, tt * 36:(tt + 1) * 36], in0=PR[:, 0:36], in1=b36[:, tt * 36:(tt + 1) * 36], op=ALU.add),
             reads=["PR", "b36"], writes=["L_all"])
        for hf in range(2):
            S.op("dve", lambda e, hf=hf: e.tensor_tensor(out=sg[:, hf * 512:(hf + 1) * 512], in0=sg[:, hf * 512:(hf + 1) * 512],
                                                        in1=ple_sb[b2][:, hf * 512:(hf + 1) * 512], op=ALU.mult),
                 reads=["sg%d" % hf, "ple%d_%d" % (b2, hf)], writes=["sg%d" % hf])
        S.op("dve", lambda e: e.scalar_tensor_tensor(out=pre[b2], in0=ht[b2], scalar=ALPHA, in1=sg, op0=ALU.mult, op1=ALU.add),
             reads=["ht%d" % b2, "sg0", "sg1"], writes=["pre%d" % b2])
        S.dma("sp", lambda e: e.dma_start(out=pre_scr[tsl, :], in_=pre[b2]), reads=["pre%d" % b2], writes=["pre_scr"])

    p1(0)
    p2a(0)
    p2(0)
    p3(0)
    for tt in range(NTT):
        if tt + 1 < NTT:
            p1(tt + 1)
        q_pe(tt)
        if tt + 1 < NTT:
            p2a(tt + 1)
        q_dve(tt)
        if tt + 1 < NTT:
            p2(tt + 1)
            p3(tt + 1)

    if stop < 4:
        S.emit()
        return nc
    S.barrier()
    A.reset(PBASE)
    L3 = L_all.rearrange("p (t c) -> p t c", t=32, c=36)
    Lg = L3[:, :, 0:4]
    Le = L3[:, :, 4:36]
    gmax = A.alloc("gmax", [128, 32], F32)
    gm4 = A.alloc("gm4", [128, 32, 4], F32)
    eg = A.alloc("eg", [128, 32, 4], F32)
    gsum = A.alloc("gsum", [128, 32], F32)
    gp = A.alloc("gp", [128, 32], F32)
    Lm = A.alloc("Lm", [128, 32, 32], F32)
    top8 = A.alloc("top8", [128, 32, 8], F32)
    Oh0 = A.alloc("Oh0", [128, 32, 32], F32)
    Oh1 = A.alloc("Oh1", [128, 32, 32], F32)
    Oh2b = A.alloc("Oh2b", [128, 32 * 32], BF16)
    dv = A.alloc("dv", [128, 32], F32)
    ev = A.alloc("ev", [128, 32], F32)
    Ra = A.alloc("Ra", [128, 1024], F32)
    Rb = A.alloc("Rb", [128, 1024], F32)
    R0 = A.alloc("R0", [128, 1024], F32)
    cnt = A.alloc("cnt", [128, 32], F32)
    nb = A.alloc("nb", [128, 32], F32)
    pa = A.alloc("pa", [128, 32], F32)
    pb = A.alloc("pb", [128, 32], F32)
    pstart = A.alloc("pstart", [128, 32], F32)
    dfl = A.alloc("dfl", [128, 64], F32)
    cmp3 = A.alloc("cmp3", [128, NBLK, 32], F32)
    bexp = A.alloc("bexp", [128, NBLK], F32)
    idxf = A.alloc("idxf", [128, NBLK * 8], F32)

    def dv_(fn, reads, writes):
        S.op("dve", fn, reads=reads, writes=writes)

    dv_(lambda e: e.tensor_reduce(out=gmax, in_=Lg, axis=AX.X, op=ALU.max), ["L_all"], ["gmax"])
    gmax_b = gmax.unsqueeze(2).to_broadcast([128, 32, 4])
    dv_(lambda e: e.tensor_tensor(out=gm4, in0=Lg, in1=gmax_b, op=ALU.is_ge), ["L_all", "gmax"], ["gm4"])
    dv_(lambda e: e.tensor_tensor(out=eg, in0=Lg, in1=gmax_b, op=ALU.subtract), ["L_all", "gmax"], ["eg"])
    S.op("act", lambda e: e.activation(out=eg, in_=eg, func=AF.Exp), reads=["eg"], writes=["eg"])
    dv_(lambda e: e.tensor_reduce(out=gsum, in_=eg, axis=AX.X, op=ALU.add), ["eg"], ["gsum"])
    dv_(lambda e: e.reciprocal(out=gp, in_=gsum), ["gsum"], ["gp"])
    dv_(lambda e: e.tensor_scalar(out=gm4, in0=gm4, scalar1=1.0, scalar2=1e30, op0=ALU.subtract, op1=ALU.mult), ["gm4"], ["gm4"])
    dv_(lambda e: e.tensor_tensor(out=Lm.rearrange("p t (g k) -> p t g k", g=4, k=8), in0=Le.rearrange("p t (g k) -> p t g k", g=4, k=8),
                                  in1=gm4.unsqueeze(3).to_broadcast([128, 32, 4, 8]), op=ALU.add), ["L_all", "gm4"], ["Lm"])
    for t in range(32):
        dv_(lambda e, t=t: e.max(out=top8[:, t, :], in_=Lm[:, t, :]), ["Lm"], ["top8"])
    v0 = top8[:, :, 0]
    v1 = top8[:, :, 1]
    dv_(lambda e: e.tensor_tensor(out=Oh0, in0=Lm, in1=top8[:, :, 0:1].to_broadcast([128, 32, 32]), op=ALU.is_equal), ["Lm", "top8"], ["Oh0"])
    dv_(lambda e: e.tensor_tensor(out=Oh1, in0=Lm, in1=top8[:, :, 1:2].to_broadcast([128, 32, 32]), op=ALU.is_equal), ["Lm", "top8"], ["Oh1"])
    dv_(lambda e: e.tensor_tensor(out=dv, in0=v1, in1=v0, op=ALU.subtract), ["top8"], ["dv"])
    S.op("act", lambda e: e.activation(out=ev, in_=dv, func=AF.Exp), reads=["dv"], writes=["ev"])
    dv_(lambda e: e.tensor_scalar(out=dv, in0=ev, scalar1=1.0, scalar2=None, op0=ALU.add), ["ev"], ["dv"])
    dv_(lambda e: e.reciprocal(out=gsum, in_=dv), ["dv"], ["gsum"])
    dv_(lambda e: e.tensor_tensor(out=g0_all, in0=gsum, in1=gp, op=ALU.mult), ["gsum", "gp"], ["g0_all"])
    dv_(lambda e: e.tensor_tensor(out=g1_all, in0=g0_all, in1=ev, op=ALU.mult), ["g0_all", "ev"], ["g1_all"])
    dv_(lambda e: e.tensor_tensor(out=Oh2b.rearrange("p (t c) -> p t c", t=32, c=32), in0=Oh0, in1=Oh1, op=ALU.add), ["Oh0", "Oh1"], ["Oh2b"])
    for hf in range(2):
        S.op("pe", lambda e, hf=hf: e.matmul(PS[hf], lhsT=c_bf[:, STRICT, :], rhs=Oh2b[:, hf * 512:(hf + 1) * 512], start=True, stop=True),
             reads=["c_bf", "Oh2b"], writes=["ps%d" % hf])
        S.op("pe", lambda e, hf=hf: e.matmul(PS[2 + hf], lhsT=c_bf[:, ONES, :], rhs=Oh2b[:, hf * 512:(hf + 1) * 512], start=True, stop=True),
             reads=["c_bf", "Oh2b"], writes=["ps%d" % (2 + hf)])
        dv_(lambda e, hf=hf: e.tensor_copy(out=R0[:, hf * 512:(hf + 1) * 512], in_=PS[2 + hf]), ["ps%d" % (2 + hf)], ["R0"])
        dv_(lambda e, hf=hf: e.tensor_copy(out=Ra[:, hf * 512:(hf + 1) * 512], in_=PS[2 + hf]), ["ps%d" % (2 + hf)], ["Ra"])
    cur, oth, cn, on = Ra, Rb, "Ra", "Rb"
    for dsh in (1, 2, 4, 8, 16):
        w_ = 32 * dsh
        dv_(lambda e, cur=cur, oth=oth, w_=w_: e.tensor_tensor(out=oth[:, w_:], in0=cur[:, w_:], in1=cur[:, :1024 - w_], op=ALU.add), [cn], [on])
        dv_(lambda e, cur=cur, oth=oth, w_=w_: e.tensor_copy(out=oth[:, :w_], in_=cur[:, :w_]), [cn, on], [on])
        cur, oth, cn, on = oth, cur, on, cn
    Rincl, rin = cur, cn
    Rk, rkn = oth, on
    dv_(lambda e: e.tensor_copy(out=cnt, in_=Rincl[:, 31 * 32:32 * 32]), [rin], ["cnt"])
    dv_(lambda e: e.tensor_tensor(out=Rk, in0=Rincl, in1=R0, op=ALU.subtract), [rin, "R0"], [rkn])
    for hf in range(2):
        dv_(lambda e, hf=hf: e.tensor_tensor(out=Rk[:, hf * 512:(hf + 1) * 512], in0=Rk[:, hf * 512:(hf + 1) * 512], in1=PS[hf], op=ALU.add),
            [rkn, "ps%d" % hf], [rkn])
    dv_(lambda e: e.memset(nb, 0.0), [], ["nb"])
    for j in range(32):
        dv_(lambda e, j=j: e.scalar_tensor_tensor(out=nb, in0=cnt, scalar=float(128 * j), in1=nb, op0=ALU.is_gt, op1=ALU.add), ["cnt", "nb"], ["nb"])
    dv_(lambda e: e.tensor_scalar(out=nb, in0=nb, scalar1=128.0, scalar2=None, op0=ALU.mult), ["nb"], ["nb"])
    dv_(lambda e: e.tensor_copy(out=pa, in_=nb), ["nb"], ["pa"])
    cur, oth, cn, on = pa, pb, "pa", "pb"
    for dsh in (1, 2, 4, 8, 16):
        dv_(lambda e, cur=cur, oth=oth, dsh=dsh: e.tensor_tensor(out=oth[:, dsh:], in0=cur[:, dsh:], in1=cur[:, :32 - dsh], op=ALU.add), [cn], [on])
        dv_(lambda e, cur=cur, oth=oth, dsh=dsh: e.tensor_copy(out=oth[:, :dsh], in_=cur[:, :dsh]), [cn, on], [on])
        cur, oth, cn, on = oth, cur, on, cn
    pend, pen_n = cur, cn
    dv_(lambda e: e.tensor_tensor(out=pstart, in0=pend, in1=nb, op=ALU.subtract), [pen_n, "nb"], ["pstart"])
    Rk3 = Rk.rearrange("p (t c) -> p t c", t=32, c=32)
    dv_(lambda e: e.tensor_tensor(out=Rk3, in0=Rk3, in1=pstart.unsqueeze(1).to_broadcast([128, 32, 32]), op=ALU.add), [rkn, "pstart"], [rkn])
    dv_(lambda e: e.tensor_tensor(out=Oh0, in0=Oh0, in1=Rk3, op=ALU.mult), ["Oh0", rkn], ["Oh0"])
    dv_(lambda e: e.tensor_tensor(out=Oh1, in0=Oh1, in1=Rk3, op=ALU.mult), ["Oh1", rkn], ["Oh1"])
    dv_(lambda e: e.tensor_reduce(out=dfl[:, 0:32], in_=Oh0, axis=AX.X, op=ALU.add), ["Oh0"], ["dfl0"])
    dv_(lambda e: e.tensor_reduce(out=dfl[:, 32:64], in_=Oh1, axis=AX.X, op=ALU.add), ["Oh1"], ["dfl1"])
    dv_(lambda e: e.tensor_copy(out=dest0_i, in_=dfl[:, 0:32]), ["dfl0"], ["dest0_i"])
    dv_(lambda e: e.tensor_copy(out=dest1_i, in_=dfl[:, 32:64]), ["dfl1"], ["dest1_i"])
    hrow = [A.alloc("hrow", [128, D], F32) for _ in range(4)]
    for tt in range(NTT):
        b2 = tt % 4
        tsl = slice(tt * 128, (tt + 1) * 128)
        S.dma("sp", lambda e, b2=b2, tsl=tsl: e.dma_start(out=hrow[b2], in_=h_scr[tsl, :]), reads=["h_scr"], writes=["hrow%d" % b2])
        for k, di in enumerate((dest0_i, dest1_i)):
            S.dma("pool", lambda e, b2=b2, tt=tt, di=di: e.indirect_dma_start(
                out=xs_scr, out_offset=bass.IndirectOffsetOnAxis(ap=di[:, tt:tt + 1], axis=0), in_=hrow[b2], in_offset=None),
                reads=["hrow%d" % b2, "dest0_i", "dest1_i"], writes=["xs_scr%d" % k])
    thr = iot_sb[:, 44:140]
    dv_(lambda e: e.tensor_tensor(out=cmp3, in0=pend.unsqueeze(1).to_broadcast([128, NBLK, 32]), in1=thr.unsqueeze(2).to_broadcast([128, NBLK, 32]),
                                  op=ALU.is_le), [pen_n, "iot"], ["cmp3"])
    dv_(lambda e: e.tensor_reduce(out=bexp, in_=cmp3, axis=AX.X, op=ALU.add), ["cmp3"], ["bexp"])
    dv_(lambda e: e.tensor_scalar(out=bexp, in0=bexp, scalar1=31.0, scalar2=None, op0=ALU.min), ["bexp"], ["bexp"])
    chg = idxf[:, 0:NBLK]
    e2 = idxf[:, NBLK:2 * NBLK]
    dv_(lambda e: e.memset(chg[:, 0:1], 1.0), [], ["chg0"])
    dv_(lambda e: e.tensor_tensor(out=chg[:, 1:NBLK], in0=bexp[:, 1:NBLK], in1=bexp[:, 0:NBLK - 1], op=ALU.not_equal), ["bexp"], ["chg1"])
    dv_(lambda e: e.tensor_scalar(out=chg, in0=chg, scalar1=-1.0e7, scalar2=1.0e7, op0=ALU.mult, op1=ALU.add), ["chg0", "chg1"], ["chg"])
    dv_(lambda e: e.tensor_scalar(out=e2, in0=bexp, scalar1=128.0, scalar2=None, op0=ALU.mult), ["bexp"], ["e2"])
    dv_(lambda e: e.tensor_tensor(out=e2, in0=e2, in1=chg, op=ALU.add), ["e2", "chg"], ["e2"])
    dv_(lambda e: e.scalar_tensor_tensor(out=e2, in0=iot_sb[:, 0:1].to_broadcast([128, NBLK]), scalar=1.0, in1=e2, op0=ALU.mult, op1=ALU.add),
        ["e2", "iot"], ["e2"])
    dv_(lambda e: e.tensor_copy(out=idxA, in_=e2), ["e2"], ["idxA"])
    dv_(lambda e: e.tensor_scalar(out=e2, in0=e2, scalar1=1.0, scalar2=None, op0=ALU.add), ["e2", "idxA"], ["e2"])
    dv_(lambda e: e.tensor_copy(out=idxB, in_=e2), ["e2"], ["idxB"])

    if stop < 5:
        S.emit()
        return nc

    if stop < 6:
        S.emit()
        return nc
    S.barrier()
    xb = [A.alloc("xb", [128, D], BF16) for _ in range(2)]
    xTk = [A.alloc("xTk", [128, 8, 128], BF16) for _ in range(2)]
    Wg = A.alloc("Wg", [128, 8, 512], BF16)
    Wu = A.alloc("Wu", [128, 8, 512], BF16)
    Wd = A.alloc("Wd", [128, 4, 1024], BF16)
    Wgs = A.alloc("Wgs", [128, 4096], F32)
    Wus = A.alloc("Wus", [128, 4096], F32)
    Wds = A.alloc("Wds", [128, 4096], F32)
    sil = A.alloc("sil", [128, 512], F32)
    hdn = A.alloc("hdn", [128, 512], BF16)
    hdT = A.alloc("hdT", [128, 4, 128], BF16)
    yb = [A.alloc("yb", [128, D], F32) for _ in range(2)]
    breg = {}

    def bound_reg(e):
        if "r" not in breg:
            r = e.alloc_register("wbound")
            e.reg_mov(r, 32 * 128 - 1)
            breg["r"] = r
        return breg["r"]

    PTb = PS[0].bitcast(BF16)
    PGa, PUa = PS[1], PS[2]
    PTh = PS[3].bitcast(BF16)
    PY = [PS[4], PS[5]]
    def wload(b, which):
        for (wt, ws, wn, src) in which:
            wflat = wt.rearrange("p a b -> p (a b)")
            S.dma("pool", lambda e, ws=ws, src=src: e.indirect_dma_start(
                out=ws, out_offset=None, in_=src,
                in_offset=bass.IndirectOffsetOnAxis(ap=idxA[:, b:b + 1], axis=0), bounds_check=bound_reg(e), oob_is_err=False),
                reads=["idxA"], writes=[wn + "s"])
            S.op("act", lambda e, wflat=wflat, ws=ws: e.activation(out=wflat[:, 0:2048], in_=ws[:, 0:2048], func=AF.Copy),
                 reads=[wn + "s"], writes=[wn + "a"])
            S.op("dve", lambda e, wflat=wflat, ws=ws: e.tensor_copy(out=wflat[:, 2048:4096], in_=ws[:, 2048:4096]),
                 reads=[wn + "s"], writes=[wn + "b"])

    WG = ((Wg, Wgs, "Wg", w_gate),)
    WU = ((Wu, Wus, "Wu", w_up),)
    WD = ((Wd, Wds, "Wd", w_down),)

    def stA(b):
        b2 = b % 2
        rsl = slice(b * 128, (b + 1) * 128)
        S.dma("sp", lambda e: e.dma_start(out=xb[b2], in_=xs_scr[rsl, :]), reads=["xs_scr0", "xs_scr1"], writes=["xb%d" % b2])
        for c in range(8):
            S.op("pe", lambda e, c=c: e.transpose(out=PTb[:, c * 128:(c + 1) * 128], in_=xb[b2][:, c * 128:(c + 1) * 128], identity=c_bf[:, IDENT, :]),
                 reads=["xb%d" % b2, "c_bf"], writes=["PTb"])
        S.op("dve", lambda e: e.tensor_copy(out=xTk[b2].rearrange("p a b -> p (a b)"), in_=PTb), reads=["PTb"], writes=["xTk%d" % b2])

    def stG(b):
        b2 = b % 2
        for c in range(8):
            S.op("pe", lambda e, c=c: e.matmul(PGa, lhsT=xTk[b2][:, c, :], rhs=Wg[:, c, :], start=(c == 0), stop=(c == 7)),
                 reads=["xTk%d" % b2, "Wga", "Wgb"], writes=["PGa"])
        S.op("act", lambda e: e.activation(out=sil, in_=PGa, func=AF.Silu), reads=["PGa"], writes=["sil"])

    def stU(b):
        b2 = b % 2
        for c in range(8):
            S.op("pe", lambda e, c=c: e.matmul(PUa, lhsT=xTk[b2][:, c, :], rhs=Wu[:, c, :], start=(c == 0), stop=(c == 7)),
                 reads=["xTk%d" % b2, "Wua", "Wub"], writes=["PUa"])
        S.op("dve", lambda e: e.tensor_tensor(out=hdn, in0=sil, in1=PUa, op=ALU.mult), reads=["sil", "PUa"], writes=["hdn"])

    def stC1(b):
        for c in range(4):
            S.op("pe", lambda e, c=c: e.transpose(out=PTh[:, c * 128:(c + 1) * 128], in_=hdn[:, c * 128:(c + 1) * 128], identity=c_bf[:, IDENT, :]),
                 reads=["hdn", "c_bf"], writes=["PTh"])
        S.op("dve", lambda e: e.tensor_copy(out=hdT.rearrange("p a b -> p (a b)"), in_=PTh[:, 0:512]), reads=["PTh"], writes=["hdT"])

    def stC2(b):
        b2 = b % 2
        rsl = slice(b * 128, (b + 1) * 128)
        for hf in range(2):
            for c in range(4):
                S.op("pe", lambda e, hf=hf, c=c: e.matmul(PY[hf], lhsT=hdT[:, c, :], rhs=Wd[:, c, hf * 512:(hf + 1) * 512],
                                                         start=(c == 0), stop=(c == 3)), reads=["hdT", "Wda", "Wdb"], writes=["PY%d" % hf])
            evac(yb[b2][:, hf * 512:(hf + 1) * 512], PY[hf], ["PY%d" % hf], ["yb%d_%d" % (b2, hf)])
        S.dma("sp", lambda e: e.dma_start(out=ys_scr[rsl, :], in_=yb[b2]), reads=["yb%d_0" % b2, "yb%d_1" % b2], writes=["ys_scr"])

    stA(0)
    wload(0, WG)
    wload(0, WU)
    for b in range(NBLK):
        if b + 1 < NBLK:
            stA(b + 1)
        if b > 0:
            stC1(b - 1)
        stG(b)
        if b + 1 < NBLK:
            wload(b + 1, WG)
        if b > 0:
            stC2(b - 1)
        wload(b, WD)
        stU(b)
        if b + 1 < NBLK:
            wload(b + 1, WU)
    stC1(NBLK - 1)
    stC2(NBLK - 1)

    if stop < 7:
        S.emit()
        return nc
    S.barrier()
    A.reset(PBASE)
    g2b = A.alloc("g2b", [128, D], F32); b2b = A.alloc("b2b", [128, D], F32)
    NB3 = 3
    y0 = [A.alloc("y0", [128, D], F32) for _ in range(NB3)]
    y1 = [A.alloc("y1", [128, D], F32) for _ in range(NB3)]
    pr = [A.alloc("pr", [128, D], F32) for _ in range(NB3)]
    ot = [A.alloc("ot", [128, D], F32) for _ in range(2)]
    st7 = [A.alloc("st7", [128, 12], F32) for _ in range(2)]
    mv7 = [A.alloc("mv7", [128, 4], F32) for _ in range(2)]
    S.dma("sp", lambda e: e.dma_start(out=g2b, in_=ln2g.partition_broadcast(128)), writes=["g2b"])
    S.dma("sp", lambda e: e.dma_start(out=b2b, in_=ln2b.partition_broadcast(128)), writes=["b2b"])

    def ld7(tt):
        b3 = tt % NB3
        tsl = slice(tt * 128, (tt + 1) * 128)
        S.dma("sp", lambda e: e.dma_start(out=pr[b3], in_=pre_scr[tsl, :]), reads=["pre_scr"], writes=["pr%d" % b3])
        S.dma("pool", lambda e: e.indirect_dma_start(
            out=y0[b3], out_offset=None, in_=ys_scr, in_offset=bass.IndirectOffsetOnAxis(ap=dest0_i[:, tt:tt + 1], axis=0)),
            reads=["ys_scr", "dest0_i"], writes=["y0_%d" % b3])
        S.dma("pool", lambda e: e.indirect_dma_start(
            out=y1[b3], out_offset=None, in_=ys_scr, in_offset=bass.IndirectOffsetOnAxis(ap=dest1_i[:, tt:tt + 1], axis=0)),
            reads=["ys_scr", "dest1_i"], writes=["y1_%d" % b3])

    def cmb7(tt):
        b3 = tt % NB3; m2 = tt % 2
        S.op("dve", lambda e: e.scalar_tensor_tensor(out=pr[b3], in0=y0[b3], scalar=g0_all[:, tt:tt + 1], in1=pr[b3], op0=ALU.mult, op1=ALU.add),
             reads=["y0_%d" % b3, "pr%d" % b3, "g0_all"], writes=["pr%d" % b3])
        S.op("dve", lambda e: e.scalar_tensor_tensor(out=pr[b3], in0=y1[b3], scalar=g1_all[:, tt:tt + 1], in1=pr[b3], op0=ALU.mult, op1=ALU.add),
             reads=["y1_%d" % b3, "pr%d" % b3, "g1_all"], writes=["pr%d" % b3])
        for hf in range(2):
            S.op("dve", lambda e, hf=hf: e.bn_stats(out=st7[m2][:, hf * 6:(hf + 1) * 6], in_=pr[b3][:, hf * 512:(hf + 1) * 512]),
                 reads=["pr%d" % b3], writes=["st7_%d_%d" % (m2, hf)])
        S.op("dve", lambda e: e.bn_aggr(out=mv7[m2][:, 0:2], in_=st7[m2]), reads=["st7_%d_0" % m2, "st7_%d_1" % m2], writes=["mv7a%d" % m2])
        S.op("act", lambda e: e.activation(out=mv7[m2][:, 2:3], in_=mv7[m2][:, 1:2], func=AF.Ln, bias=eps_c), reads=["mv7a%d" % m2, "eps_c"], writes=["mv7b%d" % m2])
        S.op("act", lambda e: e.activation(out=mv7[m2][:, 3:4], in_=mv7[m2][:, 2:3], func=AF.Exp, scale=-0.5), reads=["mv7b%d" % m2], writes=["mv7c%d" % m2])

    def fin7(tt):
        b3 = tt % NB3; m2 = tt % 2
        tsl = slice(tt * 128, (tt + 1) * 128)
        S.op("dve", lambda e: e.tensor_scalar(out=ot[m2], in0=pr[b3], scalar1=mv7[m2][:, 0:1], scalar2=mv7[m2][:, 3:4], op0=ALU.subtract, op1=ALU.mult),
             reads=["pr%d" % b3, "mv7a%d" % m2, "mv7c%d" % m2], writes=["ot%d" % m2])
        S.op("dve", lambda e: e.tensor_tensor(out=ot[m2], in0=ot[m2], in1=g2b, op=ALU.mult), reads=["ot%d" % m2, "g2b"], writes=["ot%d" % m2])
        S.op("dve", lambda e: e.tensor_tensor(out=ot[m2], in0=ot[m2], in1=b2b, op=ALU.add), reads=["ot%d" % m2, "b2b"], writes=["ot%d" % m2])
        S.dma("sp", lambda e: e.dma_start(out=out[tsl, :], in_=ot[m2]), reads=["ot%d" % m2], writes=["out"], is_output=True)

    ld7(0)
    ld7(1)
    for tt in range(NTT):
        cmb7(tt)
        if tt > 0:
            fin7(tt - 1)
        if tt + 2 < NTT:
            ld7(tt + 2)
    fin7(NTT - 1)

    S.emit()
    return nc


_CACHE = {}


def _prep_inputs(x, p, w_in, b_forget, w_out, ln_mix_g, ln_mix_b, w_group, b_group, w_router, b_router,
                 w_gate, w_up, w_down, w_ple, w_ple_gate, ln_ffn_g, ln_ffn_b):
    f32 = np.float32
    x = np.asarray(x, f32); p = np.asarray(p, f32)
    w_in = np.asarray(w_in, f32)[0]
    kp_cols, qp_cols = [], []
    for h in range(8):
        kp_cols += list(range(512 + 64 * h, 512 + 64 * h + 64)) + list(range(2048 + 64 * h, 2048 + 64 * h + 64))
        qp_cols += list(range(0 + 64 * h, 64 * h + 64)) + list(range(1536 + 64 * h, 1536 + 64 * h + 64))
    v_cols = list(range(1024, 1536)) + list(range(2560, 3072))

    def pcl(w):
        return np.ascontiguousarray(w.reshape(8, 128, -1).transpose(1, 0, 2))

    shared = {
        "w_kp": pcl(w_in[:, kp_cols]), "w_qp": pcl(w_in[:, qp_cols]), "w_v": pcl(w_in[:, v_cols]),
        "w_f": pcl(w_in[:, 3072:3080]),
        "bf_t": np.ascontiguousarray(np.tile(np.asarray(b_forget, f32)[0], 64).reshape(1, 512)),
        "w_out": pcl(np.asarray(w_out, f32)[0]),
        "ln1g": np.asarray(ln_mix_g, f32).reshape(1, D), "ln1b": np.asarray(ln_mix_b, f32).reshape(1, D),
        "ln2g": np.asarray(ln_ffn_g, f32).reshape(1, D), "ln2b": np.asarray(ln_ffn_b, f32).reshape(1, D),
        "w_r36": pcl(np.concatenate([np.asarray(w_group, f32)[0], np.asarray(w_router, f32)[0]], axis=1)),
        "b_r36": np.ascontiguousarray(np.tile(np.concatenate([np.asarray(b_group, f32)[0], np.asarray(b_router, f32)[0]]), 32).reshape(1, 32 * 36)),
        "w_gate": np.ascontiguousarray(np.asarray(w_gate, f32)[0].reshape(32, 8, 128, 512).transpose(0, 2, 1, 3).reshape(32 * 128, 4096)),
        "w_up": np.ascontiguousarray(np.asarray(w_up, f32)[0].reshape(32, 8, 128, 512).transpose(0, 2, 1, 3).reshape(32 * 128, 4096)),
        "w_down": np.ascontiguousarray(np.asarray(w_down, f32)[0].reshape(32, 4, 128, 1024).transpose(0, 2, 1, 3).reshape(32 * 128, 4096)),
        "w_ple": np.ascontiguousarray(np.asarray(w_ple, f32)[0].reshape(2, 128, 1024).transpose(1, 0, 2)),
        "w_pg": pcl(np.asarray(w_ple_gate, f32)[0]),
    }
    k = np.arange(128)[:, None]; q = np.arange(128)[None, :]
    cst = np.zeros((128, 7, 128), f32)
    cst[:, 6, :] = -1.0
    cst[:, 0, :] = (k == q)
    cst[:, 1, :] = (k <= q)
    cst[:, 2, :] = (k < q)
    cst[:, 3, :] = 1.0
    cst[:, 4, :] = -(k >= q).astype(f32)
    cst[:, 5, :] = -(k == q).astype(f32)
    shared["consts"] = cst
    iot = np.zeros((128, 140), f32)
    iot[:, 0:12] = np.arange(12)[None, :] * 128 + np.arange(128)[:, None]
    iot[:, 44:140] = np.arange(96)[None, :] * 128.0
    shared["iot"] = iot

    def diag_tiles(strict):
        t = np.zeros((4, 128, 512), f32)
        for i in range(4):
            for jq in range(4):
                blk = t[i, :, jq * 128:(jq + 1) * 128]
                if jq < i:
                    blk[:] = BIG
                elif jq == i:
                    blk[:] = np.where((k < q) if strict else (k <= q), 0.0, BIG)
        return t

    in_maps = []
    for c in range(NCORE):
        b, par = c // 2, c % 2
        G = G_PAR[par]
        loc = np.concatenate([np.arange(g * 512, (g + 1) * 512) for g in G])
        xT = np.ascontiguousarray(x[b].reshape(SEQ, 8, 128).transpose(2, 1, 0))
        m = dict(shared)
        m["xT_all"] = xT
        m["xT_loc"] = np.ascontiguousarray(xT[:, :, loc])
        m["x_loc"] = np.ascontiguousarray(x[b][loc])
        m["pT_loc"] = np.ascontiguousarray(p[0, b][loc].reshape(NLOC, 2, 128).transpose(2, 1, 0))
        mk = np.zeros((2, 2, 8, 128, 512), f32)
        for kind in range(2):
            dt_ = diag_tiles(strict=(kind == 0))
            for sp_ in range(2):
                has_max = (sp_ == 0) if par == 1 else (sp_ == 1)
                if has_max:
                    mk[kind, sp_, 4:8] = dt_
                else:
                    mk[kind, sp_, 0:4] = dt_
                    mk[kind, sp_, 4:8] = BIG
        m["masks"] = np.ascontiguousarray(mk.reshape(32, 128, 512).transpose(1, 0, 2))
        sel = np.zeros((8, 16, 8), f32)
        for s_, g in enumerate(G):
            sel[s_, g, :] = 1.0
        m["selx"] = sel.reshape(1, 1024)
        in_maps.append(m)
    return in_maps


def kernel(**inputs):
    if "nc" not in _CACHE:
        _CACHE["nc"] = build()
    nc = _CACHE["nc"]
    in_maps = _prep_inputs(**inputs)
    res = run_bass_kernel_spmd(nc, in_maps, core_ids=list(range(NCORE)))
    outp = np.zeros((NB, SEQ, D), np.float32)
    for c in range(NCORE):
        b, par = c // 2, c % 2
        loc = np.concatenate([np.arange(g * 512, (g + 1) * 512) for g in G_PAR[par]])
        outp[b, loc] = res.results[c]["out"]
    return outp
```
